# Optimizing a Trainium2 kernel written in Bass

```python
import math
import jax
import jax.numpy as jnp
from jax import lax
import numpy as np

D_MODEL = 2048
BATCH = 4
SEQ = 2048
DEPTH = 2

N_HEADS = 16
HEAD_DIM = 128
ATT_WIDTH = N_HEADS * HEAD_DIM
ATT_SCALE = HEAD_DIM ** -0.5
DIL_PATTERNS = ((128, 1), (512, 4), (2048, 16))
BAND_BLOCK = 128
REL_BUCKETS = 32
REL_MAX_EXACT = REL_BUCKETS // 2
REL_MAX_DISTANCE = 2048
NSA_KV_GROUPS = 4
NSA_HEADS_PER_GROUP = N_HEADS // NSA_KV_GROUPS
NSA_BRANCHES = 3
CMP_BLOCK = 32
CMP_STRIDE = 16
CMP_HIDDEN = 256
SLC_BLOCK = 64
SLC_TOP_N = 16
SLC_QUERY_BLOCK = 32
WIN_SIZE = 512
N_A_LAYERS = DEPTH // 2
N_B_LAYERS = DEPTH - N_A_LAYERS
RMS_EPS = 1e-6
NEG_INF = -1e30
FORCE_SCORE = 1e9

kernel_name = 'yoco_dilated_nsa_hybrid'


def rms_norm(x, g):
    xf = x.astype(jnp.float32)
    y = xf * lax.rsqrt(jnp.mean(xf * xf, axis=-1, keepdims=True) + RMS_EPS)
    return (y * g.astype(jnp.float32)).astype(x.dtype)


def rel_bucket(dist):
    n = jnp.maximum(dist, 0)
    nf = jnp.maximum(n, 1).astype(jnp.float32)
    log_b = REL_MAX_EXACT + (jnp.log(nf / REL_MAX_EXACT) / math.log(REL_MAX_DISTANCE / REL_MAX_EXACT)
                             * (REL_BUCKETS - REL_MAX_EXACT)).astype(jnp.int32)
    return jnp.where(n < REL_MAX_EXACT, n, jnp.minimum(log_b, REL_BUCKETS - 1))


def rel_bias(table, dist):
    b = jnp.take(table, rel_bucket(dist), axis=0).astype(jnp.float32)
    return jnp.moveaxis(b, -1, 0)


def dilated_attention(q, k, v, table, window, dilation):
    B, H, S, Dh = q.shape
    L = S // dilation
    n_keys = window // dilation
    nb = -(-L // BAND_BLOCK)
    Lp = nb * BAND_BLOCK

    def to_sub(t):
        t = t.reshape(B, H, L, dilation, Dh).transpose(0, 1, 3, 2, 4)
        return jnp.pad(t, ((0, 0), (0, 0), (0, 0), (0, Lp - L), (0, 0)))

    def band(t):
        tp = jnp.pad(t, ((0, 0), (0, 0), (0, 0), (BAND_BLOCK, 0), (0, 0)))
        tp = tp.reshape(B, H, dilation, nb + 1, BAND_BLOCK, Dh)
        return jnp.concatenate([tp[:, :, :, :-1], tp[:, :, :, 1:]], axis=4)

    qb = to_sub(q).reshape(B, H, dilation, nb, BAND_BLOCK, Dh)
    kb = band(to_sub(k))
    vb = band(to_sub(v))
    qi = jnp.arange(BAND_BLOCK)[:, None]
    ki = jnp.arange(2 * BAND_BLOCK)[None, :]
    delta = qi + BAND_BLOCK - ki
    key_pos = jnp.arange(nb)[:, None, None] * BAND_BLOCK - BAND_BLOCK + ki[None]
    valid = (delta >= 0) & (delta <= n_keys) & (key_pos >= 0)
    bias = rel_bias(table, delta * dilation)
    s = jnp.einsum('bhrnqd,bhrnkd->bhrnqk', qb, kb).astype(jnp.float32) * ATT_SCALE + bias[None, :, None, None]
    s = jnp.where(valid, s, NEG_INF)
    m = jnp.max(s, axis=-1, keepdims=True)
    p = jnp.exp(s - m)
    den = jnp.sum(p, axis=-1, keepdims=True)
    o = jnp.einsum('bhrnqk,bhrnkd->bhrnqd', p.astype(vb.dtype), vb).astype(jnp.float32) / den
    lse = (m + jnp.log(den))[..., 0]
    o = o.reshape(B, H, dilation, Lp, Dh)[:, :, :, :L].transpose(0, 1, 3, 2, 4).reshape(B, H, S, Dh)
    lse = lse.reshape(B, H, dilation, Lp)[..., :L].transpose(0, 1, 3, 2).reshape(B, H, S)
    return o, lse


def dilated_mixer(h, w_in, w_out, table):
    B, S, _ = h.shape
    q, k, v, z = jnp.split(h @ w_in, 4, axis=-1)

    def heads(t):
        return t.reshape(B, S, N_HEADS, HEAD_DIM).transpose(0, 2, 1, 3)

    q, k, v = heads(q), heads(k), heads(v)
    outs, lses = [], []
    for window, dilation in DIL_PATTERNS:
        o, l = dilated_attention(q, k, v, table, window, dilation)
        outs.append(o)
        lses.append(l)
    alpha = jax.nn.softmax(jnp.stack(lses), axis=0)
    o = jnp.einsum('pbhs,pbhsd->bshd', alpha, jnp.stack(outs)).reshape(B, S, ATT_WIDTH).astype(h.dtype)
    return (o * jax.nn.silu(z)) @ w_out


def compress_blocks(t, pos, w1, w2):
    B, G, S, Dh = t.shape
    c = t.reshape(B, G, S // CMP_STRIDE, CMP_STRIDE, Dh)
    blocks = jnp.concatenate([c[:, :, :-1], c[:, :, 1:]], axis=3) + pos
    flat = blocks.reshape(B, G, blocks.shape[2], CMP_BLOCK * Dh)
    return jax.nn.gelu(flat @ w1) @ w2


def shared_kv(h, kv_norm, w_kv, cmp_pos_k, cmp_pos_v, cmp_w1_k, cmp_w2_k, cmp_w1_v, cmp_w2_v):
    B, S, _ = h.shape
    kv = (rms_norm(h, kv_norm) @ w_kv).reshape(B, S, 2 * NSA_BRANCHES, NSA_KV_GROUPS, HEAD_DIM)
    kv = kv.transpose(2, 0, 3, 1, 4)
    k_cmp = compress_blocks(kv[0], cmp_pos_k, cmp_w1_k, cmp_w2_k)
    v_cmp = compress_blocks(kv[1], cmp_pos_v, cmp_w1_v, cmp_w2_v)
    return k_cmp, v_cmp, kv[2], kv[3], kv[4], kv[5]


def nsa_mixer(h, w_in, w_out, table, k_cmp, v_cmp, k_slc, v_slc, k_win, v_win):
    B, S, _ = h.shape
    G, J, Dh = NSA_KV_GROUPS, NSA_HEADS_PER_GROUP, HEAD_DIM
    f32 = jnp.float32
    proj = h @ w_in
    q = proj[..., :ATT_WIDTH].reshape(B, S, G, J, Dh).transpose(0, 2, 3, 1, 4)
    z = proj[..., ATT_WIDTH:(1 + NSA_BRANCHES) * ATT_WIDTH].reshape(B, S, NSA_BRANCHES, N_HEADS, Dh)
    gate = jax.nn.sigmoid(proj[..., (1 + NSA_BRANCHES) * ATT_WIDTH:].astype(f32)).reshape(B, S, NSA_BRANCHES, N_HEADS)
    t = jnp.arange(S)

    nc = k_cmp.shape[2]
    cmp_end = jnp.arange(nc) * CMP_STRIDE + CMP_BLOCK - 1
    dist_c = t[:, None] - cmp_end[None, :]
    valid_c = dist_c >= 0
    bias_c = rel_bias(table, dist_c).reshape(G, J, S, nc)
    s_c = jnp.einsum('bgjsd,bgcd->bgjsc', q, k_cmp).astype(f32) * ATT_SCALE + bias_c
    s_c = jnp.where(valid_c, s_c, NEG_INF)
    p_c = jnp.where(valid_c, jnp.exp(s_c - jnp.max(s_c, axis=-1, keepdims=True)), 0.0)
    p_c = p_c / jnp.maximum(jnp.sum(p_c, axis=-1, keepdims=True), 1e-30)
    o_c = jnp.einsum('bgjsc,bgcd->bgjsd', p_c.astype(v_cmp.dtype), v_cmp)

    ns = S // SLC_BLOCK
    n_sel = min(SLC_TOP_N, ns)
    ci = np.arange(nc)[:, None] * CMP_STRIDE
    sj = np.arange(ns)[None, :] * SLC_BLOCK
    overlap = jnp.asarray(((ci < sj + SLC_BLOCK) & (ci + CMP_BLOCK > sj)).astype(np.float32))
    imp = jnp.einsum('bgjsc,cn->bgsn', p_c, overlap)
    cur = (t // SLC_BLOCK)[:, None]
    blk = jnp.arange(ns)[None, :]
    forced = (blk == 0) | (blk == cur) | (blk == cur - 1)
    imp = jnp.where(forced, FORCE_SCORE, jnp.where(blk > cur, -FORCE_SCORE, imp))
    _, sel = lax.top_k(imp, n_sel)

    kb = k_slc.reshape(B, G, ns, SLC_BLOCK, Dh)
    vb = v_slc.reshape(B, G, ns, SLC_BLOCK, Dh)
    nq = S // SLC_QUERY_BLOCK
    q_blocks = q.reshape(B, G, J, nq, SLC_QUERY_BLOCK, Dh).transpose(3, 0, 1, 2, 4, 5)
    sel_blocks = sel.reshape(B, G, nq, SLC_QUERY_BLOCK, n_sel).transpose(2, 0, 1, 3, 4)
    starts = jnp.arange(nq) * SLC_QUERY_BLOCK
    table_g = table.reshape(REL_BUCKETS, G, J)
    gather = jax.vmap(jax.vmap(lambda blocks, ix: blocks[ix]))
    group_bias = jax.vmap(lambda tb, bk: tb[bk], in_axes=(1, 1), out_axes=1)

    def selected_block(args):
        qb_, ix, start = args
        kg = gather(kb, ix)
        vg = gather(vb, ix)
        tq = start + jnp.arange(SLC_QUERY_BLOCK)
        dist = tq[:, None, None] - (ix[..., None] * SLC_BLOCK + jnp.arange(SLC_BLOCK))
        bias = jnp.moveaxis(group_bias(table_g, rel_bucket(dist)), -1, 2).astype(f32)
        s = jnp.einsum('bgjqd,bgqnkd->bgjqnk', qb_, kg).astype(f32) * ATT_SCALE + bias
        s = jnp.where((dist >= 0)[:, :, None], s, NEG_INF)
        p = jax.nn.softmax(s.reshape(*s.shape[:4], -1), axis=-1).reshape(s.shape)
        return jnp.einsum('bgjqnk,bgqnkd->bgjqd', p.astype(vg.dtype), vg)

    o_s = lax.map(selected_block, (q_blocks, sel_blocks, starts))
    o_s = o_s.transpose(1, 2, 3, 0, 4, 5).reshape(B, G, J, S, Dh)

    nb = S // BAND_BLOCK
    nw = WIN_SIZE // BAND_BLOCK
    kw_len = (nw + 1) * BAND_BLOCK

    def band(t_):
        tp = jnp.pad(t_, ((0, 0), (0, 0), (WIN_SIZE, 0), (0, 0))).reshape(B, G, nb + nw, BAND_BLOCK, Dh)
        return jnp.concatenate([tp[:, :, i:i + nb] for i in range(nw + 1)], axis=3)

    qw = q.reshape(B, G, J, nb, BAND_BLOCK, Dh)
    qpos = jnp.arange(nb)[:, None] * BAND_BLOCK + jnp.arange(BAND_BLOCK)[None, :]
    kpos = jnp.arange(nb)[:, None] * BAND_BLOCK - WIN_SIZE + jnp.arange(kw_len)[None, :]
    delta = qpos[:, :, None] - kpos[:, None, :]
    valid_w = (delta >= 0) & (delta < WIN_SIZE) & (kpos[:, None, :] >= 0)
    bias_w = rel_bias(table, delta).reshape(G, J, nb, BAND_BLOCK, kw_len)
    s_w = jnp.einsum('bgjnqd,bgnkd->bgjnqk', qw, band(k_win)).astype(f32) * ATT_SCALE + bias_w
    p_w = jax.nn.softmax(jnp.where(valid_w, s_w, NEG_INF), axis=-1)
    o_w = jnp.einsum('bgjnqk,bgnkd->bgjnqd', p_w.astype(v_win.dtype), band(v_win)).reshape(B, G, J, S, Dh)

    def to_tokens(o):
        return o.transpose(0, 3, 1, 2, 4).reshape(B, S, N_HEADS, Dh)

    o_all = jnp.stack([to_tokens(o_c), to_tokens(o_s), to_tokens(o_w)], axis=2)
    y = jnp.sum(gate[..., None].astype(h.dtype) * o_all * jax.nn.silu(z), axis=2)
    return y.reshape(B, S, ATT_WIDTH) @ w_out


def setup_inputs(seed: int = 0) -> dict:
    key = jax.random.key(seed)
    ks = jax.random.split(key, 16)
    f32 = jnp.float32

    def w(k, shape, fan_in):
        return jax.random.normal(k, shape, f32) * fan_in ** -0.5

    in_a = 4 * ATT_WIDTH
    in_b = (1 + NSA_BRANCHES) * ATT_WIDTH + NSA_BRANCHES * N_HEADS
    n_kv = 2 * NSA_BRANCHES * NSA_KV_GROUPS * HEAD_DIM
    return {
        'x': jax.random.normal(ks[0], (BATCH, SEQ, D_MODEL), f32),
        'norm_pre': 1.0 + 0.02 * jax.random.normal(ks[1], (DEPTH, D_MODEL), f32),
        'norm_post': 1.0 + 0.02 * jax.random.normal(ks[2], (DEPTH, D_MODEL), f32),
        'rel_table': 0.5 * jax.random.normal(ks[3], (REL_BUCKETS, N_HEADS), f32),
        'w_in_a': w(ks[4], (N_A_LAYERS, D_MODEL, in_a), D_MODEL),
        'w_out_a': w(ks[5], (N_A_LAYERS, ATT_WIDTH, D_MODEL), ATT_WIDTH),
        'kv_norm': 1.0 + 0.02 * jax.random.normal(ks[6], (D_MODEL,), f32),
        'w_kv': w(ks[7], (D_MODEL, n_kv), D_MODEL),
        'cmp_pos_k': 0.1 * jax.random.normal(ks[8], (CMP_BLOCK, HEAD_DIM), f32),
        'cmp_pos_v': 0.1 * jax.random.normal(ks[9], (CMP_BLOCK, HEAD_DIM), f32),
        'cmp_w1_k': w(ks[10], (CMP_BLOCK * HEAD_DIM, CMP_HIDDEN), CMP_BLOCK * HEAD_DIM),
        'cmp_w2_k': w(ks[11], (CMP_HIDDEN, HEAD_DIM), CMP_HIDDEN),
        'cmp_w1_v': w(ks[12], (CMP_BLOCK * HEAD_DIM, CMP_HIDDEN), CMP_BLOCK * HEAD_DIM),
        'cmp_w2_v': w(ks[13], (CMP_HIDDEN, HEAD_DIM), CMP_HIDDEN),
        'w_in_b': w(ks[14], (N_B_LAYERS, D_MODEL, in_b), D_MODEL),
        'w_out_b': w(ks[15], (N_B_LAYERS, ATT_WIDTH, D_MODEL), ATT_WIDTH),
    }


def reference(x, norm_pre, norm_post, rel_table, w_in_a, w_out_a, kv_norm, w_kv, cmp_pos_k, cmp_pos_v,
              cmp_w1_k, cmp_w2_k, cmp_w1_v, cmp_w2_v, w_in_b, w_out_b):
    h = x
    shared = None
    for layer in range(DEPTH):
        hn = rms_norm(h, norm_pre[layer])
        if layer < N_A_LAYERS:
            y = dilated_mixer(hn, w_in_a[layer], w_out_a[layer], rel_table)
        else:
            if layer == N_A_LAYERS:
                shared = shared_kv(h, kv_norm, w_kv, cmp_pos_k, cmp_pos_v, cmp_w1_k, cmp_w2_k, cmp_w1_v, cmp_w2_v)
            b = layer - N_A_LAYERS
            y = nsa_mixer(hn, w_in_b[b], w_out_b[b], rel_table, *shared)
        h = h + rms_norm(y, norm_post[layer])
    return h
```

```python
import math
import os
import numpy as np
import concourse.bass as bass
import concourse.mybir as mybir
from concourse.bass_utils import run_bass_kernel_spmd

F32 = mybir.dt.float32
BF16 = mybir.dt.bfloat16
AF = mybir.ActivationFunctionType
ALU = mybir.AluOpType
AX = mybir.AxisListType

NEG = -30000.0
D = 2048
SEQ = 2048
NH = 16
DH = 128
NT = 16
NOWN = 8
SW = 17 * 128
SCALE = DH ** -0.5
EPS = 1e-6


class T:
    __slots__ = ("w", "r", "name", "dsem", "dcount")

    def __init__(self, name=""):
        self.w = None
        self.r = {}
        self.name = name
        self.dsem = None
        self.dcount = 0


class Op:
    __slots__ = ("eng", "fn", "deps", "marked", "ev_sem", "ev_val", "is_dma")

    def __init__(self, eng, fn, deps, is_dma=False):
        self.eng = eng
        self.fn = fn
        self.deps = deps
        self.marked = False
        self.ev_sem = None
        self.ev_val = None
        self.is_dma = is_dma


class Sched:
    def __init__(self, nc, same_engine_sync=True):
        self.nc = nc
        self.ops = []
        self.h = {"pe": nc.tensor, "act": nc.scalar, "dve": nc.vector, "pool": nc.gpsimd, "sp": nc.sync}
        self.esem = {}
        self.same_engine_sync = same_engine_sync
        self._ctx = []
        for k in self.h:
            cm = nc.semaphore("sem_" + k)
            self.esem[k] = cm.__enter__()
            self._ctx.append(cm)
        self.ndsem = 0
        self.last = {}
        self.dma_since_barrier = []

    def tile_dsem(self, t):
        if t.dsem is None:
            cm = self.nc.semaphore("ds_%d" % self.ndsem)
            self.ndsem += 1
            t.dsem = cm.__enter__()
            self._ctx.append(cm)
        return t.dsem

    def _deps(self, reads, writes, join_sem=None):
        deps = []
        for t in reads:
            if t.w is not None:
                deps.append(t.w)
        for t in writes:
            if t.w is not None and not (join_sem is not None and t.w.is_dma and t.w.ev_sem is join_sem):
                deps.append(t.w)
            deps.extend(t.r.values())
        return deps

    def op(self, eng, fn, reads=(), writes=()):
        o = Op(eng, fn, self._deps(reads, writes))
        for t in reads:
            t.r[eng] = o
        for t in writes:
            t.w = o
            t.r = {}
        self.ops.append(o)
        self.last[eng] = o
        return o

    def dma(self, q, out, in_, reads=(), writes=(), semt=None, **kw):
        if semt is None:
            semt = writes[0] if writes else reads[0]
        sem = self.tile_dsem(semt)

        def fn(h, out=out, in_=in_, kw=kw):
            return h.dma_start(out=out, in_=in_, **kw)

        o = Op(q, fn, self._deps(reads, writes, join_sem=sem), is_dma=True)
        semt.dcount += 16
        o.ev_sem = sem
        o.ev_val = semt.dcount
        key = ("dma", id(sem))
        for t in reads:
            t.r[key] = o
        for t in writes:
            t.w = o
            t.r = {}
        self.ops.append(o)
        self.dma_since_barrier.append(o)
        return o

    def barrier(self):
        deps = list(self.last.values()) + list(self.dma_since_barrier)
        for e in self.h:
            o = Op(e, None, list(deps))
            self.ops.append(o)
        self.dma_since_barrier = []

    def _skip(self, d, o):
        return (not d.is_dma) and d.eng == o.eng and (d.eng == "pe" or not self.same_engine_sync)

    def emit(self, final_wait_eng="sp", final_ops=()):
        for o in self.ops:
            for d in o.deps:
                if not d.is_dma and not self._skip(d, o):
                    d.marked = True
        for d in final_ops:
            if not d.is_dma:
                d.marked = True
        cnt = {k: 0 for k in self.h}
        for o in self.ops:
            if not o.is_dma and o.marked and o.fn is not None:
                cnt[o.eng] += 1
                o.ev_sem = self.esem[o.eng]
                o.ev_val = cnt[o.eng]
        seen = {k: {} for k in self.h}
        nwait = 0
        for o in self.ops:
            h = self.h[o.eng]
            sn = seen[o.eng]
            need = {}
            for d in o.deps:
                if self._skip(d, o) or d.ev_sem is None:
                    continue
                sid = id(d.ev_sem)
                if sn.get(sid, 0) >= d.ev_val:
                    continue
                if sid not in need or need[sid][1] < d.ev_val:
                    need[sid] = (d.ev_sem, d.ev_val)
            for sid, (sem, val) in need.items():
                h.wait_ge(sem, val)
                sn[sid] = val
                nwait += 1
            if o.fn is None:
                continue
            inst = o.fn(h)
            if o.is_dma:
                inst.then_inc(o.ev_sem, 16)
            elif o.marked:
                inst.then_inc(o.ev_sem, 1)
        h = self.h[final_wait_eng]
        for d in final_ops:
            h.wait_ge(d.ev_sem, d.ev_val)
        self.stats = dict(n_ops=len(self.ops), n_wait=nwait, marked=cnt, ndsem=self.ndsem)
        return self.stats


class Arena:
    def __init__(self, nc, nbytes, flat=None):
        self.t = nc.sbuf_tensor("arena", [128, nbytes // 2], BF16).__enter__() if flat is None else flat
        self.off = 0
        self.cap = nbytes
        self.peak = 0

    def alloc(self, shape, dt):
        esz = 4 if dt == F32 else 2
        n = 1
        for s in shape[1:]:
            n *= s
        nb = (n * esz + 31) // 32 * 32
        start = self.off
        self.off += nb
        self.peak = max(self.peak, self.off)
        assert self.off <= self.cap, ("arena overflow", self.off, self.cap)
        ap = self.t[0:shape[0], start // 2: start // 2 + (n * esz) // 2]
        if dt != BF16:
            ap = ap.bitcast(dt)
        if len(shape) == 3:
            ap = ap.rearrange("p (a b) -> p a b", a=shape[1], b=shape[2])
        elif len(shape) == 4:
            ap = ap.rearrange("p (a b c) -> p a b c", a=shape[1], b=shape[2], c=shape[3])
        return ap

    def mark(self):
        return self.off

    def reset(self, m):
        self.off = m


class Ring:
    def __init__(self, aps):
        self.aps = aps
        self.ts = [T() for _ in aps]
        self.i = 0

    def next(self):
        k = self.i % len(self.aps)
        self.i += 1
        return self.aps[k], self.ts[k]


def build(upto=99, dbg=None):
    nc = bass.Bass("TRN2", target_bir_lowering=False)
    S = Sched(nc)
    A = Arena(nc, 200 * 1024)

    def din(name, shape, dt=F32):
        return nc.dram_tensor(name, list(shape), dt, kind="ExternalInput")

    x_d = din("x", [SEQ, D])
    gains_d = din("gains", [5, D])
    w_in_a = din("w_in_a", [D, 8192])
    w_out_a = din("w_out_a", [D, D])
    w_kv = din("w_kv", [D, 3072])
    w_in_b = din("w_in_b", [D, 8240])
    w_out_b = din("w_out_b", [D, D])
    cw1k = din("cw1k", [4096, 256])
    cw1v = din("cw1v", [4096, 256])
    cw2k = din("cw2k", [256, 128])
    cw2v = din("cw2v", [256, 128])
    cposk = din("cposk", [32, 128])
    cposv = din("cposv", [32, 128])
    s0_d = din("strip0", [NH, 128, SW])
    s1_d = din("strip1", [NH, 128, SW])
    logm_d = din("logm", [128, SW])
    winm_d = din("winmask", [128, 768])
    bc_d = din("biasc", [NH, 128, 1024])
    tk_d = din("topk", [2, 128, NOWN * 32])
    ovl_d = din("ovl", [128, 33])
    e_d = din("emat", [32, 2048])
    ident_d = din("ident", [128, 128])
    blend_d = din("blend", [128, 2])
    out_d = nc.dram_tensor("out", [NOWN * 128, D], F32, kind="ExternalOutput")
    og0_d = nc.dram_tensor("og0", [NT, 128, NH, 128], BF16, kind="Internal" if dbg != "og0" else "ExternalOutput")
    h1_d = nc.dram_tensor("h1s", [SEQ, D], F32, kind="Internal" if dbg != "h1" else "ExternalOutput")
    KVW = 2048 + 2048 + 16 * 130 + 16 * 130 + 128 + 176
    kv_d = nc.dram_tensor("kvs", [4, 128, KVW], BF16, kind="Internal" if dbg != "kv" else "ExternalOutput")
    h1o_d = nc.dram_tensor("h1own", [NOWN * 128, D], F32, kind="Internal")
    if dbg == "og1":
        pass
    woa_d = nc.dram_tensor("woa_bf", [16, 128, D], BF16, kind="Internal")
    wob_d = nc.dram_tensor("wob_bf", [16, 128, D], BF16, kind="Internal")
    og1_d = nc.dram_tensor("og1", [NOWN, 128, NH, 128], BF16, kind="Internal" if dbg != "og1" else "ExternalOutput")

    banks = [nc.psum_tensor("bank%d" % i, [128, 512], F32).__enter__() for i in range(8)]
    bankT = [T("bank%d" % i) for i in range(8)]

    ident = A.alloc([128, 128], BF16)
    ident_f = A.alloc([128, 128], F32)
    t_ident = T()
    S.dma("sp", ident_f, ident_d.ap()[:, :], writes=[t_ident])
    S.op("dve", lambda h: h.tensor_copy(ident, ident_f), reads=[t_ident], writes=[t_ident])
    blend = A.alloc([128, 2], F32)
    t_blend = T()
    S.dma("sp", blend, blend_d.ap()[:, :], writes=[t_blend])
    zeros = A.alloc([128, 512], BF16)
    t_zeros = T()
    S.op("dve", lambda h: h.memset(zeros, 0.0), [], [t_zeros])
    persist_mark = A.mark()

    def mm(out, lhsT, rhs, start, stop, reads, writes):
        return S.op("pe", lambda h, o=out, l=lhsT, r=rhs, s=start, e=stop: h.matmul(o, l, r, start=s, stop=e),
                    reads, writes)

    def tr(out, in_, reads, writes):
        return S.op("pe", lambda h, o=out, i=in_: h.transpose(o, i, ident), list(reads) + [t_ident], writes)

    def gain_tile(idx):
        g = A.alloc([128, D], F32)
        tg = T()
        src = bass.AP(gains_d, idx * D, [[0, 128], [1, D]])
        S.dma("sp", g, src, writes=[tg])
        return g, tg

    def norm_to_T(src, t_src, gain, t_gain, dstT, t_dst, col0, junk, t_junk, small, hb_ring, bank_ids):
        ssq, t_ssq = small.next()
        S.op("act", lambda h, j=junk, s=src, a=ssq: h.activation(j, s, AF.Square, accum_out=a[:, 0:1]),
             [t_src], [t_junk, t_ssq])
        S.op("dve", lambda h, a=ssq: h.tensor_scalar(a[:, 1:2], a[:, 0:1], 1.0 / D, EPS, ALU.mult, ALU.add),
             [t_ssq], [t_ssq])
        S.op("act", lambda h, a=ssq: h.activation(a[:, 2:3], a[:, 1:2], AF.Sqrt), [t_ssq], [t_ssq])
        S.op("dve", lambda h, a=ssq: h.reciprocal(a[:, 3:4], a[:, 2:3]), [t_ssq], [t_ssq])
        hb, t_hb = hb_ring.next()
        S.op("dve", lambda h, o=hb, s=src, a=ssq, g=gain: h.scalar_tensor_tensor(o, s, a[:, 3:4], g, ALU.mult, ALU.mult),
             [t_src, t_ssq, t_gain], [t_hb])
        return lambda: norm_stage_b(hb, t_hb, dstT, t_dst, col0, bank_ids)

    def norm_stage_b(hb, t_hb, dstT, t_dst, col0, bank_ids):
        for half in range(2):
            b = bank_ids[half]
            pv = banks[b][:, :].bitcast(BF16).rearrange("p (a c) -> p a c", a=8, c=128)
            for k in range(8):
                c = half * 8 + k
                tr(pv[:, k, :], hb[:, c * 128:(c + 1) * 128], [t_hb], BT(b))
            eng = "act" if half == 0 else "dve"
            dst = dstT[:, half * 8:half * 8 + 8, col0:col0 + 128]
            if eng == "act":
                S.op("act", lambda h, o=dst, i=pv: h.copy(o, i), BT(b), [t_dst[half]])
            else:
                S.op("dve", lambda h, o=dst, i=pv: h.tensor_copy(o, i), BT(b), [t_dst[half]])

    def convert_wo_chunk(w_d, wbf_d, c, st, t_st, sb, t_sb, eng):
        S.dma("sp", st, w_d.ap()[c * 128:(c + 1) * 128, :], writes=[t_st])
        if eng == "act":
            S.op("act", lambda h, o=sb, i=st: h.copy(o, i), [t_st], [t_sb])
        else:
            S.op("dve", lambda h, o=sb, i=st: h.tensor_copy(o, i), [t_st], [t_sb])
        return S.dma("pool", wbf_d.ap()[c, :, :], sb, reads=[t_sb])

    class WPrefetch:
        def __init__(self, reqs, stage_ring, wring, depth, cast_eng):
            self.reqs = reqs
            self.stage_ring, self.wring, self.depth, self.cast_eng = stage_ring, wring, depth, cast_eng
            self.issued = 0
            self.taken = 0
            self.ready = []

        def get(self):
            while self.issued < min(len(self.reqs), self.taken + 1 + self.depth):
                w_d, ncols, c0 = self.reqs[self.issued]
                self.ready.append(load_wslice(w_d, ncols, c0, self.stage_ring, self.wring, cast_eng=self.cast_eng))
                self.issued += 1
            self.taken += 1
            return self.ready.pop(0)

    def load_wslice(w_d, ncols, c0, stage_ring, wring, width=128, cast_eng="pool"):
        st, t_st = stage_ring.next()
        for hh in range(2):
            src = bass.AP(w_d, c0 + hh * 8 * 128 * ncols, [[ncols, 128], [128 * ncols, 8], [1, width]])
            S.dma("sp", st[:, hh * 8:(hh + 1) * 8, 0:width], src, writes=[t_st])
        wb, t_wb = wring.next()
        if cast_eng == "act":
            S.op("act", lambda h, o=wb, i=st, w=width: h.copy(o[:, :, 0:w], i[:, :, 0:w]), [t_st], [t_wb])
        else:
            S.op(cast_eng, lambda h, o=wb, i=st, w=width: h.tensor_copy(o[:, :, 0:w], i[:, :, 0:w]), [t_st], [t_wb])
        return wb, t_wb

    def proj_T(wb, t_wb, srcT, t_srcs, ntok, evac, bank_ids):
        ng = ntok // 512
        for tg in range(ng):
            DQ.tick()
            b = bank_ids[tg % len(bank_ids)]
            for c in range(16):
                mm(banks[b][:, :], wb[:, c, :], srcT(c, tg * 512, 512), c == 0, c == 15,
                   [t_wb] + t_srcs, BT(b))
            evac(tg, banks[b], BT(b))

    class Deferred:
        def __init__(self):
            self.q = []
            self.t = 0

        def push(self, fn):
            self.q.append((self.t, fn))

        def tick(self, lag=2):
            self.t += 1
            while self.q and self.q[0][0] <= self.t - lag:
                self.q.pop(0)[1]()

        def flush(self):
            while self.q:
                self.q.pop(0)[1]()

    DQ = Deferred()

    def attention(QT, t_q, n_qt, kt_lo, kt_hi, qbase, qstep, KTt, t_k, Vt, t_v, estrip, t_es,
                  pt_ring, finish, o_slots, extra=None, vw=129, s_banks=(0, 1, 6)):
        es3 = estrip.rearrange("p (n c) -> p n c", c=128)
        step_no = [0]
        for g in range((n_qt + 3) // 4):
            tiles = list(range(4 * g, min(4 * g + 4, n_qt)))
            lo = min(kt_lo(i) for i in tiles)
            hi = max(kt_hi(i) for i in tiles)
            oslot = {}
            for i in tiles:
                oslot[i] = o_slots.next()
            steps = []
            for ki in range(lo, hi + 1):
                act = [i for i in tiles if kt_lo(i) <= ki <= kt_hi(i)]
                if not act:
                    continue
                steps.append((ki, act[0], act[-1] + 1))

            def front(st):
                ki, ia, ib = st
                n = ib - ia
                b = s_banks[step_no[0] % len(s_banks)]
                step_no[0] += 1
                N = n * 128
                mm(banks[b][:, 0:N], KTt(ki), QT[:, ia * 128:ib * 128], True, False, [t_k, t_q], BT(b))
                if extra is not None:
                    el, er, et = extra(ki, ia, ib)
                    mm(banks[b][:, 0:N], el, er, False, False, et, BT(b))
                b0 = qbase(ia) - ki
                if qstep == 1:
                    mm(banks[b][:, 0:N], ident, estrip[:, b0 * 128:(b0 + n) * 128], False, True, [t_es, t_ident], BT(b))
                else:
                    esv = es3[:, b0:b0 + (n - 1) * qstep + 1:qstep, :]
                    mm(banks[b][:, 0:N].rearrange("p (n c) -> p n c", c=128), ident, esv, False, True,
                       [t_es, t_ident], BT(b))
                pt, t_pt = pt_ring.next()
                S.op("act", lambda h, o=pt[:, 0:N], i=banks[b][:, 0:N]: h.activation(o, i, AF.Exp, scale=SCALE),
                     BT(b), [t_pt])
                return (ki, ia, ib, pt, t_pt)

            def back(fr):
                ki, ia, ib, pt, t_pt = fr
                DQ.tick()
                for i in range(ia, ib):
                    oap, t_o = oslot[i]
                    mm(oap[:, 0:vw], pt[:, (i - ia) * 128:(i - ia + 1) * 128], Vt(ki), ki == kt_lo(i),
                       ki == kt_hi(i), [t_pt, t_v], [t_o])
                    if ki == kt_hi(i):
                        DQ.push(finish(i, oap, t_o))

            fq = []
            for st in steps:
                fq.append(front(st))
                if len(fq) > 2:
                    back(fq.pop(0))
            while fq:
                back(fq.pop(0))

    def make_oslots():
        r = Ring([banks[b][:, :] for b in (2, 3, 4, 5)])
        r.ts = [bankT[b] for b in (2, 3, 4, 5)]
        return r

    tslots = Ring([banks[7][:, 0:64]])
    tslots.ts = [bankT[7]]

    def BT(b):
        return [bankT[b]]

    hT = A.alloc([128, 16, SEQ], BF16)
    t_hT = [[T(), T()] for t in range(NT)]
    p0_mark = A.mark()
    g0, t_g0 = gain_tile(0)
    xs_ring = Ring([A.alloc([128, D], F32) for _ in range(4)])
    hb_ring = Ring([A.alloc([128, D], BF16) for _ in range(2)])
    junk = A.alloc([128, D], BF16)
    t_junk = T()
    small = Ring([A.alloc([128, 4], F32) for _ in range(4)])
    prev_b = None
    for t in range(NT):
        xs, t_xs = xs_ring.next()
        S.dma("sp", xs, x_d.ap()[t * 128:(t + 1) * 128, :], writes=[t_xs])
        stb = norm_to_T(xs, t_xs, g0, t_g0, hT, t_hT[t], t * 128, junk, t_junk, small, hb_ring, (6, 7))
        if prev_b is not None:
            prev_b()
        prev_b = stb
    prev_b()
    S.barrier()
    A.reset(p0_mark)
    final_ops = []
    if dbg == "hT":
        dbg_d = nc.dram_tensor("dbg_hT", [128, 16 * SEQ], BF16, kind="ExternalOutput")
        final_ops = [S.dma("sp", dbg_d.ap()[:, c * SEQ:(c + 1) * SEQ], hT[:, c, :], reads=[x_ for p_ in t_hT for x_ in p_], semt=t_hT[c][0]) for c in range(16)]

    if upto >= 1:
        QT = A.alloc([128, SEQ], BF16); t_QT = T()
        KT = A.alloc([128, SEQ], BF16); t_KT = T()
        VT = A.alloc([128, SEQ], BF16); t_VT = T()
        zT = A.alloc([128, SEQ], BF16); t_zT = T()
        Vaug = A.alloc([128, 16, 130], BF16); t_V = T()
        S.op("dve", lambda h: h.memset(Vaug, 1.0), [], [t_V])
        logm = A.alloc([128, SW], F32); t_logm = T()
        S.dma("sp", logm, logm_d.ap()[:, :], writes=[t_logm])
        S.op("dve", lambda h: h.tensor_scalar(logm, logm, 1.0 / SCALE, None, ALU.mult), [t_logm], [t_logm])
        strip_ring = Ring([A.alloc([128, SW], F32) for _ in range(1)])
        esb_ring = Ring([A.alloc([128, SW], BF16) for _ in range(2)])
        stage_ring = Ring([A.alloc([128, 16, 128], F32) for _ in range(3)])
        wring = Ring([A.alloc([128, 16, 128], BF16) for _ in range(8)])
        pt_ring = Ring([A.alloc([128, 512], BF16) for _ in range(4)])
        og_ring = Ring([A.alloc([128, SEQ], BF16) for _ in range(2)])
        on_ring = Ring([A.alloc([128, 128], BF16) for _ in range(4)])
        rd_ring = Ring([A.alloc([128, 2], F32) for _ in range(4)])
        o_slots = make_oslots()
        hT_all = [x_ for p_ in t_hT for x_ in p_]
        nheads = NH if upto >= 2 or dbg is None else 1
        wpf = WPrefetch([(w_in_a, 8192, k * 2048 + hd * 128) for hd in range(NH) for k in range(4)],
                        stage_ring, wring, 5, "dve")

        def load_head(hd):
            ws = None
            sf, t_sf = strip_ring.next()
            S.dma("sp", sf, s0_d.ap()[hd, :, :], writes=[t_sf])
            es, t_es = esb_ring.next()
            S.op("dve", lambda h, o=es, e=sf: h.scalar_tensor_tensor(o, e, 1.0 / SCALE, logm, ALU.mult, ALU.add),
                 [t_sf, t_logm], [t_es])
            return ws, es, t_es

        cst = A.alloc([128, D], F32); t_cst = T()
        csb = A.alloc([128, D], BF16); t_csb = T()
        conv_a = [T() for _ in range(16)]
        nxt = load_head(0)
        for hd in range(NH):
            cop = convert_wo_chunk(w_out_a, woa_d, hd, cst, t_cst, csb, t_csb, "act")
            conv_a[hd].w = cop
            _, es, t_es = nxt
            wq = wpf.get()
            srcT = lambda c, c0, n: hT[:, c, c0:c0 + n]

            def evac_copy(dst, t_dst):
                def f(tg, bank, t_bank):
                    S.op("dve", lambda h, o=dst[:, tg * 512:(tg + 1) * 512], i=bank[:, :]: h.tensor_copy(o, i),
                         t_bank, [t_dst])
                return f

            def evac_silu(dst, t_dst):
                def f(tg, bank, t_bank):
                    S.op("act", lambda h, o=dst[:, tg * 512:(tg + 1) * 512], i=bank[:, :]: h.activation(o, i, AF.Silu),
                         t_bank, [t_dst])
                return f

            proj_T(wq[0], wq[1], srcT, hT_all, SEQ, evac_copy(QT, t_QT), (6, 7))
            wk = wpf.get()
            proj_T(wk[0], wk[1], srcT, hT_all, SEQ, evac_copy(KT, t_KT), (6, 7))
            wv = wpf.get()
            proj_T(wv[0], wv[1], srcT, hT_all, SEQ, evac_copy(VT, t_VT), (6, 7))
            wz = wpf.get()
            proj_T(wz[0], wz[1], srcT, hT_all, SEQ, evac_silu(zT, t_zT), (6, 7))
            if hd + 1 < NH:
                nxt = load_head(hd + 1)
            for half in range(2):
                b = 6 + half
                pv = banks[b][:, :].bitcast(BF16).rearrange("p (a c) -> p a c", a=8, c=128)
                for k in range(8):
                    t = half * 8 + k
                    tr(pv[:, k, :], VT[:, t * 128:(t + 1) * 128], [t_VT], BT(b))
                S.op("dve", lambda h, o=Vaug[:, half * 8:half * 8 + 8, 0:128], i=pv: h.tensor_copy(o, i),
                     BT(b), [t_V])
            og, t_og = og_ring.next()

            def finish(i, oap, t_o, og=og, t_og=t_og):
                rd, t_rd = rd_ring.next()
                S.op("dve", lambda h, o=rd, a=oap: h.reciprocal(o[:, 0:1], a[:, 128:129]), [t_o], [t_rd])
                on, t_on = on_ring.next()
                S.op("dve", lambda h, o=on, a=oap, r=rd: h.tensor_scalar(o, a[:, 0:128], r[:, 0:1], None, ALU.mult),
                     [t_o, t_rd], [t_on])

                def later(i=i, on=on, t_on=t_on):
                    tp, t_tp = tslots.next()
                    pv = tp.bitcast(BF16)
                    tr(pv, on, [t_on], [t_tp])
                    S.op("dve", lambda h, o=og[:, i * 128:(i + 1) * 128], p=pv, z=zT[:, i * 128:(i + 1) * 128]:
                         h.tensor_tensor(o, p, z, ALU.mult), [t_tp, t_zT], [t_og])
                return later

            attention(QT, t_QT, NT, lambda i: 0, lambda i: i, lambda i: i, 1,
                      lambda ki: KT[:, ki * 128:(ki + 1) * 128], t_KT,
                      lambda ki: Vaug[:, ki, 0:129], t_V, es, t_es, pt_ring, finish, o_slots)
            dst = og0_d.ap()[:, :, hd, :].rearrange("t p c -> p t c")
            def spill(dst=dst, og=og, t_og=t_og):
                o_sp = S.dma("pool", dst, og.rearrange("p (t c) -> p t c", c=128), reads=[t_og])
                if dbg == "og0":
                    final_ops.append(o_sp)
            DQ.push(spill)
        DQ.flush()
        S.barrier()
        A.reset(p0_mark)

    def outproj(w_d, conv_tiles, gain_idx, og_d, n_tiles, resid_fn, dst_fn):
        m = A.mark()
        wo = hT
        t_wos = [T() for _ in range(16)]
        for c in range(16):
            S.dma("sp" if c % 2 == 0 else "act", wo[:, c, :], w_d.ap()[c, :, :], reads=[conv_tiles[c]], writes=[t_wos[c]])
        gp, t_gp = gain_tile(gain_idx)
        ogt_ring = Ring([A.alloc([128, 16, 128], BF16) for _ in range(2)])
        xs_ring2 = Ring([A.alloc([128, D], F32) for _ in range(2)])
        h1_ring = Ring([A.alloc([128, D], F32) for _ in range(2)])
        small2 = Ring([A.alloc([128, 8], F32) for _ in range(4)])
        junk2 = A.alloc([128, 512], BF16); t_junk2 = T()
        last = []
        for t in range(n_tiles):
            ogt, t_ogt = ogt_ring.next()
            S.dma("sp", ogt, og_d.ap()[t, :, :, :], writes=[t_ogt])
            bs = (0, 1, 2, 3) if t % 2 == 0 else (4, 5, 6, 7)
            for n in range(4):
                b = bs[n]
                for c in range(16):
                    mm(banks[b][:, :], ogt[:, c, :], wo[:, c, n * 512:(n + 1) * 512], c == 0, c == 15,
                       [t_ogt, t_wos[c]], BT(b))
            sm, t_sm = small2.next()
            for n in range(4):
                b = bs[n]
                S.op("act", lambda h, j=junk2, i=banks[b][:, :], a=sm[:, n:n + 1]: h.activation(j, i, AF.Square, accum_out=a),
                     BT(b), [t_junk2, t_sm])
            S.op("dve", lambda h, a=sm: h.tensor_reduce(a[:, 4:5], a[:, 0:4], AX.X, ALU.add), [t_sm], [t_sm])
            S.op("dve", lambda h, a=sm: h.tensor_scalar(a[:, 5:6], a[:, 4:5], 1.0 / D, EPS, ALU.mult, ALU.add), [t_sm], [t_sm])
            S.op("act", lambda h, a=sm: h.activation(a[:, 6:7], a[:, 5:6], AF.Sqrt), [t_sm], [t_sm])
            S.op("dve", lambda h, a=sm: h.reciprocal(a[:, 7:8], a[:, 6:7]), [t_sm], [t_sm])
            res, t_res = resid_fn(t, xs_ring2)
            h1, t_h1 = h1_ring.next()
            for n in range(4):
                b = bs[n]
                S.op("dve", lambda h, o=h1[:, n * 512:(n + 1) * 512], i=banks[b][:, :], a=sm, g=gp[:, n * 512:(n + 1) * 512]:
                     h.scalar_tensor_tensor(o, i, a[:, 7:8], g, ALU.mult, ALU.mult), BT(b) + [t_sm, t_gp], [t_h1])
            S.op("dve", lambda h, o=h1, r=res: h.tensor_tensor(o, o, r, ALU.add), [t_h1, t_res], [t_h1])
            last.append(S.dma("pool", dst_fn(t), h1, reads=[t_h1]))
        S.barrier()
        A.reset(m)
        return last

    if upto >= 2:
        def resid0(t, ring):
            xs, t_xs = ring.next()
            S.dma("sp", xs, x_d.ap()[t * 128:(t + 1) * 128, :], writes=[t_xs])
            return xs, t_xs
        final_ops = outproj(woa_d, conv_a, 1, og0_d, NT, resid0, lambda t: h1_d.ap()[t * 128:(t + 1) * 128, :])

    if upto >= 3:
        m3 = A.mark()
        gk, t_gk = gain_tile(2)
        xs_ring = Ring([A.alloc([128, D], F32) for _ in range(4)])
        hb_ring = Ring([A.alloc([128, D], BF16) for _ in range(2)])
        junk = A.alloc([128, D], BF16); t_junk = T()
        small = Ring([A.alloc([128, 4], F32) for _ in range(4)])
        t_hT = [[T(), T()] for t in range(NT)]
        prev_b = None
        for t in range(NT):
            xs, t_xs = xs_ring.next()
            S.dma("sp", xs, h1_d.ap()[t * 128:(t + 1) * 128, :], writes=[t_xs])
            stb = norm_to_T(xs, t_xs, gk, t_gk, hT, t_hT[t], t * 128, junk, t_junk, small, hb_ring, (6, 7))
            if prev_b is not None:
                prev_b()
            prev_b = stb
        prev_b()
        S.barrier()
        A.reset(m3)
        stage_ring = Ring([A.alloc([128, 16, 128], F32) for _ in range(2)])
        wring = Ring([A.alloc([128, 16, 128], BF16) for _ in range(6)])
        w1 = [A.alloc([128, 32, 256], BF16) for _ in range(2)]
        t_w1 = [T(), T()]
        w2 = [A.alloc([128, 2, 128], BF16) for _ in range(2)]
        t_w2 = [T(), T()]
        posT = [A.alloc([128, 32], BF16) for _ in range(2)]
        t_posT = [T(), T()]
        pbias = [A.alloc([128, 2], F32) for _ in range(2)]
        t_pb = [T(), T()]
        ovlb = A.alloc([128, 33], BF16); t_ovl = T()
        for kvi, (w1_d, w2_d, pos_d) in enumerate(((cw1k, cw2k, cposk), (cw1v, cw2v, cposv))):
            for q4 in range(4):
                st, t_st = stage_ring.next()
                stv = st.rearrange("p a b -> p (a b)").rearrange("p (i n) -> p i n", i=8, n=256)
                src = bass.AP(w1_d, q4 * 8 * 128 * 256, [[256, 128], [128 * 256, 8], [1, 256]])
                S.dma("sp", stv, src, writes=[t_st])
                S.op("dve", lambda h, o=w1[kvi][:, q4 * 8:(q4 + 1) * 8, :], i=stv: h.tensor_copy(o, i), [t_st], [t_w1[kvi]])
            st, t_st = stage_ring.next()
            stv = st.rearrange("p a b -> p (a b)")[:, 0:256].rearrange("p (i n) -> p i n", i=2, n=128)
            src = bass.AP(w2_d, 0, [[128, 128], [128 * 128, 2], [1, 128]])
            S.dma("sp", stv, src, writes=[t_st])
            S.op("dve", lambda h, o=w2[kvi], i=stv: h.tensor_copy(o, i), [t_st], [t_w2[kvi]])
            st, t_st = stage_ring.next()
            stf = st.rearrange("p a b -> p (a b)")
            S.dma("sp", stf[0:32, 0:128], pos_d.ap()[:, :], writes=[t_st])
            S.op("dve", lambda h, o=stf[0:32, 256:320].bitcast(BF16), i=stf[0:32, 0:128]: h.tensor_copy(o, i), [t_st], [t_st])
            pv = banks[6][:, 0:16].bitcast(BF16)
            S.op("pe", lambda h, o=pv, i=stf[0:32, 256:320].bitcast(BF16): h.transpose(o, i, ident[0:32, 0:32]),
                 [t_st, t_ident], BT(6))
            S.op("dve", lambda h, o=posT[kvi], i=pv: h.tensor_copy(o, i), BT(6), [t_posT[kvi]])
            for hc in range(2):
                for i in range(32):
                    mm(banks[7][:, hc:hc + 1], w1[kvi][:, i, hc * 128:(hc + 1) * 128], posT[kvi][:, i:i + 1], i == 0, i == 31,
                       [t_w1[kvi], t_posT[kvi]], BT(7))
                S.op("dve", lambda h, o=pbias[kvi][:, hc:hc + 1], i=banks[7][:, hc:hc + 1]: h.tensor_copy(o, i),
                     BT(7), [t_pb[kvi]])
        st, t_st = stage_ring.next()
        stf = st.rearrange("p a b -> p (a b)")
        S.dma("sp", stf[:, 0:33], ovl_d.ap()[:, :], writes=[t_st])
        S.op("dve", lambda h, o=ovlb, i=stf[:, 0:33]: h.tensor_copy(o, i), [t_st], [t_ovl])

        tmpT = [A.alloc([128, SEQ], BF16) for _ in range(4)]
        t_tmpT = [T() for _ in range(4)]
        kvbuf = A.alloc([128, KVW], BF16); t_kv = T()
        O_KS, O_KW, O_VS, O_VW, O_KC, O_VC = 0, 2048, 4096, 4096 + 2080, 4096 + 4160, 4096 + 4160 + 128
        xg = A.alloc([128, 128], F32); t_xg = T()
        x2 = A.alloc([128, 128], F32); t_x2 = T()
        gT = [[A.alloc([128, 128], BF16) for _ in range(2)] for _ in range(2)]
        t_gT = [[T(), T()], [T(), T()]]
        hT_all = [x_ for p_ in t_hT for x_ in p_]
        srcT = lambda c, c0, n: hT[:, c, c0:c0 + n]

        def evac_to(dst, t_dst, eng="dve"):
            def f(tg, bank, t_bank):
                if eng == "dve":
                    S.op("dve", lambda h, o=dst[:, tg * 512:(tg + 1) * 512], i=bank[:, :]: h.tensor_copy(o, i), t_bank, [t_dst])
                else:
                    S.op("act", lambda h, o=dst[:, tg * 512:(tg + 1) * 512], i=bank[:, :]: h.copy(o, i), t_bank, [t_dst])
            return f

        kv_out = []
        wpf3 = WPrefetch([(w_kv, 3072, i * 512 + g * 128) for g in range(4) for i in range(6)], stage_ring, wring, 4, "dve")
        cst = A.alloc([128, D], F32); t_cst = T()
        csb = A.alloc([128, D], BF16); t_csb = T()
        conv_b = [T() for _ in range(16)]
        for g in range(4):
            for c4 in range(4):
                cop = convert_wo_chunk(w_out_b, wob_d, 4 * g + c4, cst, t_cst, csb, t_csb, "act" if c4 % 2 == 0 else "dve")
                conv_b[4 * g + c4].w = cop
            S.op("dve", lambda h, o=kvbuf[:, O_VS:O_KC]: h.memset(o, 1.0), [], [t_kv])
            S.op("dve", lambda h, o=kvbuf[:, O_KC:KVW]: h.memset(o, 0.0), [], [t_kv])
            S.op("dve", lambda h, o=kvbuf[:, O_VC + 128:O_VC + 161], i=ovlb: h.tensor_copy(o, i), [t_ovl], [t_kv])
            dsts = [(tmpT[0], t_tmpT[0]), (tmpT[1], t_tmpT[1]), (kvbuf[:, O_KS:O_KS + 2048], t_kv),
                    (tmpT[2], t_tmpT[2]), (kvbuf[:, O_KW:O_KW + 2048], t_kv), (tmpT[3], t_tmpT[3])]
            for i in range(6):
                wsi = wpf3.get()
                proj_T(wsi[0], wsi[1], srcT, hT_all, SEQ, evac_to(dsts[i][0], dsts[i][1], "dve" if i % 2 == 0 else "act"), (4, 5))
            for which, off in ((2, O_VS), (3, O_VW)):
                aug = kvbuf[:, off:off + 2080].rearrange("p (t c) -> p t c", c=130)
                for half in range(2):
                    b = 6 + half
                    pv = banks[b][:, :].bitcast(BF16).rearrange("p (a c) -> p a c", a=8, c=128)
                    for k in range(8):
                        t = half * 8 + k
                        tr(pv[:, k, :], tmpT[which][:, t * 128:(t + 1) * 128], [t_tmpT[which]], BT(b))
                    S.op("dve", lambda h, o=aug[:, half * 8:half * 8 + 8, 0:128], i=pv: h.tensor_copy(o, i), BT(b), [t_kv])
            for kvi in range(2):
                srcv = tmpT[kvi].rearrange("p (c i) -> p c i", i=16)
                for hc in range(2):
                    b = 4 + hc
                    for i in range(32):
                        mm(banks[b][:, 0:127], w1[kvi][:, i, hc * 128:(hc + 1) * 128],
                           srcv[:, (i // 16):(i // 16) + 127, i % 16], i == 0, i == 31, [t_w1[kvi], t_tmpT[kvi]], BT(b))
                    S.op("act", lambda h, o=xg[:, 0:127], i=banks[b][:, 0:127], bb=pbias[kvi][:, hc:hc + 1]:
                         h.activation(o, i, AF.Identity, bias=bb), BT(b) + [t_pb[kvi]], [t_xg])
                    S.op("dve", lambda h: h.tensor_tensor(x2[:, 0:127], xg[:, 0:127], xg[:, 0:127], ALU.mult), [t_xg], [t_x2])
                    S.op("dve", lambda h: h.tensor_scalar(x2[:, 0:127], x2[:, 0:127], 0.044715, 1.0, ALU.mult, ALU.add), [t_x2], [t_x2])
                    S.op("dve", lambda h: h.tensor_tensor(x2[:, 0:127], x2[:, 0:127], xg[:, 0:127], ALU.mult), [t_x2, t_xg], [t_x2])
                    S.op("act", lambda h: h.activation(x2[:, 0:127], x2[:, 0:127], AF.Sigmoid, scale=1.5957691216057308), [t_x2], [t_x2])
                    S.op("dve", lambda h, o=gT[kvi][hc][:, 0:127]: h.tensor_tensor(o, x2[:, 0:127], xg[:, 0:127], ALU.mult),
                         [t_x2, t_xg], [t_gT[kvi][hc]])
                if kvi == 0:
                    for hc in range(2):
                        mm(banks[6][:, 0:127], w2[0][:, hc, :], gT[0][hc][:, 0:127], hc == 0, hc == 1,
                           [t_w2[0], t_gT[0][hc]], BT(6))
                    S.op("dve", lambda h, o=kvbuf[:, O_KC:O_KC + 127], i=banks[6][:, 0:127]: h.tensor_copy(o, i), BT(6), [t_kv])
                else:
                    for hc in range(2):
                        mm(banks[7][0:127, 0:128], gT[1][hc][:, 0:127], w2[1][:, hc, :], hc == 0, hc == 1,
                           [t_w2[1], t_gT[1][hc]], BT(7))
                    S.op("dve", lambda h, o=kvbuf[0:127, O_VC:O_VC + 128], i=banks[7][0:127, 0:128]: h.tensor_copy(o, i),
                         BT(7), [t_kv])
            kv_out.append(S.dma("pool", kv_d.ap()[g, :, :], kvbuf, reads=[t_kv]))
        final_ops = kv_out
        S.barrier()
        A.reset(m3)

    if upto >= 4:
        m4 = A.mark()
        A2 = Arena(nc, 65536, flat=hT.rearrange("p a b -> p (a b)"))
        hn1T = A2.alloc([128, 16, NOWN * 128], BF16)
        t_hn1 = [[T(), T()] for _ in range(NOWN)]
        gate = A.alloc([128, NOWN, 48], F32); t_gate = T()
        stage_ring = Ring([A.alloc([128, 16, 128], F32) for _ in range(2)])
        wring = Ring([A.alloc([128, 16, 128], BF16) for _ in range(6)])
        m4a = A.mark()
        gp1, t_gp1 = gain_tile(3)
        xs_ring = Ring([A.alloc([128, D], F32) for _ in range(6)])
        hb_ring = Ring([A.alloc([128, D], BF16) for _ in range(2)])
        junk = A.alloc([128, D], BF16); t_junk = T()
        small = Ring([A.alloc([128, 4], F32) for _ in range(4)])
        wg, t_wg = load_wslice(w_in_b, 8240, 8192, stage_ring, wring, width=48)
        prev_b = None
        for i in range(NOWN):
            xs0, t_x0 = xs_ring.next()
            xs1, t_x1 = xs_ring.next()
            S.dma("sp", xs0, h1_d.ap()[(2 * i) * 128:(2 * i + 1) * 128, :], writes=[t_x0])
            S.dma("sp", xs1, h1_d.ap()[(2 * i + 1) * 128:(2 * i + 2) * 128, :], writes=[t_x1])
            S.op("dve", lambda h, a=xs0: h.tensor_scalar(a, a, blend[:, 0:1], None, ALU.mult), [t_x0, t_blend], [t_x0])
            S.op("dve", lambda h, a=xs0, b=xs1: h.scalar_tensor_tensor(a, b, blend[:, 1:2], a, ALU.mult, ALU.add),
                 [t_x0, t_x1, t_blend], [t_x0])
            S.dma("pool", h1o_d.ap()[i * 128:(i + 1) * 128, :], xs0, reads=[t_x0])
            stb = norm_to_T(xs0, t_x0, gp1, t_gp1, hn1T, t_hn1[i], i * 128, junk, t_junk, small, hb_ring, (6, 7))

            def stage_b(i=i, stb=stb):
                stb()
                b = 4 + (i % 2)
                for c in range(16):
                    mm(banks[b][:, 0:48], hn1T[:, c, i * 128:(i + 1) * 128], wg[:, c, 0:48], c == 0, c == 15,
                       t_hn1[i] + [t_wg], BT(b))
                S.op("act", lambda h, o=gate[:, i, :], p=banks[b][:, 0:48]: h.activation(o, p, AF.Sigmoid), BT(b), [t_gate])
            if prev_b is not None:
                prev_b()
            prev_b = stage_b
        prev_b()
        S.barrier()
        A.reset(m4a)
        tkc = A.alloc([128, 2, NOWN, 32], F32); t_tkc = T()
        S.dma("sp", tkc.rearrange("p a b c -> p a (b c)"), tk_d.ap().rearrange("a p n -> p a n"), writes=[t_tkc])
        emat = A.alloc([128, 2048], BF16); t_emat = T()
        S.op("dve", lambda h: h.memset(emat, 0.0), [], [t_emat])
        for q4 in range(2):
            st, t_st = stage_ring.next()
            stf = st.rearrange("p a b -> p (a b)")
            S.dma("sp", stf[0:32, 0:1024], e_d.ap()[:, q4 * 1024:(q4 + 1) * 1024], writes=[t_st])
            S.op("dve", lambda h, o=emat[0:32, q4 * 1024:(q4 + 1) * 1024], i=stf[0:32, 0:1024]: h.tensor_copy(o, i), [t_st, t_emat], [t_emat])
        winm = A.alloc([128, 768], F32); t_winm = T()
        S.dma("sp", winm, winm_d.ap()[:, :], writes=[t_winm])
        S.op("dve", lambda h: h.tensor_scalar(winm, winm, 1.0 / SCALE, None, ALU.mult), [t_winm], [t_winm])
        kvbuf = A2.alloc([128, KVW], BF16); t_kv = T()
        O_KS, O_KW, O_VS, O_VW, O_KC, O_VC = 0, 2048, 4096, 4096 + 2080, 4096 + 4160, 4096 + 4160 + 128
        QTs = [A2.alloc([128, NOWN * 128], BF16) for _ in range(4)]
        t_QTs = [T() for _ in range(4)]
        ocn = [A.alloc([128, NOWN, 128], BF16) for _ in range(4)]
        t_ocn = [T() for _ in range(4)]
        zsets = [[A.alloc([128, NOWN * 128], BF16) for _ in range(3)] for _ in range(2)]
        t_zsets = [[T() for _ in range(3)] for _ in range(2)]
        sf_ring = Ring([A.alloc([128, SW], F32) for _ in range(1)])
        es_ring = Ring([A.alloc([128, SW], BF16) for _ in range(2)])
        wes_ring = Ring([A.alloc([128, 768], BF16) for _ in range(2)])
        bcf_ring = Ring([A.alloc([128, NOWN * 128], F32) for _ in range(1)])
        bc_ring = Ring([A.alloc([128, NOWN * 128], BF16) for _ in range(2)])
        yTs = [A2.alloc([128, NOWN * 128], F32), A.alloc([128, NOWN * 128], F32)]
        t_yTs = [T(), T()]
        tmpf = Ring([A.alloc([128, 128], F32) for _ in range(3)])
        og_ring = Ring([A.alloc([128, NOWN * 128], BF16) for _ in range(2)])
        pt_ring = Ring([A.alloc([128, 512], BF16) for _ in range(4)])
        on_ring = Ring([A.alloc([128, 128], BF16) for _ in range(4)])
        rd_ring = Ring([A.alloc([128, 4], F32) for _ in range(6)])
        imp = A.alloc([128, NOWN, 32], F32); t_imp = T()
        impf = A.alloc([128, 32], F32); t_impf = T()
        imp2 = A.alloc([128, 32], F32); t_imp2 = T()
        m8 = A.alloc([128, 16], F32); t_m8 = T()
        selb_ring = Ring([A.alloc([128, 32], BF16) for _ in range(8)])
        selmT = A.alloc([128, NOWN * 128], BF16); t_selmT = T()
        S.op("dve", lambda h: h.memset(selmT, 0.0), [], [t_selmT])
        o_slots = make_oslots()
        hn_all = [x_ for p_ in t_hn1 for x_ in p_]
        srcT1 = lambda c, c0, n: hn1T[:, c, c0:c0 + n]
        KsT = kvbuf[:, O_KS:O_KS + 2048]
        KwT = kvbuf[:, O_KW:O_KW + 2048]
        Vs = kvbuf[:, O_VS:O_VS + 2080].rearrange("p (t c) -> p t c", c=130)
        Vw = kvbuf[:, O_VW:O_VW + 2080].rearrange("p (t c) -> p t c", c=130)
        KcT = kvbuf[:, O_KC:O_KC + 128]
        Vc = kvbuf[:, O_VC:O_VC + 161]

        def evac_copy1(dst, t_dst):
            def f(tg, bank, t_bank):
                S.op("dve", lambda h, o=dst[:, tg * 512:(tg + 1) * 512], i=bank[:, :]: h.tensor_copy(o, i), t_bank, [t_dst])
            return f

        def evac_silu1(dst, t_dst):
            def f(tg, bank, t_bank):
                S.op("act", lambda h, o=dst[:, tg * 512:(tg + 1) * 512], i=bank[:, :]: h.activation(o, i, AF.Silu), t_bank, [t_dst])
            return f

        og_out = []
        reqs4 = []
        for g_ in range(4):
            for j_ in range(4):
                reqs4.append((w_in_b, 8240, (4 * g_ + j_) * 128))
            for j_ in range(4):
                for br_ in range(3):
                    reqs4.append((w_in_b, 8240, 2048 + br_ * 2048 + (4 * g_ + j_) * 128))
        wpf4 = WPrefetch(reqs4, stage_ring, wring, 4, "act")
        P4CUT = int(os.environ.get("P4CUT", "99"))
        for g in range(4 if P4CUT > 10 else 1):
            if P4CUT <= 1:
                break
            S.dma("sp", kvbuf, kv_d.ap()[g, :, :], writes=[t_kv])
            S.op("dve", lambda h: h.memset(imp, 0.0), [], [t_imp])
            for j in range(4):
                hd = 4 * g + j
                wq = wpf4.get()
                bcf, t_bcf = bcf_ring.next()
                S.dma("sp", bcf, bc_d.ap()[hd, :, :], writes=[t_bcf])
                bc, t_bc = bc_ring.next()
                S.op("act", lambda h, o=bc, e=bcf: h.activation(o, e, AF.Copy, scale=1.0 / SCALE), [t_bcf], [t_bc])
                proj_T(wq[0], wq[1], srcT1, hn_all, NOWN * 128, evac_copy1(QTs[j], t_QTs[j]), (6, 7))
                for half in range(2):
                    b = half
                    mm(banks[b][0:127, :], KcT[:, 0:127], QTs[j][:, half * 512:(half + 1) * 512], True, False,
                       [t_kv, t_QTs[j]], BT(b))
                    mm(banks[b][0:127, :], ident[0:127, 0:127], bc[0:127, half * 512:(half + 1) * 512], False, True,
                       [t_bc, t_ident], BT(b))
                    pt, t_pt = pt_ring.next()
                    S.op("act", lambda h, o=pt[0:127, :], i=banks[b][0:127, :]: h.activation(o, i, AF.Exp, scale=SCALE),
                         BT(b), [t_pt])
                    for ii in range(4):
                        i = half * 4 + ii
                        ob = 2 + ii
                        mm(banks[ob][:, 0:161], pt[0:127, ii * 128:(ii + 1) * 128], Vc[0:127, :], True, True,
                           [t_pt, t_kv], BT(ob))
                        rd, t_rd = rd_ring.next()
                        S.op("dve", lambda h, o=rd, a=banks[ob]: h.tensor_scalar(o[:, 0:1], a[:, 160:161], 1e-30, None, ALU.max),
                             BT(ob), [t_rd])
                        S.op("dve", lambda h, o=rd: h.reciprocal(o[:, 1:2], o[:, 0:1]), [t_rd], [t_rd])
                        S.op("dve", lambda h, o=imp[:, i, :], a=banks[ob], r=rd: h.scalar_tensor_tensor(o, a[:, 128:160], r[:, 1:2], o, ALU.mult, ALU.add),
                             BT(ob) + [t_rd, t_imp], [t_imp])
                        S.op("dve", lambda h, r=rd, gg=gate[:, i, hd:hd + 1]: h.tensor_tensor(r[:, 2:3], r[:, 1:2], gg, ALU.mult),
                             [t_rd, t_gate], [t_rd])
                        S.op("dve", lambda h, o=ocn[j][:, i, :], a=banks[ob], r=rd: h.tensor_scalar(o, a[:, 0:128], r[:, 2:3], None, ALU.mult),
                             BT(ob) + [t_rd], [t_ocn[j]])
            def zproj(hd):
                for br in range(3):
                    wz = wpf4.get()
                    proj_T(wz[0], wz[1], srcT1, hn_all, NOWN * 128, evac_silu1(zsets[hd % 2][br], t_zsets[hd % 2][br]), (6, 7))

            def prep_strips(hd):
                sf, t_sf = sf_ring.next()
                S.dma("sp", sf, s1_d.ap()[hd, :, :], writes=[t_sf])
                es, t_es = es_ring.next()
                wes, t_wes = wes_ring.next()
                S.op("dve", lambda h, o=wes, e=sf: h.scalar_tensor_tensor(o, e[:, 0:768], 1.0 / SCALE, winm, ALU.mult, ALU.add),
                     [t_sf, t_winm], [t_wes])
                S.op("act", lambda h, o=es, e=sf: h.activation(o, e, AF.Copy, scale=1.0 / SCALE), [t_sf], [t_es])
                return es, t_es, wes, t_wes

            if P4CUT > 3:
                nxt_strips = prep_strips(4 * g)
                zproj(4 * g)
            if P4CUT <= 2:
                break
            for i in range(NOWN):
                S.op("dve", lambda h, a=imp[:, i, :], k=tkc[:, 0, i, :]: h.tensor_tensor(impf, a, k, ALU.mult), [t_imp, t_tkc], [t_impf])
                S.op("dve", lambda h, f=tkc[:, 1, i, :]: h.tensor_tensor(impf, impf, f, ALU.add), [t_impf, t_tkc], [t_impf])
                S.op("dve", lambda h: h.max(m8[:, 0:8], impf), [t_impf], [t_m8])
                S.op("dve", lambda h: h.match_replace(imp2, m8[:, 0:8], impf, -3e9), [t_impf, t_m8], [t_imp2])
                S.op("dve", lambda h: h.max(m8[:, 8:16], imp2), [t_imp2], [t_m8])
                S.op("dve", lambda h: h.tensor_scalar(imp2, impf, m8[:, 15:16], None, ALU.is_ge), [t_impf, t_m8], [t_imp2])
                sb, t_sb = selb_ring.next()
                S.op("dve", lambda h, sb=sb: h.tensor_scalar(sb, imp2, 29952.0, -29952.0, ALU.mult, ALU.add), [t_imp2], [t_sb])
                def selT_later(i=i, sb=sb, t_sb=t_sb):
                    tp, t_tp = tslots.next()
                    pv = tp[0:32, :].bitcast(BF16)
                    tr(pv, sb, [t_sb], [t_tp])
                    S.op("dve", lambda h, o=selmT[0:32, i * 128:(i + 1) * 128], p=pv: h.tensor_copy(o, p), [t_tp, t_selmT], [t_selmT])
                DQ.push(selT_later)
            if P4CUT <= 3 and P4CUT < 30:
                break
            for j in range(4 if P4CUT > 10 else 1):
                hd = 4 * g + j
                if j > 0:
                    zproj(hd)
                zTs, t_zTs = zsets[hd % 2], t_zsets[hd % 2]
                yT, t_yT = yTs[hd % 2], t_yTs[hd % 2]
                es, t_es, wes, t_wes = nxt_strips
                if j < 3:
                    nxt_strips = prep_strips(hd + 1)
                if P4CUT == 32:
                    break
                og, t_og = og_ring.next()
                pv8 = banks[7][:, :].bitcast(BF16)
                for i in range(NOWN):
                    tr(pv8[:, i * 128:(i + 1) * 128], ocn[j][:, i, :], [t_ocn[j]], BT(7))
                S.op("dve", lambda h, o=yT, p=pv8, z=zTs[0]: h.tensor_tensor(o, p, z, ALU.mult), BT(7) + [t_zTs[0]], [t_yT])

                def mk_finish(br, gcol, last, zTs=zTs, t_zTs=t_zTs, yT=yT, t_yT=t_yT):
                    def finish(i, oap, t_o, og=og, t_og=t_og):
                        rd, t_rd = rd_ring.next()
                        S.op("dve", lambda h, o=rd, a=oap: h.reciprocal(o[:, 0:1], a[:, 128:129]), [t_o], [t_rd])
                        on, t_on = on_ring.next()
                        S.op("dve", lambda h, o=on, a=oap, r=rd, gg=gate[:, i, gcol:gcol + 1]:
                             h.tensor_scalar(o, a[:, 0:128], r[:, 0:1], gg, ALU.mult, ALU.mult), [t_o, t_rd, t_gate], [t_on])

                        def later(i=i, on=on, t_on=t_on):
                            tp, t_tp = tslots.next()
                            pv = tp.bitcast(BF16)
                            tr(pv, on, [t_on], [t_tp])
                            tm, t_tm = tmpf.next()
                            S.op("dve", lambda h, o=tm, p=pv, z=zTs[br][:, i * 128:(i + 1) * 128]: h.tensor_tensor(o, p, z, ALU.mult),
                                 [t_tp, t_zTs[br]], [t_tm])
                            if not last:
                                S.op("dve", lambda h, o=yT[:, i * 128:(i + 1) * 128], a=tm: h.tensor_tensor(o, o, a, ALU.add),
                                     [t_tm, t_yT], [t_yT])
                            else:
                                S.op("dve", lambda h, o=og[:, i * 128:(i + 1) * 128], y=yT[:, i * 128:(i + 1) * 128], a=tm:
                                     h.tensor_tensor(o, y, a, ALU.add), [t_tm, t_yT], [t_og])
                        return later
                    return finish

                if P4CUT <= 4 or P4CUT in (31, 32, 33):
                    break
                attention(QTs[j], t_QTs[j], NOWN, lambda i: max(0, 2 * i - 4), lambda i: 2 * i + 1, lambda i: 2 * i + 1, 2,
                          lambda ki: KwT[:, ki * 128:(ki + 1) * 128], t_kv, lambda ki: Vw[:, ki, 0:129], t_kv,
                          wes, t_wes, pt_ring, mk_finish(2, 32 + hd, False), o_slots)
                if P4CUT <= 5:
                    break
                if j == 0:
                    DQ.flush()

                def extra(ki, ia, ib):
                    return emat[:, ki * 128:(ki + 1) * 128], selmT[:, ia * 128:ib * 128], [t_emat, t_selmT]
                attention(QTs[j], t_QTs[j], NOWN, lambda i: 0, lambda i: 2 * i + 1, lambda i: 2 * i + 1, 2,
                          lambda ki: KsT[:, ki * 128:(ki + 1) * 128], t_kv, lambda ki: Vs[:, ki, 0:129], t_kv,
                          es, t_es, pt_ring, mk_finish(1, 16 + hd, True), o_slots, extra=extra)
                dst = og1_d.ap()[:, :, hd, :].rearrange("t p c -> p t c")

                def spill1(dst=dst, og=og, t_og=t_og):
                    og_out.append(S.dma("pool", dst, og.rearrange("p (t c) -> p t c", c=128), reads=[t_og]))
                DQ.push(spill1)
        DQ.flush()
        final_ops = og_out
        S.barrier()
        A.reset(m4)

    if upto >= 5:
        def resid1(t, ring):
            xs, t_xs = ring.next()
            S.dma("sp", xs, h1o_d.ap()[t * 128:(t + 1) * 128, :], writes=[t_xs])
            return xs, t_xs
        final_ops = outproj(wob_d, conv_b, 4, og1_d, NOWN, resid1, lambda t: out_d.ap()[t * 128:(t + 1) * 128, :])

    st = S.emit("sp", final_ops)
    return nc, st, A.peak


def rel_bucket_np(dist):
    n = np.maximum(dist, 0)
    nf = np.maximum(n, 1).astype(np.float32)
    lb = 16 + (np.log(nf / np.float32(16)) / np.float32(math.log(2048 / 16)) * np.float32(16)).astype(np.int32)
    return np.where(n < 16, n, np.minimum(lb, 31)).astype(np.int64)


def host_consts(rel_table, par):
    rel_table = np.asarray(rel_table, np.float32)
    k = np.arange(128)[:, None]
    j = np.arange(SW)[None, :]
    d0 = j - k
    idx0 = rel_bucket_np(d0)
    s0 = np.where((d0 >= 0)[None], rel_table[idx0].transpose(2, 0, 1), np.float32(NEG)).astype(np.float32)
    d1 = j - k - 128 * (1 - par)
    idx1 = rel_bucket_np(d1)
    s1 = np.where((d1 >= 0)[None], rel_table[idx1].transpose(2, 0, 1), np.float32(NEG)).astype(np.float32)
    mult = ((d0 >= 0) & (d0 <= 128)).astype(np.float32) + ((d0 >= 0) & (d0 % 4 == 0) & (d0 <= 512)) + \
        ((d0 >= 0) & (d0 % 16 == 0) & (d0 <= 2048))
    logm = np.where(mult > 0, np.log(np.maximum(mult, 1)), NEG).astype(np.float32)
    d1w = d1[:, :768]
    winm = np.where((d1w >= 0) & (d1w < 512), 0.0, NEG).astype(np.float32)
    i = np.arange(NOWN)
    tq = ((2 * i + par)[:, None] * 128 + np.arange(128)[None, :]).reshape(-1)
    c = np.arange(128)
    dc = tq[None, :] - (c[:, None] * 16 + 31)
    bc = np.where(((dc >= 0) & (c[:, None] < 127))[None], rel_table[rel_bucket_np(dc)].transpose(2, 0, 1),
                  np.float32(NEG)).astype(np.float32)
    cur = (tq // 64).reshape(NOWN, 128).T[:, :, None]
    blk = np.arange(32)[None, None, :]
    forced = (blk == 0) | (blk == cur) | (blk == cur - 1)
    invalid = blk > cur
    keep = (~(forced | invalid)).astype(np.float32)
    force = np.where(forced, 1e9 + blk * 1e6, np.where(invalid, -1e9 - blk * 1e6, 0.0)).astype(np.float32)
    topk = np.stack([keep.reshape(128, -1), force.reshape(128, -1)]).astype(np.float32)
    ci = np.arange(128)[:, None] * 16
    sj = np.arange(32)[None, :] * 64
    ovl = ((ci < sj + 64) & (ci + 32 > sj) & (np.arange(128)[:, None] < 127)).astype(np.float32)
    ovl = np.concatenate([ovl, np.ones((128, 1), np.float32)], axis=1)
    emat = (np.arange(2048)[None, :] // 64 == np.arange(32)[:, None]).astype(np.float32)
    blend = np.zeros((128, 2), np.float32)
    blend[:, par] = 1.0
    return dict(strip0=s0, strip1=s1, logm=logm, winmask=winm, biasc=bc, topk=topk, ovl=ovl, emat=emat,
                ident=np.eye(128, dtype=np.float32), blend=blend)


def make_in_maps(inputs):
    f = lambda a: np.ascontiguousarray(np.asarray(a, dtype=np.float32))
    x = f(inputs["x"])
    gains = np.stack([f(inputs["norm_pre"])[0], f(inputs["norm_post"])[0], f(inputs["kv_norm"]),
                      f(inputs["norm_pre"])[1], f(inputs["norm_post"])[1]])
    common = dict(gains=gains, w_in_a=f(inputs["w_in_a"])[0], w_out_a=f(inputs["w_out_a"])[0], w_kv=f(inputs["w_kv"]),
                  w_in_b=f(inputs["w_in_b"])[0], w_out_b=f(inputs["w_out_b"])[0],
                  cw1k=f(inputs["cmp_w1_k"]), cw1v=f(inputs["cmp_w1_v"]), cw2k=f(inputs["cmp_w2_k"]),
                  cw2v=f(inputs["cmp_w2_v"]), cposk=f(inputs["cmp_pos_k"]), cposv=f(inputs["cmp_pos_v"]))
    hc = [host_consts(inputs["rel_table"], par) for par in range(2)]
    maps = []
    for c in range(8):
        m = dict(common)
        m["x"] = x[c // 2]
        m.update(hc[c % 2])
        maps.append(m)
    return maps


_CACHE = {}


def kernel(**inputs):
    if "nc" not in _CACHE:
        _CACHE["nc"] = build()[0]
    nc = _CACHE["nc"]
    maps = make_in_maps(inputs)
    res = run_bass_kernel_spmd(nc, maps, core_ids=list(range(8)))
    out = np.zeros((4, SEQ, D), np.float32)
    for c in range(8):
        o = np.asarray(res.results[c]["out"]).reshape(NOWN, 128, D)
        b, par = c // 2, c % 2
        out[b].reshape(NT, 128, D)[par::2] = o
    return out
```

```python
import math
import os
import numpy as np
import concourse.bass as bass
import concourse.mybir as mybir
from concourse.bass_utils import run_bass_kernel_spmd

F32 = mybir.dt.float32
BF16 = mybir.dt.bfloat16
AF = mybir.ActivationFunctionType
ALU = mybir.AluOpType
AX = mybir.AxisListType

NEG = -30000.0
D = 2048
SEQ = 2048
NH = 16
DH = 128
NT = 16
NOWN = 8
SW = 17 * 128
SCALE = DH ** -0.5
EPS = 1e-6


class T:
    __slots__ = ("w", "r", "name", "dsem", "dcount")

    def __init__(self, name=""):
        self.w = None
        self.r = {}
        self.name = name
        self.dsem = None
        self.dcount = 0


class Op:
    __slots__ = ("eng", "fn", "deps", "marked", "ev_sem", "ev_val", "is_dma")

    def __init__(self, eng, fn, deps, is_dma=False):
        self.eng = eng
        self.fn = fn
        self.deps = deps
        self.marked = False
        self.ev_sem = None
        self.ev_val = None
        self.is_dma = is_dma


class Sched:
    def __init__(self, nc, same_engine_sync=True):
        self.nc = nc
        self.ops = []
        self.h = {"pe": nc.tensor, "act": nc.scalar, "dve": nc.vector, "pool": nc.gpsimd, "sp": nc.sync}
        self.esem = {}
        self.same_engine_sync = same_engine_sync
        self._ctx = []
        for k in self.h:
            cm = nc.semaphore("sem_" + k)
            self.esem[k] = cm.__enter__()
            self._ctx.append(cm)
        self.ndsem = 0
        self.last = {}
        self.dma_since_barrier = []

    def tile_dsem(self, t):
        if t.dsem is None:
            cm = self.nc.semaphore("ds_%d" % self.ndsem)
            self.ndsem += 1
            t.dsem = cm.__enter__()
            self._ctx.append(cm)
        return t.dsem

    def _deps(self, reads, writes, join_sem=None):
        deps = []
        for t in reads:
            if t.w is not None:
                deps.append(t.w)
        for t in writes:
            if t.w is not None and not (join_sem is not None and t.w.is_dma and t.w.ev_sem is join_sem):
                deps.append(t.w)
            deps.extend(t.r.values())
        return deps

    def op(self, eng, fn, reads=(), writes=()):
        o = Op(eng, fn, self._deps(reads, writes))
        for t in reads:
            t.r[eng] = o
        for t in writes:
            t.w = o
            t.r = {}
        self.ops.append(o)
        self.last[eng] = o
        return o

    def dma(self, q, out, in_, reads=(), writes=(), semt=None, **kw):
        if semt is None:
            semt = writes[0] if writes else reads[0]
        sem = self.tile_dsem(semt)

        def fn(h, out=out, in_=in_, kw=kw):
            return h.dma_start(out=out, in_=in_, **kw)

        o = Op(q, fn, self._deps(reads, writes, join_sem=sem), is_dma=True)
        semt.dcount += 16
        o.ev_sem = sem
        o.ev_val = semt.dcount
        key = ("dma", id(sem))
        for t in reads:
            t.r[key] = o
        for t in writes:
            t.w = o
            t.r = {}
        self.ops.append(o)
        self.dma_since_barrier.append(o)
        return o

    def barrier(self):
        deps = list(self.last.values()) + list(self.dma_since_barrier)
        for e in self.h:
            o = Op(e, None, list(deps))
            self.ops.append(o)
        self.dma_since_barrier = []

    def _skip(self, d, o):
        return (not d.is_dma) and d.eng == o.eng and (d.eng == "pe" or not self.same_engine_sync)

    def emit(self, final_wait_eng="sp", final_ops=()):
        for o in self.ops:
            for d in o.deps:
                if not d.is_dma and not self._skip(d, o):
                    d.marked = True
        for d in final_ops:
            if not d.is_dma:
                d.marked = True
        cnt = {k: 0 for k in self.h}
        for o in self.ops:
            if not o.is_dma and o.marked and o.fn is not None:
                cnt[o.eng] += 1
                o.ev_sem = self.esem[o.eng]
                o.ev_val = cnt[o.eng]
        seen = {k: {} for k in self.h}
        nwait = 0
        for o in self.ops:
            h = self.h[o.eng]
            sn = seen[o.eng]
            need = {}
            for d in o.deps:
                if self._skip(d, o) or d.ev_sem is None:
                    continue
                sid = id(d.ev_sem)
                if sn.get(sid, 0) >= d.ev_val:
                    continue
                if sid not in need or need[sid][1] < d.ev_val:
                    need[sid] = (d.ev_sem, d.ev_val)
            for sid, (sem, val) in need.items():
                h.wait_ge(sem, val)
                sn[sid] = val
                nwait += 1
            if o.fn is None:
                continue
            inst = o.fn(h)
            if o.is_dma:
                inst.then_inc(o.ev_sem, 16)
            elif o.marked:
                inst.then_inc(o.ev_sem, 1)
        h = self.h[final_wait_eng]
        for d in final_ops:
            h.wait_ge(d.ev_sem, d.ev_val)
        self.stats = dict(n_ops=len(self.ops), n_wait=nwait, marked=cnt, ndsem=self.ndsem)
        return self.stats


class Arena:
    def __init__(self, nc, nbytes, flat=None):
        self.t = nc.sbuf_tensor("arena", [128, nbytes // 2], BF16).__enter__() if flat is None else flat
        self.off = 0
        self.cap = nbytes
        self.peak = 0

    def alloc(self, shape, dt):
        esz = 4 if dt == F32 else 2
        n = 1
        for s in shape[1:]:
            n *= s
        nb = (n * esz + 31) // 32 * 32
        start = self.off
        self.off += nb
        self.peak = max(self.peak, self.off)
        assert self.off <= self.cap, ("arena overflow", self.off, self.cap)
        ap = self.t[0:shape[0], start // 2: start // 2 + (n * esz) // 2]
        if dt != BF16:
            ap = ap.bitcast(dt)
        if len(shape) == 3:
            ap = ap.rearrange("p (a b) -> p a b", a=shape[1], b=shape[2])
        elif len(shape) == 4:
            ap = ap.rearrange("p (a b c) -> p a b c", a=shape[1], b=shape[2], c=shape[3])
        return ap

    def mark(self):
        return self.off

    def reset(self, m):
        self.off = m


class Ring:
    def __init__(self, aps):
        self.aps = aps
        self.ts = [T() for _ in aps]
        self.i = 0

    def next(self):
        k = self.i % len(self.aps)
        self.i += 1
        return self.aps[k], self.ts[k]


def build(upto=99, dbg=None):
    nc = bass.Bass("TRN2", target_bir_lowering=False)
    S = Sched(nc)
    A = Arena(nc, 200 * 1024)

    def din(name, shape, dt=F32):
        return nc.dram_tensor(name, list(shape), dt, kind="ExternalInput")

    x_d = din("x", [SEQ, D])
    gains_d = din("gains", [5, D])
    w_in_a = din("w_in_a", [D, 8192])
    w_out_a = din("w_out_a", [D, D])
    w_kv = din("w_kv", [D, 3072])
    w_in_b = din("w_in_b", [D, 8240])
    w_out_b = din("w_out_b", [D, D])
    cw1k = din("cw1k", [4096, 256])
    cw1v = din("cw1v", [4096, 256])
    cw2k = din("cw2k", [256, 128])
    cw2v = din("cw2v", [256, 128])
    cposk = din("cposk", [32, 128])
    cposv = din("cposv", [32, 128])
    s0_d = din("strip0", [NH, 128, SW])
    s1_d = din("strip1", [NH, 128, SW])
    logm_d = din("logm", [128, SW])
    winm_d = din("winmask", [128, 768])
    bc_d = din("biasc", [NH, 128, 1024])
    tk_d = din("topk", [2, 128, NOWN * 32])
    ovl_d = din("ovl", [128, 33])
    e_d = din("emat", [32, 2048])
    ident_d = din("ident", [128, 128])
    blend_d = din("blend", [128, 2])
    out_d = nc.dram_tensor("out", [NOWN * 128, D], F32, kind="ExternalOutput")
    og0_d = nc.dram_tensor("og0", [NT, 128, NH, 128], BF16, kind="Internal" if dbg != "og0" else "ExternalOutput")
    h1_d = nc.dram_tensor("h1s", [SEQ, D], F32, kind="Internal" if dbg != "h1" else "ExternalOutput")
    KVW = 2048 + 2048 + 16 * 130 + 16 * 130 + 128 + 176
    kv_d = nc.dram_tensor("kvs", [4, 128, KVW], BF16, kind="Internal" if dbg != "kv" else "ExternalOutput")
    h1o_d = nc.dram_tensor("h1own", [NOWN * 128, D], F32, kind="Internal")
    if dbg == "og1":
        pass
    og1_d = nc.dram_tensor("og1", [NOWN, 128, NH, 128], BF16, kind="Internal" if dbg != "og1" else "ExternalOutput")

    banks = [nc.psum_tensor("bank%d" % i, [128, 512], F32).__enter__() for i in range(8)]
    bankT = [T("bank%d" % i) for i in range(8)]

    ident = A.alloc([128, 128], BF16)
    ident_f = A.alloc([128, 128], F32)
    t_ident = T()
    S.dma("sp", ident_f, ident_d.ap()[:, :], writes=[t_ident])
    S.op("dve", lambda h: h.tensor_copy(ident, ident_f), reads=[t_ident], writes=[t_ident])
    blend = A.alloc([128, 2], F32)
    t_blend = T()
    S.dma("sp", blend, blend_d.ap()[:, :], writes=[t_blend])
    zeros = A.alloc([128, 512], BF16)
    t_zeros = T()
    S.op("dve", lambda h: h.memset(zeros, 0.0), [], [t_zeros])
    persist_mark = A.mark()

    def mm(out, lhsT, rhs, start, stop, reads, writes):
        return S.op("pe", lambda h, o=out, l=lhsT, r=rhs, s=start, e=stop: h.matmul(o, l, r, start=s, stop=e),
                    reads, writes)

    def tr(out, in_, reads, writes):
        return S.op("pe", lambda h, o=out, i=in_: h.transpose(o, i, ident), list(reads) + [t_ident], writes)

    def gain_tile(idx):
        g = A.alloc([128, D], F32)
        tg = T()
        src = bass.AP(gains_d, idx * D, [[0, 128], [1, D]])
        S.dma("sp", g, src, writes=[tg])
        return g, tg

    def norm_to_T(src, t_src, gain, t_gain, dstT, t_dst, col0, junk, t_junk, small, hb_ring, bank_ids):
        ssq, t_ssq = small.next()
        S.op("act", lambda h, j=junk, s=src, a=ssq: h.activation(j, s, AF.Square, accum_out=a[:, 0:1]),
             [t_src], [t_junk, t_ssq])
        S.op("dve", lambda h, a=ssq: h.tensor_scalar(a[:, 1:2], a[:, 0:1], 1.0 / D, EPS, ALU.mult, ALU.add),
             [t_ssq], [t_ssq])
        S.op("act", lambda h, a=ssq: h.activation(a[:, 2:3], a[:, 1:2], AF.Sqrt), [t_ssq], [t_ssq])
        S.op("dve", lambda h, a=ssq: h.reciprocal(a[:, 3:4], a[:, 2:3]), [t_ssq], [t_ssq])
        hb, t_hb = hb_ring.next()
        S.op("dve", lambda h, o=hb, s=src, a=ssq, g=gain: h.scalar_tensor_tensor(o, s, a[:, 3:4], g, ALU.mult, ALU.mult),
             [t_src, t_ssq, t_gain], [t_hb])
        return lambda: norm_stage_b(hb, t_hb, dstT, t_dst, col0, bank_ids)

    def norm_stage_b(hb, t_hb, dstT, t_dst, col0, bank_ids):
        for half in range(2):
            b = bank_ids[half]
            pv = banks[b][:, :].bitcast(BF16).rearrange("p (a c) -> p a c", a=8, c=128)
            for k in range(8):
                c = half * 8 + k
                tr(pv[:, k, :], hb[:, c * 128:(c + 1) * 128], [t_hb], BT(b))
            eng = "act" if half == 0 else "dve"
            dst = dstT[:, half * 8:half * 8 + 8, col0:col0 + 128]
            if eng == "act":
                S.op("act", lambda h, o=dst, i=pv: h.copy(o, i), BT(b), [t_dst[half]])
            else:
                S.op("dve", lambda h, o=dst, i=pv: h.tensor_copy(o, i), BT(b), [t_dst[half]])

    class WPrefetch:
        def __init__(self, reqs, stage_ring, wring, depth, cast_eng):
            self.reqs = reqs
            self.stage_ring, self.wring, self.depth, self.cast_eng = stage_ring, wring, depth, cast_eng
            self.issued = 0
            self.taken = 0
            self.ready = []

        def get(self):
            while self.issued < min(len(self.reqs), self.taken + 1 + self.depth):
                w_d, ncols, c0 = self.reqs[self.issued]
                self.ready.append(load_wslice(w_d, ncols, c0, self.stage_ring, self.wring, cast_eng=self.cast_eng))
                self.issued += 1
            self.taken += 1
            return self.ready.pop(0)

    def load_wslice(w_d, ncols, c0, stage_ring, wring, width=128, cast_eng="pool"):
        st, t_st = stage_ring.next()
        for hh in range(2):
            src = bass.AP(w_d, c0 + hh * 8 * 128 * ncols, [[ncols, 128], [128 * ncols, 8], [1, width]])
            S.dma("sp", st[:, hh * 8:(hh + 1) * 8, 0:width], src, writes=[t_st])
        wb, t_wb = wring.next()
        if cast_eng == "act":
            S.op("act", lambda h, o=wb, i=st, w=width: h.copy(o[:, :, 0:w], i[:, :, 0:w]), [t_st], [t_wb])
        else:
            S.op(cast_eng, lambda h, o=wb, i=st, w=width: h.tensor_copy(o[:, :, 0:w], i[:, :, 0:w]), [t_st], [t_wb])
        return wb, t_wb

    def proj_T(wb, t_wb, srcT, t_srcs, ntok, evac, bank_ids):
        ng = ntok // 512
        for tg in range(ng):
            DQ.tick()
            b = bank_ids[tg % len(bank_ids)]
            for c in range(16):
                mm(banks[b][:, :], wb[:, c, :], srcT(c, tg * 512, 512), c == 0, c == 15,
                   [t_wb] + t_srcs, BT(b))
            evac(tg, banks[b], BT(b))

    class Deferred:
        def __init__(self):
            self.q = []
            self.t = 0

        def push(self, fn):
            self.q.append((self.t, fn))

        def tick(self, lag=2):
            self.t += 1
            while self.q and self.q[0][0] <= self.t - lag:
                self.q.pop(0)[1]()

        def flush(self):
            while self.q:
                self.q.pop(0)[1]()

    DQ = Deferred()

    def attention(QT, t_q, n_qt, kt_lo, kt_hi, qbase, qstep, KTt, t_k, Vt, t_v, estrip, t_es,
                  pt_ring, finish, o_slots, extra=None, vw=129, s_banks=(0, 1, 6)):
        es3 = estrip.rearrange("p (n c) -> p n c", c=128)
        step_no = [0]
        for g in range((n_qt + 3) // 4):
            tiles = list(range(4 * g, min(4 * g + 4, n_qt)))
            lo = min(kt_lo(i) for i in tiles)
            hi = max(kt_hi(i) for i in tiles)
            oslot = {}
            for i in tiles:
                oslot[i] = o_slots.next()
            steps = []
            for ki in range(lo, hi + 1):
                act = [i for i in tiles if kt_lo(i) <= ki <= kt_hi(i)]
                if not act:
                    continue
                steps.append((ki, act[0], act[-1] + 1))

            def front(st):
                ki, ia, ib = st
                n = ib - ia
                b = s_banks[step_no[0] % len(s_banks)]
                step_no[0] += 1
                N = n * 128
                mm(banks[b][:, 0:N], KTt(ki), QT[:, ia * 128:ib * 128], True, False, [t_k, t_q], BT(b))
                if extra is not None:
                    el, er, et = extra(ki, ia, ib)
                    mm(banks[b][:, 0:N], el, er, False, False, et, BT(b))
                b0 = qbase(ia) - ki
                if qstep == 1:
                    mm(banks[b][:, 0:N], ident, estrip[:, b0 * 128:(b0 + n) * 128], False, True, [t_es, t_ident], BT(b))
                else:
                    esv = es3[:, b0:b0 + (n - 1) * qstep + 1:qstep, :]
                    mm(banks[b][:, 0:N].rearrange("p (n c) -> p n c", c=128), ident, esv, False, True,
                       [t_es, t_ident], BT(b))
                pt, t_pt = pt_ring.next()
                S.op("act", lambda h, o=pt[:, 0:N], i=banks[b][:, 0:N]: h.activation(o, i, AF.Exp, scale=SCALE),
                     BT(b), [t_pt])
                return (ki, ia, ib, pt, t_pt)

            def back(fr):
                ki, ia, ib, pt, t_pt = fr
                DQ.tick()
                for i in range(ia, ib):
                    oap, t_o = oslot[i]
                    mm(oap[:, 0:vw], pt[:, (i - ia) * 128:(i - ia + 1) * 128], Vt(ki), ki == kt_lo(i),
                       ki == kt_hi(i), [t_pt, t_v], [t_o])
                    if ki == kt_hi(i):
                        DQ.push(finish(i, oap, t_o))

            fq = []
            for st in steps:
                fq.append(front(st))
                if len(fq) > 2:
                    back(fq.pop(0))
            while fq:
                back(fq.pop(0))

    def make_oslots():
        r = Ring([banks[b][:, :] for b in (2, 3, 4, 5)])
        r.ts = [bankT[b] for b in (2, 3, 4, 5)]
        return r

    tslots = Ring([banks[7][:, 0:64]])
    tslots.ts = [bankT[7]]

    def BT(b):
        return [bankT[b]]

    hT = A.alloc([128, 16, SEQ], BF16)
    t_hT = [[T(), T()] for t in range(NT)]
    p0_mark = A.mark()
    g0, t_g0 = gain_tile(0)
    xs_ring = Ring([A.alloc([128, D], F32) for _ in range(4)])
    hb_ring = Ring([A.alloc([128, D], BF16) for _ in range(2)])
    junk = A.alloc([128, D], BF16)
    t_junk = T()
    small = Ring([A.alloc([128, 4], F32) for _ in range(4)])
    prev_b = None
    for t in range(NT):
        xs, t_xs = xs_ring.next()
        S.dma("sp", xs, x_d.ap()[t * 128:(t + 1) * 128, :], writes=[t_xs])
        stb = norm_to_T(xs, t_xs, g0, t_g0, hT, t_hT[t], t * 128, junk, t_junk, small, hb_ring, (6, 7))
        if prev_b is not None:
            prev_b()
        prev_b = stb
    prev_b()
    S.barrier()
    A.reset(p0_mark)
    final_ops = []
    if dbg == "hT":
        dbg_d = nc.dram_tensor("dbg_hT", [128, 16 * SEQ], BF16, kind="ExternalOutput")
        final_ops = [S.dma("sp", dbg_d.ap()[:, c * SEQ:(c + 1) * SEQ], hT[:, c, :], reads=[x_ for p_ in t_hT for x_ in p_], semt=t_hT[c][0]) for c in range(16)]

    if upto >= 1:
        QT = A.alloc([128, SEQ], BF16); t_QT = T()
        KT = A.alloc([128, SEQ], BF16); t_KT = T()
        VT = A.alloc([128, SEQ], BF16); t_VT = T()
        zT = A.alloc([128, SEQ], BF16); t_zT = T()
        Vaug = A.alloc([128, 16, 130], BF16); t_V = T()
        S.op("dve", lambda h: h.memset(Vaug, 1.0), [], [t_V])
        logm = A.alloc([128, SW], F32); t_logm = T()
        S.dma("sp", logm, logm_d.ap()[:, :], writes=[t_logm])
        S.op("dve", lambda h: h.tensor_scalar(logm, logm, 1.0 / SCALE, None, ALU.mult), [t_logm], [t_logm])
        strip_ring = Ring([A.alloc([128, SW], F32) for _ in range(1)])
        esb_ring = Ring([A.alloc([128, SW], BF16) for _ in range(2)])
        stage_ring = Ring([A.alloc([128, 16, 128], F32) for _ in range(3)])
        wring = Ring([A.alloc([128, 16, 128], BF16) for _ in range(8)])
        pt_ring = Ring([A.alloc([128, 512], BF16) for _ in range(4)])
        og_ring = Ring([A.alloc([128, SEQ], BF16) for _ in range(2)])
        on_ring = Ring([A.alloc([128, 128], BF16) for _ in range(4)])
        rd_ring = Ring([A.alloc([128, 2], F32) for _ in range(4)])
        o_slots = make_oslots()
        hT_all = [x_ for p_ in t_hT for x_ in p_]
        nheads = NH if upto >= 2 or dbg is None else 1
        wpf = WPrefetch([(w_in_a, 8192, k * 2048 + hd * 128) for hd in range(NH) for k in range(4)],
                        stage_ring, wring, 5, "dve")

        def load_head(hd):
            ws = None
            sf, t_sf = strip_ring.next()
            S.dma("sp", sf, s0_d.ap()[hd, :, :], writes=[t_sf])
            es, t_es = esb_ring.next()
            S.op("dve", lambda h, o=es, e=sf: h.scalar_tensor_tensor(o, e, 1.0 / SCALE, logm, ALU.mult, ALU.add),
                 [t_sf, t_logm], [t_es])
            return ws, es, t_es

        nxt = load_head(0)
        for hd in range(NH):
            _, es, t_es = nxt
            wq = wpf.get()
            srcT = lambda c, c0, n: hT[:, c, c0:c0 + n]

            def evac_copy(dst, t_dst):
                def f(tg, bank, t_bank):
                    S.op("dve", lambda h, o=dst[:, tg * 512:(tg + 1) * 512], i=bank[:, :]: h.tensor_copy(o, i),
                         t_bank, [t_dst])
                return f

            def evac_silu(dst, t_dst):
                def f(tg, bank, t_bank):
                    S.op("act", lambda h, o=dst[:, tg * 512:(tg + 1) * 512], i=bank[:, :]: h.activation(o, i, AF.Silu),
                         t_bank, [t_dst])
                return f

            proj_T(wq[0], wq[1], srcT, hT_all, SEQ, evac_copy(QT, t_QT), (6, 7))
            wk = wpf.get()
            proj_T(wk[0], wk[1], srcT, hT_all, SEQ, evac_copy(KT, t_KT), (6, 7))
            wv = wpf.get()
            proj_T(wv[0], wv[1], srcT, hT_all, SEQ, evac_copy(VT, t_VT), (6, 7))
            wz = wpf.get()
            proj_T(wz[0], wz[1], srcT, hT_all, SEQ, evac_silu(zT, t_zT), (6, 7))
            if hd + 1 < NH:
                nxt = load_head(hd + 1)
            for half in range(2):
                b = 6 + half
                pv = banks[b][:, :].bitcast(BF16).rearrange("p (a c) -> p a c", a=8, c=128)
                for k in range(8):
                    t = half * 8 + k
                    tr(pv[:, k, :], VT[:, t * 128:(t + 1) * 128], [t_VT], BT(b))
                S.op("dve", lambda h, o=Vaug[:, half * 8:half * 8 + 8, 0:128], i=pv: h.tensor_copy(o, i),
                     BT(b), [t_V])
            og, t_og = og_ring.next()

            def finish(i, oap, t_o, og=og, t_og=t_og):
                rd, t_rd = rd_ring.next()
                S.op("dve", lambda h, o=rd, a=oap: h.reciprocal(o[:, 0:1], a[:, 128:129]), [t_o], [t_rd])
                on, t_on = on_ring.next()
                S.op("dve", lambda h, o=on, a=oap, r=rd: h.tensor_scalar(o, a[:, 0:128], r[:, 0:1], None, ALU.mult),
                     [t_o, t_rd], [t_on])

                def later(i=i, on=on, t_on=t_on):
                    tp, t_tp = tslots.next()
                    pv = tp.bitcast(BF16)
                    tr(pv, on, [t_on], [t_tp])
                    S.op("dve", lambda h, o=og[:, i * 128:(i + 1) * 128], p=pv, z=zT[:, i * 128:(i + 1) * 128]:
                         h.tensor_tensor(o, p, z, ALU.mult), [t_tp, t_zT], [t_og])
                return later

            attention(QT, t_QT, NT, lambda i: 0, lambda i: i, lambda i: i, 1,
                      lambda ki: KT[:, ki * 128:(ki + 1) * 128], t_KT,
                      lambda ki: Vaug[:, ki, 0:129], t_V, es, t_es, pt_ring, finish, o_slots)
            dst = og0_d.ap()[:, :, hd, :].rearrange("t p c -> p t c")
            def spill(dst=dst, og=og, t_og=t_og):
                o_sp = S.dma("pool", dst, og.rearrange("p (t c) -> p t c", c=128), reads=[t_og])
                if dbg == "og0":
                    final_ops.append(o_sp)
            DQ.push(spill)
        DQ.flush()
        S.barrier()
        A.reset(p0_mark)

    def outproj(w_d, gain_idx, og_d, n_tiles, resid_fn, dst_fn):
        m = A.mark()
        wo = hT
        t_wos = [T() for _ in range(16)]
        st_ring = Ring([A.alloc([128, D], F32) for _ in range(3)])
        for c in range(16):
            st, t_st = st_ring.next()
            S.dma("sp" if c % 2 == 0 else "act", st, w_d.ap()[c * 128:(c + 1) * 128, :], writes=[t_st])
            ce = ("dve", "act")[c % 2]
            if ce == "act":
                S.op("act", lambda h, o=wo[:, c, :], i=st: h.copy(o, i), [t_st], [t_wos[c]])
            else:
                S.op(ce, lambda h, o=wo[:, c, :], i=st: h.tensor_copy(o, i), [t_st], [t_wos[c]])
        gp, t_gp = gain_tile(gain_idx)
        ogt_ring = Ring([A.alloc([128, 16, 128], BF16) for _ in range(2)])
        xs_ring2 = Ring([A.alloc([128, D], F32) for _ in range(2)])
        h1_ring = Ring([A.alloc([128, D], F32) for _ in range(2)])
        small2 = Ring([A.alloc([128, 8], F32) for _ in range(4)])
        junk2 = A.alloc([128, 512], BF16); t_junk2 = T()
        last = []
        for t in range(n_tiles):
            ogt, t_ogt = ogt_ring.next()
            S.dma("sp", ogt, og_d.ap()[t, :, :, :], writes=[t_ogt])
            bs = (0, 1, 2, 3) if t % 2 == 0 else (4, 5, 6, 7)
            for n in range(4):
                b = bs[n]
                for c in range(16):
                    mm(banks[b][:, :], ogt[:, c, :], wo[:, c, n * 512:(n + 1) * 512], c == 0, c == 15,
                       [t_ogt, t_wos[c]], BT(b))
            sm, t_sm = small2.next()
            for n in range(4):
                b = bs[n]
                S.op("act", lambda h, j=junk2, i=banks[b][:, :], a=sm[:, n:n + 1]: h.activation(j, i, AF.Square, accum_out=a),
                     BT(b), [t_junk2, t_sm])
            S.op("dve", lambda h, a=sm: h.tensor_reduce(a[:, 4:5], a[:, 0:4], AX.X, ALU.add), [t_sm], [t_sm])
            S.op("dve", lambda h, a=sm: h.tensor_scalar(a[:, 5:6], a[:, 4:5], 1.0 / D, EPS, ALU.mult, ALU.add), [t_sm], [t_sm])
            S.op("act", lambda h, a=sm: h.activation(a[:, 6:7], a[:, 5:6], AF.Sqrt), [t_sm], [t_sm])
            S.op("dve", lambda h, a=sm: h.reciprocal(a[:, 7:8], a[:, 6:7]), [t_sm], [t_sm])
            res, t_res = resid_fn(t, xs_ring2)
            h1, t_h1 = h1_ring.next()
            for n in range(4):
                b = bs[n]
                S.op("dve", lambda h, o=h1[:, n * 512:(n + 1) * 512], i=banks[b][:, :], a=sm, g=gp[:, n * 512:(n + 1) * 512]:
                     h.scalar_tensor_tensor(o, i, a[:, 7:8], g, ALU.mult, ALU.mult), BT(b) + [t_sm, t_gp], [t_h1])
            S.op("dve", lambda h, o=h1, r=res: h.tensor_tensor(o, o, r, ALU.add), [t_h1, t_res], [t_h1])
            last.append(S.dma("pool", dst_fn(t), h1, reads=[t_h1]))
        S.barrier()
        A.reset(m)
        return last

    if upto >= 2:
        def resid0(t, ring):
            xs, t_xs = ring.next()
            S.dma("sp", xs, x_d.ap()[t * 128:(t + 1) * 128, :], writes=[t_xs])
            return xs, t_xs
        final_ops = outproj(w_out_a, 1, og0_d, NT, resid0, lambda t: h1_d.ap()[t * 128:(t + 1) * 128, :])

    if upto >= 3:
        m3 = A.mark()
        gk, t_gk = gain_tile(2)
        xs_ring = Ring([A.alloc([128, D], F32) for _ in range(4)])
        hb_ring = Ring([A.alloc([128, D], BF16) for _ in range(2)])
        junk = A.alloc([128, D], BF16); t_junk = T()
        small = Ring([A.alloc([128, 4], F32) for _ in range(4)])
        t_hT = [[T(), T()] for t in range(NT)]
        prev_b = None
        for t in range(NT):
            xs, t_xs = xs_ring.next()
            S.dma("sp", xs, h1_d.ap()[t * 128:(t + 1) * 128, :], writes=[t_xs])
            stb = norm_to_T(xs, t_xs, gk, t_gk, hT, t_hT[t], t * 128, junk, t_junk, small, hb_ring, (6, 7))
            if prev_b is not None:
                prev_b()
            prev_b = stb
        prev_b()
        S.barrier()
        A.reset(m3)
        stage_ring = Ring([A.alloc([128, 16, 128], F32) for _ in range(2)])
        wring = Ring([A.alloc([128, 16, 128], BF16) for _ in range(6)])
        w1 = [A.alloc([128, 32, 256], BF16) for _ in range(2)]
        t_w1 = [T(), T()]
        w2 = [A.alloc([128, 2, 128], BF16) for _ in range(2)]
        t_w2 = [T(), T()]
        posT = [A.alloc([128, 32], BF16) for _ in range(2)]
        t_posT = [T(), T()]
        pbias = [A.alloc([128, 2], F32) for _ in range(2)]
        t_pb = [T(), T()]
        ovlb = A.alloc([128, 33], BF16); t_ovl = T()
        for kvi, (w1_d, w2_d, pos_d) in enumerate(((cw1k, cw2k, cposk), (cw1v, cw2v, cposv))):
            for q4 in range(4):
                st, t_st = stage_ring.next()
                stv = st.rearrange("p a b -> p (a b)").rearrange("p (i n) -> p i n", i=8, n=256)
                src = bass.AP(w1_d, q4 * 8 * 128 * 256, [[256, 128], [128 * 256, 8], [1, 256]])
                S.dma("sp", stv, src, writes=[t_st])
                S.op("dve", lambda h, o=w1[kvi][:, q4 * 8:(q4 + 1) * 8, :], i=stv: h.tensor_copy(o, i), [t_st], [t_w1[kvi]])
            st, t_st = stage_ring.next()
            stv = st.rearrange("p a b -> p (a b)")[:, 0:256].rearrange("p (i n) -> p i n", i=2, n=128)
            src = bass.AP(w2_d, 0, [[128, 128], [128 * 128, 2], [1, 128]])
            S.dma("sp", stv, src, writes=[t_st])
            S.op("dve", lambda h, o=w2[kvi], i=stv: h.tensor_copy(o, i), [t_st], [t_w2[kvi]])
            st, t_st = stage_ring.next()
            stf = st.rearrange("p a b -> p (a b)")
            S.dma("sp", stf[0:32, 0:128], pos_d.ap()[:, :], writes=[t_st])
            S.op("dve", lambda h, o=stf[0:32, 256:320].bitcast(BF16), i=stf[0:32, 0:128]: h.tensor_copy(o, i), [t_st], [t_st])
            pv = banks[6][:, 0:16].bitcast(BF16)
            S.op("pe", lambda h, o=pv, i=stf[0:32, 256:320].bitcast(BF16): h.transpose(o, i, ident[0:32, 0:32]),
                 [t_st, t_ident], BT(6))
            S.op("dve", lambda h, o=posT[kvi], i=pv: h.tensor_copy(o, i), BT(6), [t_posT[kvi]])
            for hc in range(2):
                for i in range(32):
                    mm(banks[7][:, hc:hc + 1], w1[kvi][:, i, hc * 128:(hc + 1) * 128], posT[kvi][:, i:i + 1], i == 0, i == 31,
                       [t_w1[kvi], t_posT[kvi]], BT(7))
                S.op("dve", lambda h, o=pbias[kvi][:, hc:hc + 1], i=banks[7][:, hc:hc + 1]: h.tensor_copy(o, i),
                     BT(7), [t_pb[kvi]])
        st, t_st = stage_ring.next()
        stf = st.rearrange("p a b -> p (a b)")
        S.dma("sp", stf[:, 0:33], ovl_d.ap()[:, :], writes=[t_st])
        S.op("dve", lambda h, o=ovlb, i=stf[:, 0:33]: h.tensor_copy(o, i), [t_st], [t_ovl])

        tmpT = [A.alloc([128, SEQ], BF16) for _ in range(4)]
        t_tmpT = [T() for _ in range(4)]
        kvbuf = A.alloc([128, KVW], BF16); t_kv = T()
        O_KS, O_KW, O_VS, O_VW, O_KC, O_VC = 0, 2048, 4096, 4096 + 2080, 4096 + 4160, 4096 + 4160 + 128
        xg = A.alloc([128, 128], F32); t_xg = T()
        x2 = A.alloc([128, 128], F32); t_x2 = T()
        gT = [[A.alloc([128, 128], BF16) for _ in range(2)] for _ in range(2)]
        t_gT = [[T(), T()], [T(), T()]]
        hT_all = [x_ for p_ in t_hT for x_ in p_]
        srcT = lambda c, c0, n: hT[:, c, c0:c0 + n]

        def evac_to(dst, t_dst, eng="dve"):
            def f(tg, bank, t_bank):
                if eng == "dve":
                    S.op("dve", lambda h, o=dst[:, tg * 512:(tg + 1) * 512], i=bank[:, :]: h.tensor_copy(o, i), t_bank, [t_dst])
                else:
                    S.op("act", lambda h, o=dst[:, tg * 512:(tg + 1) * 512], i=bank[:, :]: h.copy(o, i), t_bank, [t_dst])
            return f

        kv_out = []
        wpf3 = WPrefetch([(w_kv, 3072, i * 512 + g * 128) for g in range(4) for i in range(6)], stage_ring, wring, 4, "dve")
        for g in range(4):
            S.op("dve", lambda h, o=kvbuf[:, O_VS:O_KC]: h.memset(o, 1.0), [], [t_kv])
            S.op("dve", lambda h, o=kvbuf[:, O_KC:KVW]: h.memset(o, 0.0), [], [t_kv])
            S.op("dve", lambda h, o=kvbuf[:, O_VC + 128:O_VC + 161], i=ovlb: h.tensor_copy(o, i), [t_ovl], [t_kv])
            dsts = [(tmpT[0], t_tmpT[0]), (tmpT[1], t_tmpT[1]), (kvbuf[:, O_KS:O_KS + 2048], t_kv),
                    (tmpT[2], t_tmpT[2]), (kvbuf[:, O_KW:O_KW + 2048], t_kv), (tmpT[3], t_tmpT[3])]
            for i in range(6):
                wsi = wpf3.get()
                proj_T(wsi[0], wsi[1], srcT, hT_all, SEQ, evac_to(dsts[i][0], dsts[i][1], "dve" if i % 2 == 0 else "act"), (4, 5))
            for which, off in ((2, O_VS), (3, O_VW)):
                aug = kvbuf[:, off:off + 2080].rearrange("p (t c) -> p t c", c=130)
                for half in range(2):
                    b = 6 + half
                    pv = banks[b][:, :].bitcast(BF16).rearrange("p (a c) -> p a c", a=8, c=128)
                    for k in range(8):
                        t = half * 8 + k
                        tr(pv[:, k, :], tmpT[which][:, t * 128:(t + 1) * 128], [t_tmpT[which]], BT(b))
                    S.op("dve", lambda h, o=aug[:, half * 8:half * 8 + 8, 0:128], i=pv: h.tensor_copy(o, i), BT(b), [t_kv])
            for kvi in range(2):
                srcv = tmpT[kvi].rearrange("p (c i) -> p c i", i=16)
                for hc in range(2):
                    b = 4 + hc
                    for i in range(32):
                        mm(banks[b][:, 0:127], w1[kvi][:, i, hc * 128:(hc + 1) * 128],
                           srcv[:, (i // 16):(i // 16) + 127, i % 16], i == 0, i == 31, [t_w1[kvi], t_tmpT[kvi]], BT(b))
                    S.op("act", lambda h, o=xg[:, 0:127], i=banks[b][:, 0:127], bb=pbias[kvi][:, hc:hc + 1]:
                         h.activation(o, i, AF.Identity, bias=bb), BT(b) + [t_pb[kvi]], [t_xg])
                    S.op("dve", lambda h: h.tensor_tensor(x2[:, 0:127], xg[:, 0:127], xg[:, 0:127], ALU.mult), [t_xg], [t_x2])
                    S.op("dve", lambda h: h.tensor_scalar(x2[:, 0:127], x2[:, 0:127], 0.044715, 1.0, ALU.mult, ALU.add), [t_x2], [t_x2])
                    S.op("dve", lambda h: h.tensor_tensor(x2[:, 0:127], x2[:, 0:127], xg[:, 0:127], ALU.mult), [t_x2, t_xg], [t_x2])
                    S.op("act", lambda h: h.activation(x2[:, 0:127], x2[:, 0:127], AF.Sigmoid, scale=1.5957691216057308), [t_x2], [t_x2])
                    S.op("dve", lambda h, o=gT[kvi][hc][:, 0:127]: h.tensor_tensor(o, x2[:, 0:127], xg[:, 0:127], ALU.mult),
                         [t_x2, t_xg], [t_gT[kvi][hc]])
                if kvi == 0:
                    for hc in range(2):
                        mm(banks[6][:, 0:127], w2[0][:, hc, :], gT[0][hc][:, 0:127], hc == 0, hc == 1,
                           [t_w2[0], t_gT[0][hc]], BT(6))
                    S.op("dve", lambda h, o=kvbuf[:, O_KC:O_KC + 127], i=banks[6][:, 0:127]: h.tensor_copy(o, i), BT(6), [t_kv])
                else:
                    for hc in range(2):
                        mm(banks[7][0:127, 0:128], gT[1][hc][:, 0:127], w2[1][:, hc, :], hc == 0, hc == 1,
                           [t_w2[1], t_gT[1][hc]], BT(7))
                    S.op("dve", lambda h, o=kvbuf[0:127, O_VC:O_VC + 128], i=banks[7][0:127, 0:128]: h.tensor_copy(o, i),
                         BT(7), [t_kv])
            kv_out.append(S.dma("pool", kv_d.ap()[g, :, :], kvbuf, reads=[t_kv]))
        final_ops = kv_out
        S.barrier()
        A.reset(m3)

    if upto >= 4:
        m4 = A.mark()
        A2 = Arena(nc, 65536, flat=hT.rearrange("p a b -> p (a b)"))
        hn1T = A2.alloc([128, 16, NOWN * 128], BF16)
        t_hn1 = [[T(), T()] for _ in range(NOWN)]
        gate = A.alloc([128, NOWN, 48], F32); t_gate = T()
        stage_ring = Ring([A.alloc([128, 16, 128], F32) for _ in range(2)])
        wring = Ring([A.alloc([128, 16, 128], BF16) for _ in range(6)])
        m4a = A.mark()
        gp1, t_gp1 = gain_tile(3)
        xs_ring = Ring([A.alloc([128, D], F32) for _ in range(6)])
        hb_ring = Ring([A.alloc([128, D], BF16) for _ in range(2)])
        junk = A.alloc([128, D], BF16); t_junk = T()
        small = Ring([A.alloc([128, 4], F32) for _ in range(4)])
        wg, t_wg = load_wslice(w_in_b, 8240, 8192, stage_ring, wring, width=48)
        prev_b = None
        for i in range(NOWN):
            xs0, t_x0 = xs_ring.next()
            xs1, t_x1 = xs_ring.next()
            S.dma("sp", xs0, h1_d.ap()[(2 * i) * 128:(2 * i + 1) * 128, :], writes=[t_x0])
            S.dma("sp", xs1, h1_d.ap()[(2 * i + 1) * 128:(2 * i + 2) * 128, :], writes=[t_x1])
            S.op("dve", lambda h, a=xs0: h.tensor_scalar(a, a, blend[:, 0:1], None, ALU.mult), [t_x0, t_blend], [t_x0])
            S.op("dve", lambda h, a=xs0, b=xs1: h.scalar_tensor_tensor(a, b, blend[:, 1:2], a, ALU.mult, ALU.add),
                 [t_x0, t_x1, t_blend], [t_x0])
            S.dma("pool", h1o_d.ap()[i * 128:(i + 1) * 128, :], xs0, reads=[t_x0])
            stb = norm_to_T(xs0, t_x0, gp1, t_gp1, hn1T, t_hn1[i], i * 128, junk, t_junk, small, hb_ring, (6, 7))

            def stage_b(i=i, stb=stb):
                stb()
                b = 4 + (i % 2)
                for c in range(16):
                    mm(banks[b][:, 0:48], hn1T[:, c, i * 128:(i + 1) * 128], wg[:, c, 0:48], c == 0, c == 15,
                       t_hn1[i] + [t_wg], BT(b))
                S.op("act", lambda h, o=gate[:, i, :], p=banks[b][:, 0:48]: h.activation(o, p, AF.Sigmoid), BT(b), [t_gate])
            if prev_b is not None:
                prev_b()
            prev_b = stage_b
        prev_b()
        S.barrier()
        A.reset(m4a)
        tkc = A.alloc([128, 2, NOWN, 32], F32); t_tkc = T()
        S.dma("sp", tkc.rearrange("p a b c -> p a (b c)"), tk_d.ap().rearrange("a p n -> p a n"), writes=[t_tkc])
        emat = A.alloc([128, 2048], BF16); t_emat = T()
        S.op("dve", lambda h: h.memset(emat, 0.0), [], [t_emat])
        for q4 in range(2):
            st, t_st = stage_ring.next()
            stf = st.rearrange("p a b -> p (a b)")
            S.dma("sp", stf[0:32, 0:1024], e_d.ap()[:, q4 * 1024:(q4 + 1) * 1024], writes=[t_st])
            S.op("dve", lambda h, o=emat[0:32, q4 * 1024:(q4 + 1) * 1024], i=stf[0:32, 0:1024]: h.tensor_copy(o, i), [t_st, t_emat], [t_emat])
        winm = A.alloc([128, 768], F32); t_winm = T()
        S.dma("sp", winm, winm_d.ap()[:, :], writes=[t_winm])
        S.op("dve", lambda h: h.tensor_scalar(winm, winm, 1.0 / SCALE, None, ALU.mult), [t_winm], [t_winm])
        kvbuf = A2.alloc([128, KVW], BF16); t_kv = T()
        O_KS, O_KW, O_VS, O_VW, O_KC, O_VC = 0, 2048, 4096, 4096 + 2080, 4096 + 4160, 4096 + 4160 + 128
        QTs = [A2.alloc([128, NOWN * 128], BF16) for _ in range(4)]
        t_QTs = [T() for _ in range(4)]
        ocn = [A.alloc([128, NOWN, 128], BF16) for _ in range(4)]
        t_ocn = [T() for _ in range(4)]
        zsets = [[A.alloc([128, NOWN * 128], BF16) for _ in range(3)] for _ in range(2)]
        t_zsets = [[T() for _ in range(3)] for _ in range(2)]
        sf_ring = Ring([A.alloc([128, SW], F32) for _ in range(1)])
        es_ring = Ring([A.alloc([128, SW], BF16) for _ in range(2)])
        wes_ring = Ring([A.alloc([128, 768], BF16) for _ in range(2)])
        bcf_ring = Ring([A.alloc([128, NOWN * 128], F32) for _ in range(1)])
        bc_ring = Ring([A.alloc([128, NOWN * 128], BF16) for _ in range(2)])
        yTs = [A2.alloc([128, NOWN * 128], F32), A.alloc([128, NOWN * 128], F32)]
        t_yTs = [T(), T()]
        tmpf = Ring([A.alloc([128, 128], F32) for _ in range(3)])
        og_ring = Ring([A.alloc([128, NOWN * 128], BF16) for _ in range(2)])
        pt_ring = Ring([A.alloc([128, 512], BF16) for _ in range(4)])
        on_ring = Ring([A.alloc([128, 128], BF16) for _ in range(4)])
        rd_ring = Ring([A.alloc([128, 4], F32) for _ in range(6)])
        imp = A.alloc([128, NOWN, 32], F32); t_imp = T()
        impf = A.alloc([128, 32], F32); t_impf = T()
        imp2 = A.alloc([128, 32], F32); t_imp2 = T()
        m8 = A.alloc([128, 16], F32); t_m8 = T()
        selb_ring = Ring([A.alloc([128, 32], BF16) for _ in range(8)])
        selmT = A.alloc([128, NOWN * 128], BF16); t_selmT = T()
        S.op("dve", lambda h: h.memset(selmT, 0.0), [], [t_selmT])
        o_slots = make_oslots()
        hn_all = [x_ for p_ in t_hn1 for x_ in p_]
        srcT1 = lambda c, c0, n: hn1T[:, c, c0:c0 + n]
        KsT = kvbuf[:, O_KS:O_KS + 2048]
        KwT = kvbuf[:, O_KW:O_KW + 2048]
        Vs = kvbuf[:, O_VS:O_VS + 2080].rearrange("p (t c) -> p t c", c=130)
        Vw = kvbuf[:, O_VW:O_VW + 2080].rearrange("p (t c) -> p t c", c=130)
        KcT = kvbuf[:, O_KC:O_KC + 128]
        Vc = kvbuf[:, O_VC:O_VC + 161]

        def evac_copy1(dst, t_dst):
            def f(tg, bank, t_bank):
                S.op("dve", lambda h, o=dst[:, tg * 512:(tg + 1) * 512], i=bank[:, :]: h.tensor_copy(o, i), t_bank, [t_dst])
            return f

        def evac_silu1(dst, t_dst):
            def f(tg, bank, t_bank):
                S.op("act", lambda h, o=dst[:, tg * 512:(tg + 1) * 512], i=bank[:, :]: h.activation(o, i, AF.Silu), t_bank, [t_dst])
            return f

        og_out = []
        reqs4 = [(w_in_b, 8240, j_ * 128) for j_ in range(4)]
        for g_ in range(4):
            for j_ in range(4):
                for br_ in range(3):
                    reqs4.append((w_in_b, 8240, 2048 + br_ * 2048 + (4 * g_ + j_) * 128))
                if g_ < 3:
                    reqs4.append((w_in_b, 8240, (4 * (g_ + 1) + j_) * 128))
        wpf4 = WPrefetch(reqs4, stage_ring, wring, 4, "act")
        P4CUT = 99
        imps = [imp, A.alloc([128, NOWN, 32], F32)]
        t_imps = [t_imp, T()]
        kcbufs = [A.alloc([128, KVW - O_KC], BF16) for _ in range(2)]
        t_kcs = [T(), T()]

        def load_kc(g):
            S.dma("sp", kcbufs[g % 2], kv_d.ap()[g, :, O_KC:KVW], writes=[t_kcs[g % 2]])
            S.op("dve", lambda h, o=imps[g % 2]: h.memset(o, 0.0), [], [t_imps[g % 2]])

        def pass1_head(g, j):
            hd = 4 * g + j
            kc, t_kc = kcbufs[g % 2], t_kcs[g % 2]
            KcT_g = kc[:, 0:128]
            Vc_g = kc[:, 128:128 + 161]
            imp_g, t_imp_g = imps[g % 2], t_imps[g % 2]
            wq = wpf4.get()
            bcf, t_bcf = bcf_ring.next()
            S.dma("sp", bcf, bc_d.ap()[hd, :, :], writes=[t_bcf])
            bc, t_bc = bc_ring.next()
            S.op("act", lambda h, o=bc, e=bcf: h.activation(o, e, AF.Copy, scale=1.0 / SCALE), [t_bcf], [t_bc])
            proj_T(wq[0], wq[1], srcT1, hn_all, NOWN * 128, evac_copy1(QTs[j], t_QTs[j]), (6, 7))
            for half in range(2):
                b = half
                mm(banks[b][0:127, :], KcT_g[:, 0:127], QTs[j][:, half * 512:(half + 1) * 512], True, False,
                   [t_kc, t_QTs[j]], BT(b))
                mm(banks[b][0:127, :], ident[0:127, 0:127], bc[0:127, half * 512:(half + 1) * 512], False, True,
                   [t_bc, t_ident], BT(b))
                pt, t_pt = pt_ring.next()
                S.op("act", lambda h, o=pt[0:127, :], i=banks[b][0:127, :]: h.activation(o, i, AF.Exp, scale=SCALE),
                     BT(b), [t_pt])
                for ii in range(4):
                    i = half * 4 + ii
                    ob = 2 + ii
                    mm(banks[ob][:, 0:161], pt[0:127, ii * 128:(ii + 1) * 128], Vc_g[0:127, :], True, True,
                       [t_pt, t_kc], BT(ob))
                    rd, t_rd = rd_ring.next()
                    S.op("dve", lambda h, o=rd, a=banks[ob]: h.tensor_scalar(o[:, 0:1], a[:, 160:161], 1e-30, None, ALU.max),
                         BT(ob), [t_rd])
                    S.op("dve", lambda h, o=rd: h.reciprocal(o[:, 1:2], o[:, 0:1]), [t_rd], [t_rd])
                    S.op("dve", lambda h, o=imp_g[:, i, :], a=banks[ob], r=rd: h.scalar_tensor_tensor(o, a[:, 128:160], r[:, 1:2], o, ALU.mult, ALU.add),
                         BT(ob) + [t_rd, t_imp_g], [t_imp_g])
                    S.op("dve", lambda h, r=rd, gg=gate[:, i, hd:hd + 1]: h.tensor_tensor(r[:, 2:3], r[:, 1:2], gg, ALU.mult),
                         [t_rd, t_gate], [t_rd])
                    S.op("dve", lambda h, o=ocn[j][:, i, :], a=banks[ob], r=rd: h.tensor_scalar(o, a[:, 0:128], r[:, 2:3], None, ALU.mult),
                         BT(ob) + [t_rd], [t_ocn[j]])

        load_kc(0)
        for j in range(4):
            pass1_head(0, j)
        for g in range(4):
            S.dma("sp", kvbuf, kv_d.ap()[g, :, :], writes=[t_kv])
            imp, t_imp = imps[g % 2], t_imps[g % 2]
            def zproj(hd):
                for br in range(3):
                    wz = wpf4.get()
                    proj_T(wz[0], wz[1], srcT1, hn_all, NOWN * 128, evac_silu1(zsets[hd % 2][br], t_zsets[hd % 2][br]), (6, 7))

            def prep_strips(hd):
                sf, t_sf = sf_ring.next()
                S.dma("sp", sf, s1_d.ap()[hd, :, :], writes=[t_sf])
                es, t_es = es_ring.next()
                wes, t_wes = wes_ring.next()
                S.op("dve", lambda h, o=wes, e=sf: h.scalar_tensor_tensor(o, e[:, 0:768], 1.0 / SCALE, winm, ALU.mult, ALU.add),
                     [t_sf, t_winm], [t_wes])
                S.op("act", lambda h, o=es, e=sf: h.activation(o, e, AF.Copy, scale=1.0 / SCALE), [t_sf], [t_es])
                return es, t_es, wes, t_wes

            if P4CUT > 3:
                nxt_strips = prep_strips(4 * g)
                zproj(4 * g)
            if P4CUT <= 2:
                break
            for i in range(NOWN):
                S.op("dve", lambda h, a=imp[:, i, :], k=tkc[:, 0, i, :]: h.tensor_tensor(impf, a, k, ALU.mult), [t_imp, t_tkc], [t_impf])
                S.op("dve", lambda h, f=tkc[:, 1, i, :]: h.tensor_tensor(impf, impf, f, ALU.add), [t_impf, t_tkc], [t_impf])
                S.op("dve", lambda h: h.max(m8[:, 0:8], impf), [t_impf], [t_m8])
                S.op("dve", lambda h: h.match_replace(imp2, m8[:, 0:8], impf, -3e9), [t_impf, t_m8], [t_imp2])
                S.op("dve", lambda h: h.max(m8[:, 8:16], imp2), [t_imp2], [t_m8])
                S.op("dve", lambda h: h.tensor_scalar(imp2, impf, m8[:, 15:16], None, ALU.is_ge), [t_impf, t_m8], [t_imp2])
                sb, t_sb = selb_ring.next()
                S.op("dve", lambda h, sb=sb: h.tensor_scalar(sb, imp2, 29952.0, -29952.0, ALU.mult, ALU.add), [t_imp2], [t_sb])
                def selT_later(i=i, sb=sb, t_sb=t_sb):
                    tp, t_tp = tslots.next()
                    pv = tp[0:32, :].bitcast(BF16)
                    tr(pv, sb, [t_sb], [t_tp])
                    S.op("dve", lambda h, o=selmT[0:32, i * 128:(i + 1) * 128], p=pv: h.tensor_copy(o, p), [t_tp, t_selmT], [t_selmT])
                DQ.push(selT_later)
            if P4CUT <= 3 and P4CUT < 30:
                break
            for j in range(4 if P4CUT > 10 else 1):
                hd = 4 * g + j
                if j > 0:
                    zproj(hd)
                zTs, t_zTs = zsets[hd % 2], t_zsets[hd % 2]
                yT, t_yT = yTs[hd % 2], t_yTs[hd % 2]
                es, t_es, wes, t_wes = nxt_strips
                if j < 3:
                    nxt_strips = prep_strips(hd + 1)
                if P4CUT == 32:
                    break
                og, t_og = og_ring.next()
                pv8 = banks[7][:, :].bitcast(BF16)
                for i in range(NOWN):
                    tr(pv8[:, i * 128:(i + 1) * 128], ocn[j][:, i, :], [t_ocn[j]], BT(7))
                S.op("dve", lambda h, o=yT, p=pv8, z=zTs[0]: h.tensor_tensor(o, p, z, ALU.mult), BT(7) + [t_zTs[0]], [t_yT])

                def mk_finish(br, gcol, last, zTs=zTs, t_zTs=t_zTs, yT=yT, t_yT=t_yT):
                    def finish(i, oap, t_o, og=og, t_og=t_og):
                        rd, t_rd = rd_ring.next()
                        S.op("dve", lambda h, o=rd, a=oap: h.reciprocal(o[:, 0:1], a[:, 128:129]), [t_o], [t_rd])
                        on, t_on = on_ring.next()
                        S.op("dve", lambda h, o=on, a=oap, r=rd, gg=gate[:, i, gcol:gcol + 1]:
                             h.tensor_scalar(o, a[:, 0:128], r[:, 0:1], gg, ALU.mult, ALU.mult), [t_o, t_rd, t_gate], [t_on])

                        def later(i=i, on=on, t_on=t_on):
                            tp, t_tp = tslots.next()
                            pv = tp.bitcast(BF16)
                            tr(pv, on, [t_on], [t_tp])
                            tm, t_tm = tmpf.next()
                            S.op("dve", lambda h, o=tm, p=pv, z=zTs[br][:, i * 128:(i + 1) * 128]: h.tensor_tensor(o, p, z, ALU.mult),
                                 [t_tp, t_zTs[br]], [t_tm])
                            if not last:
                                S.op("dve", lambda h, o=yT[:, i * 128:(i + 1) * 128], a=tm: h.tensor_tensor(o, o, a, ALU.add),
                                     [t_tm, t_yT], [t_yT])
                            else:
                                S.op("dve", lambda h, o=og[:, i * 128:(i + 1) * 128], y=yT[:, i * 128:(i + 1) * 128], a=tm:
                                     h.tensor_tensor(o, y, a, ALU.add), [t_tm, t_yT], [t_og])
                        return later
                    return finish

                if P4CUT <= 4 or P4CUT in (31, 32, 33):
                    break
                attention(QTs[j], t_QTs[j], NOWN, lambda i: max(0, 2 * i - 4), lambda i: 2 * i + 1, lambda i: 2 * i + 1, 2,
                          lambda ki: KwT[:, ki * 128:(ki + 1) * 128], t_kv, lambda ki: Vw[:, ki, 0:129], t_kv,
                          wes, t_wes, pt_ring, mk_finish(2, 32 + hd, False), o_slots)
                if P4CUT <= 5:
                    break
                if j == 0:
                    DQ.flush()

                def extra(ki, ia, ib):
                    return emat[:, ki * 128:(ki + 1) * 128], selmT[:, ia * 128:ib * 128], [t_emat, t_selmT]
                attention(QTs[j], t_QTs[j], NOWN, lambda i: 0, lambda i: 2 * i + 1, lambda i: 2 * i + 1, 2,
                          lambda ki: KsT[:, ki * 128:(ki + 1) * 128], t_kv, lambda ki: Vs[:, ki, 0:129], t_kv,
                          es, t_es, pt_ring, mk_finish(1, 16 + hd, True), o_slots, extra=extra)
                dst = og1_d.ap()[:, :, hd, :].rearrange("t p c -> p t c")

                def spill1(dst=dst, og=og, t_og=t_og):
                    og_out.append(S.dma("pool", dst, og.rearrange("p (t c) -> p t c", c=128), reads=[t_og]))
                DQ.push(spill1)
                if g + 1 < 4:
                    if j == 0:
                        load_kc(g + 1)
                    pass1_head(g + 1, j)
        DQ.flush()
        final_ops = og_out
        S.barrier()
        A.reset(m4)

    if upto >= 5:
        def resid1(t, ring):
            xs, t_xs = ring.next()
            S.dma("sp", xs, h1o_d.ap()[t * 128:(t + 1) * 128, :], writes=[t_xs])
            return xs, t_xs
        final_ops = outproj(w_out_b, 4, og1_d, NOWN, resid1, lambda t: out_d.ap()[t * 128:(t + 1) * 128, :])

    st = S.emit("sp", final_ops)
    return nc, st, A.peak


def rel_bucket_np(dist):
    n = np.maximum(dist, 0)
    nf = np.maximum(n, 1).astype(np.float32)
    lb = 16 + (np.log(nf / np.float32(16)) / np.float32(math.log(2048 / 16)) * np.float32(16)).astype(np.int32)
    return np.where(n < 16, n, np.minimum(lb, 31)).astype(np.int64)


def host_consts(rel_table, par):
    rel_table = np.asarray(rel_table, np.float32)
    k = np.arange(128)[:, None]
    j = np.arange(SW)[None, :]
    d0 = j - k
    idx0 = rel_bucket_np(d0)
    s0 = np.where((d0 >= 0)[None], rel_table[idx0].transpose(2, 0, 1), np.float32(NEG)).astype(np.float32)
    d1 = j - k - 128 * (1 - par)
    idx1 = rel_bucket_np(d1)
    s1 = np.where((d1 >= 0)[None], rel_table[idx1].transpose(2, 0, 1), np.float32(NEG)).astype(np.float32)
    mult = ((d0 >= 0) & (d0 <= 128)).astype(np.float32) + ((d0 >= 0) & (d0 % 4 == 0) & (d0 <= 512)) + \
        ((d0 >= 0) & (d0 % 16 == 0) & (d0 <= 2048))
    logm = np.where(mult > 0, np.log(np.maximum(mult, 1)), NEG).astype(np.float32)
    d1w = d1[:, :768]
    winm = np.where((d1w >= 0) & (d1w < 512), 0.0, NEG).astype(np.float32)
    i = np.arange(NOWN)
    tq = ((2 * i + par)[:, None] * 128 + np.arange(128)[None, :]).reshape(-1)
    c = np.arange(128)
    dc = tq[None, :] - (c[:, None] * 16 + 31)
    bc = np.where(((dc >= 0) & (c[:, None] < 127))[None], rel_table[rel_bucket_np(dc)].transpose(2, 0, 1),
                  np.float32(NEG)).astype(np.float32)
    cur = (tq // 64).reshape(NOWN, 128).T[:, :, None]
    blk = np.arange(32)[None, None, :]
    forced = (blk == 0) | (blk == cur) | (blk == cur - 1)
    invalid = blk > cur
    keep = (~(forced | invalid)).astype(np.float32)
    force = np.where(forced, 1e9 + blk * 1e6, np.where(invalid, -1e9 - blk * 1e6, 0.0)).astype(np.float32)
    topk = np.stack([keep.reshape(128, -1), force.reshape(128, -1)]).astype(np.float32)
    ci = np.arange(128)[:, None] * 16
    sj = np.arange(32)[None, :] * 64
    ovl = ((ci < sj + 64) & (ci + 32 > sj) & (np.arange(128)[:, None] < 127)).astype(np.float32)
    ovl = np.concatenate([ovl, np.ones((128, 1), np.float32)], axis=1)
    emat = (np.arange(2048)[None, :] // 64 == np.arange(32)[:, None]).astype(np.float32)
    blend = np.zeros((128, 2), np.float32)
    blend[:, par] = 1.0
    return dict(strip0=s0, strip1=s1, logm=logm, winmask=winm, biasc=bc, topk=topk, ovl=ovl, emat=emat,
                ident=np.eye(128, dtype=np.float32), blend=blend)


def make_in_maps(inputs):
    f = lambda a: np.ascontiguousarray(np.asarray(a, dtype=np.float32))
    x = f(inputs["x"])
    gains = np.stack([f(inputs["norm_pre"])[0], f(inputs["norm_post"])[0], f(inputs["kv_norm"]),
                      f(inputs["norm_pre"])[1], f(inputs["norm_post"])[1]])
    common = dict(gains=gains, w_in_a=f(inputs["w_in_a"])[0], w_out_a=f(inputs["w_out_a"])[0], w_kv=f(inputs["w_kv"]),
                  w_in_b=f(inputs["w_in_b"])[0], w_out_b=f(inputs["w_out_b"])[0],
                  cw1k=f(inputs["cmp_w1_k"]), cw1v=f(inputs["cmp_w1_v"]), cw2k=f(inputs["cmp_w2_k"]),
                  cw2v=f(inputs["cmp_w2_v"]), cposk=f(inputs["cmp_pos_k"]), cposv=f(inputs["cmp_pos_v"]))
    hc = [host_consts(inputs["rel_table"], par) for par in range(2)]
    maps = []
    for c in range(8):
        m = dict(common)
        m["x"] = x[c // 2]
        m.update(hc[c % 2])
        maps.append(m)
    return maps


_CACHE = {}


def kernel(**inputs):
    if "nc" not in _CACHE:
        _CACHE["nc"] = build()[0]
    nc = _CACHE["nc"]
    maps = make_in_maps(inputs)
    res = run_bass_kernel_spmd(nc, maps, core_ids=list(range(8)))
    out = np.zeros((4, SEQ, D), np.float32)
    for c in range(8):
        o = np.asarray(res.results[c]["out"]).reshape(NOWN, 128, D)
        b, par = c // 2, c % 2
        out[b].reshape(NT, 128, D)[par::2] = o
    return out
```

```python
import math
import os
import numpy as np
import concourse.bass as bass
import concourse.mybir as mybir
from concourse.bass_utils import run_bass_kernel_spmd

F32 = mybir.dt.float32
BF16 = mybir.dt.bfloat16
AF = mybir.ActivationFunctionType
ALU = mybir.AluOpType
AX = mybir.AxisListType

NEG = -30000.0
D = 2048
SEQ = 2048
NH = 16
DH = 128
NT = 16
NOWN = 8
SW = 17 * 128
SCALE = DH ** -0.5
EPS = 1e-6


class T:
    __slots__ = ("w", "r", "name", "dsem", "dcount")

    def __init__(self, name=""):
        self.w = None
        self.r = {}
        self.name = name
        self.dsem = None
        self.dcount = 0


class Op:
    __slots__ = ("eng", "fn", "deps", "marked", "ev_sem", "ev_val", "is_dma")

    def __init__(self, eng, fn, deps, is_dma=False):
        self.eng = eng
        self.fn = fn
        self.deps = deps
        self.marked = False
        self.ev_sem = None
        self.ev_val = None
        self.is_dma = is_dma


class Sched:
    def __init__(self, nc, same_engine_sync=True):
        self.nc = nc
        self.ops = []
        self.h = {"pe": nc.tensor, "act": nc.scalar, "dve": nc.vector, "pool": nc.gpsimd, "sp": nc.sync}
        self.esem = {}
        self.same_engine_sync = same_engine_sync
        self._ctx = []
        for k in self.h:
            cm = nc.semaphore("sem_" + k)
            self.esem[k] = cm.__enter__()
            self._ctx.append(cm)
        self.ndsem = 0
        self.last = {}
        self.dma_since_barrier = []

    def tile_dsem(self, t):
        if t.dsem is None:
            cm = self.nc.semaphore("ds_%d" % self.ndsem)
            self.ndsem += 1
            t.dsem = cm.__enter__()
            self._ctx.append(cm)
        return t.dsem

    def _deps(self, reads, writes, join_sem=None):
        deps = []
        for t in reads:
            if t.w is not None:
                deps.append(t.w)
        for t in writes:
            if t.w is not None and not (join_sem is not None and t.w.is_dma and t.w.ev_sem is join_sem):
                deps.append(t.w)
            deps.extend(t.r.values())
        return deps

    def op(self, eng, fn, reads=(), writes=()):
        o = Op(eng, fn, self._deps(reads, writes))
        for t in reads:
            t.r[eng] = o
        for t in writes:
            t.w = o
            t.r = {}
        self.ops.append(o)
        self.last[eng] = o
        return o

    def dma(self, q, out, in_, reads=(), writes=(), semt=None, **kw):
        if semt is None:
            semt = writes[0] if writes else reads[0]
        sem = self.tile_dsem(semt)

        def fn(h, out=out, in_=in_, kw=kw):
            return h.dma_start(out=out, in_=in_, **kw)

        o = Op(q, fn, self._deps(reads, writes, join_sem=sem), is_dma=True)
        semt.dcount += 16
        o.ev_sem = sem
        o.ev_val = semt.dcount
        key = ("dma", id(sem))
        for t in reads:
            t.r[key] = o
        for t in writes:
            t.w = o
            t.r = {}
        self.ops.append(o)
        self.dma_since_barrier.append(o)
        return o

    def barrier(self):
        deps = list(self.last.values()) + list(self.dma_since_barrier)
        for e in self.h:
            o = Op(e, None, list(deps))
            self.ops.append(o)
        self.dma_since_barrier = []

    def _skip(self, d, o):
        return (not d.is_dma) and d.eng == o.eng and (d.eng == "pe" or not self.same_engine_sync)

    def emit(self, final_wait_eng="sp", final_ops=()):
        for o in self.ops:
            for d in o.deps:
                if not d.is_dma and not self._skip(d, o):
                    d.marked = True
        for d in final_ops:
            if not d.is_dma:
                d.marked = True
        cnt = {k: 0 for k in self.h}
        for o in self.ops:
            if not o.is_dma and o.marked and o.fn is not None:
                cnt[o.eng] += 1
                o.ev_sem = self.esem[o.eng]
                o.ev_val = cnt[o.eng]
        seen = {k: {} for k in self.h}
        nwait = 0
        for o in self.ops:
            h = self.h[o.eng]
            sn = seen[o.eng]
            need = {}
            for d in o.deps:
                if self._skip(d, o) or d.ev_sem is None:
                    continue
                sid = id(d.ev_sem)
                if sn.get(sid, 0) >= d.ev_val:
                    continue
                if sid not in need or need[sid][1] < d.ev_val:
                    need[sid] = (d.ev_sem, d.ev_val)
            for sid, (sem, val) in need.items():
                h.wait_ge(sem, val)
                sn[sid] = val
                nwait += 1
            if o.fn is None:
                continue
            inst = o.fn(h)
            if o.is_dma:
                inst.then_inc(o.ev_sem, 16)
            elif o.marked:
                inst.then_inc(o.ev_sem, 1)
        h = self.h[final_wait_eng]
        for d in final_ops:
            h.wait_ge(d.ev_sem, d.ev_val)
        self.stats = dict(n_ops=len(self.ops), n_wait=nwait, marked=cnt, ndsem=self.ndsem)
        return self.stats


class Arena:
    def __init__(self, nc, nbytes, flat=None):
        self.t = nc.sbuf_tensor("arena", [128, nbytes // 2], BF16).__enter__() if flat is None else flat
        self.off = 0
        self.cap = nbytes
        self.peak = 0

    def alloc(self, shape, dt):
        esz = 4 if dt == F32 else 2
        n = 1
        for s in shape[1:]:
            n *= s
        nb = (n * esz + 31) // 32 * 32
        start = self.off
        self.off += nb
        self.peak = max(self.peak, self.off)
        assert self.off <= self.cap, ("arena overflow", self.off, self.cap)
        ap = self.t[0:shape[0], start // 2: start // 2 + (n * esz) // 2]
        if dt != BF16:
            ap = ap.bitcast(dt)
        if len(shape) == 3:
            ap = ap.rearrange("p (a b) -> p a b", a=shape[1], b=shape[2])
        elif len(shape) == 4:
            ap = ap.rearrange("p (a b c) -> p a b c", a=shape[1], b=shape[2], c=shape[3])
        return ap

    def mark(self):
        return self.off

    def reset(self, m):
        self.off = m


class Ring:
    def __init__(self, aps):
        self.aps = aps
        self.ts = [T() for _ in aps]
        self.i = 0

    def next(self):
        k = self.i % len(self.aps)
        self.i += 1
        return self.aps[k], self.ts[k]


def build(upto=99, dbg=None):
    nc = bass.Bass("TRN2", target_bir_lowering=False)
    S = Sched(nc)
    A = Arena(nc, 200 * 1024)

    def din(name, shape, dt=F32):
        return nc.dram_tensor(name, list(shape), dt, kind="ExternalInput")

    x_d = din("x", [SEQ, D])
    gains_d = din("gains", [5, D])
    w_in_a = din("w_in_a", [D, 8192])
    w_out_a = din("w_out_a", [D, D])
    w_kv = din("w_kv", [D, 3072])
    w_in_b = din("w_in_b", [D, 8240])
    w_out_b = din("w_out_b", [D, D])
    cw1k = din("cw1k", [4096, 256])
    cw1v = din("cw1v", [4096, 256])
    cw2k = din("cw2k", [256, 128])
    cw2v = din("cw2v", [256, 128])
    cposk = din("cposk", [32, 128])
    cposv = din("cposv", [32, 128])
    s0_d = din("strip0", [NH, 128, SW])
    s1_d = din("strip1", [NH, 128, SW])
    logm_d = din("logm", [128, SW])
    winm_d = din("winmask", [128, 768])
    bc_d = din("biasc", [NH, 128, 1024])
    tk_d = din("topk", [2, 128, NOWN * 32])
    ovl_d = din("ovl", [128, 33])
    e_d = din("emat", [32, 2048])
    ident_d = din("ident", [128, 128])
    blend_d = din("blend", [128, 2])
    out_d = nc.dram_tensor("out", [NOWN * 128, D], F32, kind="ExternalOutput")
    og0_d = nc.dram_tensor("og0", [NT, 128, NH, 128], BF16, kind="Internal" if dbg != "og0" else "ExternalOutput")
    h1_d = nc.dram_tensor("h1s", [SEQ, D], F32, kind="Internal" if dbg != "h1" else "ExternalOutput")
    KVW = 2048 + 2048 + 16 * 130 + 16 * 130 + 128 + 176
    kv_d = nc.dram_tensor("kvs", [4, 128, KVW], BF16, kind="Internal" if dbg != "kv" else "ExternalOutput")
    h1o_d = nc.dram_tensor("h1own", [NOWN * 128, D], F32, kind="Internal")
    if dbg == "og1":
        pass
    og1_d = nc.dram_tensor("og1", [NOWN, 128, NH, 128], BF16, kind="Internal" if dbg != "og1" else "ExternalOutput")

    banks = [nc.psum_tensor("bank%d" % i, [128, 512], F32).__enter__() for i in range(8)]
    bankT = [T("bank%d" % i) for i in range(8)]

    ident = A.alloc([128, 128], BF16)
    ident_f = A.alloc([128, 128], F32)
    t_ident = T()
    S.dma("sp", ident_f, ident_d.ap()[:, :], writes=[t_ident])
    S.op("dve", lambda h: h.tensor_copy(ident, ident_f), reads=[t_ident], writes=[t_ident])
    blend = A.alloc([128, 2], F32)
    t_blend = T()
    S.dma("sp", blend, blend_d.ap()[:, :], writes=[t_blend])
    zeros = A.alloc([128, 512], BF16)
    t_zeros = T()
    S.op("dve", lambda h: h.memset(zeros, 0.0), [], [t_zeros])
    persist_mark = A.mark()

    def mm(out, lhsT, rhs, start, stop, reads, writes):
        return S.op("pe", lambda h, o=out, l=lhsT, r=rhs, s=start, e=stop: h.matmul(o, l, r, start=s, stop=e),
                    reads, writes)

    def tr(out, in_, reads, writes):
        return S.op("pe", lambda h, o=out, i=in_: h.transpose(o, i, ident), list(reads) + [t_ident], writes)

    def gain_tile(idx):
        g = A.alloc([128, D], F32)
        tg = T()
        src = bass.AP(gains_d, idx * D, [[0, 128], [1, D]])
        S.dma("sp", g, src, writes=[tg])
        return g, tg

    def norm_to_T(src, t_src, gain, t_gain, dstT, t_dst, col0, junk, t_junk, small, hb_ring, bank_ids):
        ssq, t_ssq = small.next()
        S.op("act", lambda h, j=junk, s=src, a=ssq: h.activation(j, s, AF.Square, accum_out=a[:, 0:1]),
             [t_src], [t_junk, t_ssq])
        S.op("dve", lambda h, a=ssq: h.tensor_scalar(a[:, 1:2], a[:, 0:1], 1.0 / D, EPS, ALU.mult, ALU.add),
             [t_ssq], [t_ssq])
        S.op("act", lambda h, a=ssq: h.activation(a[:, 2:3], a[:, 1:2], AF.Sqrt), [t_ssq], [t_ssq])
        S.op("dve", lambda h, a=ssq: h.reciprocal(a[:, 3:4], a[:, 2:3]), [t_ssq], [t_ssq])
        hb, t_hb = hb_ring.next()
        S.op("dve", lambda h, o=hb, s=src, a=ssq, g=gain: h.scalar_tensor_tensor(o, s, a[:, 3:4], g, ALU.mult, ALU.mult),
             [t_src, t_ssq, t_gain], [t_hb])
        return lambda: norm_stage_b(hb, t_hb, dstT, t_dst, col0, bank_ids)

    def norm_stage_b(hb, t_hb, dstT, t_dst, col0, bank_ids):
        for half in range(2):
            b = bank_ids[half]
            pv = banks[b][:, :].bitcast(BF16).rearrange("p (a c) -> p a c", a=8, c=128)
            for k in range(8):
                c = half * 8 + k
                tr(pv[:, k, :], hb[:, c * 128:(c + 1) * 128], [t_hb], BT(b))
            eng = "act" if half == 0 else "dve"
            dst = dstT[:, half * 8:half * 8 + 8, col0:col0 + 128]
            if eng == "act":
                S.op("act", lambda h, o=dst, i=pv: h.copy(o, i), BT(b), [t_dst[half]])
            else:
                S.op("dve", lambda h, o=dst, i=pv: h.tensor_copy(o, i), BT(b), [t_dst[half]])

    class WPrefetch:
        def __init__(self, reqs, stage_ring, wring, depth, cast_eng):
            self.reqs = reqs
            self.stage_ring, self.wring, self.depth, self.cast_eng = stage_ring, wring, depth, cast_eng
            self.issued = 0
            self.taken = 0
            self.ready = []

        def get(self):
            while self.issued < min(len(self.reqs), self.taken + 1 + self.depth):
                w_d, ncols, c0 = self.reqs[self.issued]
                self.ready.append(load_wslice(w_d, ncols, c0, self.stage_ring, self.wring, cast_eng=self.cast_eng))
                self.issued += 1
            self.taken += 1
            return self.ready.pop(0)

    def load_wslice(w_d, ncols, c0, stage_ring, wring, width=128, cast_eng="pool"):
        st, t_st = stage_ring.next()
        for hh in range(2):
            src = bass.AP(w_d, c0 + hh * 8 * 128 * ncols, [[ncols, 128], [128 * ncols, 8], [1, width]])
            S.dma("sp", st[:, hh * 8:(hh + 1) * 8, 0:width], src, writes=[t_st])
        wb, t_wb = wring.next()
        if cast_eng == "act":
            S.op("act", lambda h, o=wb, i=st, w=width: h.copy(o[:, :, 0:w], i[:, :, 0:w]), [t_st], [t_wb])
        else:
            S.op(cast_eng, lambda h, o=wb, i=st, w=width: h.tensor_copy(o[:, :, 0:w], i[:, :, 0:w]), [t_st], [t_wb])
        return wb, t_wb

    def proj_T(wb, t_wb, srcT, t_srcs, ntok, evac, bank_ids):
        ng = ntok // 512
        for tg in range(ng):
            DQ.tick()
            DQ2.tick()
            b = bank_ids[tg % len(bank_ids)]
            for c in range(16):
                mm(banks[b][:, :], wb[:, c, :], srcT(c, tg * 512, 512), c == 0, c == 15,
                   [t_wb] + t_srcs, BT(b))
            evac(tg, banks[b], BT(b))

    class Deferred:
        def __init__(self):
            self.q = []
            self.t = 0

        def push(self, fn):
            self.q.append((self.t, fn))

        def tick(self, lag=4):
            self.t += 1
            while self.q and self.q[0][0] <= self.t - lag:
                self.q.pop(0)[1]()

        def flush(self):
            while self.q:
                self.q.pop(0)[1]()

    DQ = Deferred()

    class Trickle:
        def __init__(self):
            self.q = []

        def push(self, fn):
            self.q.append(fn)

        def tick(self):
            if self.q:
                self.q.pop(0)()

        def flush(self):
            while self.q:
                self.q.pop(0)()

    DQ2 = Trickle()

    def attention(QT, t_q, n_qt, kt_lo, kt_hi, qbase, qstep, KTt, t_k, Vt, t_v, estrip, t_es,
                  pt_ring, finish, o_slots, extra=None, vw=129, s_banks=(0, 1, 6)):
        es3 = estrip.rearrange("p (n c) -> p n c", c=128)
        step_no = [0]
        for g in range((n_qt + 3) // 4):
            tiles = list(range(4 * g, min(4 * g + 4, n_qt)))
            lo = min(kt_lo(i) for i in tiles)
            hi = max(kt_hi(i) for i in tiles)
            oslot = {}
            for i in tiles:
                oslot[i] = o_slots.next()
            steps = []
            for ki in range(lo, hi + 1):
                act = [i for i in tiles if kt_lo(i) <= ki <= kt_hi(i)]
                if not act:
                    continue
                steps.append((ki, act[0], act[-1] + 1))

            def front(st):
                ki, ia, ib = st
                n = ib - ia
                b = s_banks[step_no[0] % len(s_banks)]
                step_no[0] += 1
                N = n * 128
                mm(banks[b][:, 0:N], KTt(ki), QT[:, ia * 128:ib * 128], True, False, [t_k, t_q], BT(b))
                if extra is not None:
                    el, er, et = extra(ki, ia, ib)
                    mm(banks[b][:, 0:N], el, er, False, False, et, BT(b))
                b0 = qbase(ia) - ki
                if qstep == 1:
                    mm(banks[b][:, 0:N], ident, estrip[:, b0 * 128:(b0 + n) * 128], False, True, [t_es, t_ident], BT(b))
                else:
                    esv = es3[:, b0:b0 + (n - 1) * qstep + 1:qstep, :]
                    mm(banks[b][:, 0:N].rearrange("p (n c) -> p n c", c=128), ident, esv, False, True,
                       [t_es, t_ident], BT(b))
                pt, t_pt = pt_ring.next()
                S.op("act", lambda h, o=pt[:, 0:N], i=banks[b][:, 0:N]: h.activation(o, i, AF.Exp, scale=SCALE),
                     BT(b), [t_pt])
                return (ki, ia, ib, pt, t_pt)

            def back(fr):
                ki, ia, ib, pt, t_pt = fr
                DQ.tick()
                DQ2.tick()
                for i in range(ia, ib):
                    oap, t_o = oslot[i]
                    mm(oap[:, 0:vw], pt[:, (i - ia) * 128:(i - ia + 1) * 128], Vt(ki), ki == kt_lo(i),
                       ki == kt_hi(i), [t_pt, t_v], [t_o])
                    if ki == kt_hi(i):
                        DQ.push(finish(i, oap, t_o))

            fq = []
            for st in steps:
                fq.append(front(st))
                if len(fq) > 2:
                    back(fq.pop(0))
            while fq:
                back(fq.pop(0))

    def make_oslots():
        r = Ring([banks[b][:, :] for b in (2, 3, 4, 5)])
        r.ts = [bankT[b] for b in (2, 3, 4, 5)]
        return r

    tslots = Ring([banks[7][:, 0:64]])
    tslots.ts = [bankT[7]]

    def BT(b):
        return [bankT[b]]

    hT = A.alloc([128, 16, SEQ], BF16)
    t_hT = [[T(), T()] for t in range(NT)]
    p0_mark = A.mark()
    g0, t_g0 = gain_tile(0)
    xs_ring = Ring([A.alloc([128, D], F32) for _ in range(4)])
    hb_ring = Ring([A.alloc([128, D], BF16) for _ in range(2)])
    junk = A.alloc([128, D], BF16)
    t_junk = T()
    small = Ring([A.alloc([128, 4], F32) for _ in range(4)])
    prev_b = None
    for t in range(NT):
        xs, t_xs = xs_ring.next()
        S.dma("sp", xs, x_d.ap()[t * 128:(t + 1) * 128, :], writes=[t_xs])
        stb = norm_to_T(xs, t_xs, g0, t_g0, hT, t_hT[t], t * 128, junk, t_junk, small, hb_ring, (6, 7))
        if prev_b is not None:
            prev_b()
        prev_b = stb
    prev_b()
    S.barrier()
    A.reset(p0_mark)
    final_ops = []
    if dbg == "hT":
        dbg_d = nc.dram_tensor("dbg_hT", [128, 16 * SEQ], BF16, kind="ExternalOutput")
        final_ops = [S.dma("sp", dbg_d.ap()[:, c * SEQ:(c + 1) * SEQ], hT[:, c, :], reads=[x_ for p_ in t_hT for x_ in p_], semt=t_hT[c][0]) for c in range(16)]

    if upto >= 1:
        QT = A.alloc([128, SEQ], BF16); t_QT = T()
        KT = A.alloc([128, SEQ], BF16); t_KT = T()
        VT = A.alloc([128, SEQ], BF16); t_VT = T()
        zT = A.alloc([128, SEQ], BF16); t_zT = T()
        Vaug = A.alloc([128, 16, 130], BF16); t_V = T()
        S.op("dve", lambda h: h.memset(Vaug, 1.0), [], [t_V])
        logm = A.alloc([128, SW], F32); t_logm = T()
        S.dma("sp", logm, logm_d.ap()[:, :], writes=[t_logm])
        S.op("dve", lambda h: h.tensor_scalar(logm, logm, 1.0 / SCALE, None, ALU.mult), [t_logm], [t_logm])
        strip_ring = Ring([A.alloc([128, SW], F32) for _ in range(1)])
        esb_ring = Ring([A.alloc([128, SW], BF16) for _ in range(2)])
        stage_ring = Ring([A.alloc([128, 16, 128], F32) for _ in range(3)])
        wring = Ring([A.alloc([128, 16, 128], BF16) for _ in range(8)])
        pt_ring = Ring([A.alloc([128, 512], BF16) for _ in range(4)])
        og_ring = Ring([A.alloc([128, SEQ], BF16) for _ in range(2)])
        on_ring = Ring([A.alloc([128, 128], BF16) for _ in range(4)])
        rd_ring = Ring([A.alloc([128, 2], F32) for _ in range(4)])
        o_slots = make_oslots()
        hT_all = [x_ for p_ in t_hT for x_ in p_]
        nheads = NH if upto >= 2 or dbg is None else 1
        wpf = WPrefetch([(w_in_a, 8192, k * 2048 + hd * 128) for hd in range(NH) for k in range(4)],
                        stage_ring, wring, 5, "dve")

        def load_head(hd):
            ws = None
            sf, t_sf = strip_ring.next()
            S.dma("sp", sf, s0_d.ap()[hd, :, :], writes=[t_sf])
            es, t_es = esb_ring.next()
            S.op("dve", lambda h, o=es, e=sf: h.scalar_tensor_tensor(o, e, 1.0 / SCALE, logm, ALU.mult, ALU.add),
                 [t_sf, t_logm], [t_es])
            return ws, es, t_es

        nxt = load_head(0)
        for hd in range(NH):
            _, es, t_es = nxt
            wq = wpf.get()
            srcT = lambda c, c0, n: hT[:, c, c0:c0 + n]

            def evac_copy(dst, t_dst):
                def f(tg, bank, t_bank):
                    S.op("dve", lambda h, o=dst[:, tg * 512:(tg + 1) * 512], i=bank[:, :]: h.tensor_copy(o, i),
                         t_bank, [t_dst])
                return f

            def evac_silu(dst, t_dst):
                def f(tg, bank, t_bank):
                    S.op("act", lambda h, o=dst[:, tg * 512:(tg + 1) * 512], i=bank[:, :]: h.activation(o, i, AF.Silu),
                         t_bank, [t_dst])
                return f

            proj_T(wq[0], wq[1], srcT, hT_all, SEQ, evac_copy(QT, t_QT), (6, 7))
            wk = wpf.get()
            proj_T(wk[0], wk[1], srcT, hT_all, SEQ, evac_copy(KT, t_KT), (6, 7))
            wv = wpf.get()
            proj_T(wv[0], wv[1], srcT, hT_all, SEQ, evac_copy(VT, t_VT), (6, 7))
            wz = wpf.get()
            proj_T(wz[0], wz[1], srcT, hT_all, SEQ, evac_silu(zT, t_zT), (6, 7))
            if hd + 1 < NH:
                nxt = load_head(hd + 1)
            for half in range(2):
                b = 6 + half
                pv = banks[b][:, :].bitcast(BF16).rearrange("p (a c) -> p a c", a=8, c=128)
                for k in range(8):
                    t = half * 8 + k
                    tr(pv[:, k, :], VT[:, t * 128:(t + 1) * 128], [t_VT], BT(b))
                S.op("dve", lambda h, o=Vaug[:, half * 8:half * 8 + 8, 0:128], i=pv: h.tensor_copy(o, i),
                     BT(b), [t_V])
            og, t_og = og_ring.next()

            def finish(i, oap, t_o, og=og, t_og=t_og):
                rd, t_rd = rd_ring.next()
                S.op("dve", lambda h, o=rd, a=oap: h.reciprocal(o[:, 0:1], a[:, 128:129]), [t_o], [t_rd])
                on, t_on = on_ring.next()
                S.op("dve", lambda h, o=on, a=oap, r=rd: h.tensor_scalar(o, a[:, 0:128], r[:, 0:1], None, ALU.mult),
                     [t_o, t_rd], [t_on])

                def later(i=i, on=on, t_on=t_on):
                    tp, t_tp = tslots.next()
                    pv = tp.bitcast(BF16)
                    tr(pv, on, [t_on], [t_tp])
                    S.op("dve", lambda h, o=og[:, i * 128:(i + 1) * 128], p=pv, z=zT[:, i * 128:(i + 1) * 128]:
                         h.tensor_tensor(o, p, z, ALU.mult), [t_tp, t_zT], [t_og])
                return later

            attention(QT, t_QT, NT, lambda i: 0, lambda i: i, lambda i: i, 1,
                      lambda ki: KT[:, ki * 128:(ki + 1) * 128], t_KT,
                      lambda ki: Vaug[:, ki, 0:129], t_V, es, t_es, pt_ring, finish, o_slots)
            dst = og0_d.ap()[:, :, hd, :].rearrange("t p c -> p t c")
            def spill(dst=dst, og=og, t_og=t_og):
                o_sp = S.dma("pool", dst, og.rearrange("p (t c) -> p t c", c=128), reads=[t_og])
                if dbg == "og0":
                    final_ops.append(o_sp)
            DQ.push(spill)
        DQ.flush()
        S.barrier()
        A.reset(p0_mark)

    def outproj(w_d, gain_idx, og_d, n_tiles, resid_fn, dst_fn):
        m = A.mark()
        wo = hT
        t_wos = [T() for _ in range(16)]
        st_ring = Ring([A.alloc([128, D], F32) for _ in range(3)])
        for c in range(16):
            st, t_st = st_ring.next()
            S.dma("sp" if c % 2 == 0 else "act", st, w_d.ap()[c * 128:(c + 1) * 128, :], writes=[t_st])
            ce = ("dve", "act")[c % 2]
            if ce == "act":
                S.op("act", lambda h, o=wo[:, c, :], i=st: h.copy(o, i), [t_st], [t_wos[c]])
            else:
                S.op(ce, lambda h, o=wo[:, c, :], i=st: h.tensor_copy(o, i), [t_st], [t_wos[c]])
        gp, t_gp = gain_tile(gain_idx)
        ogt_ring = Ring([A.alloc([128, 16, 128], BF16) for _ in range(2)])
        xs_ring2 = Ring([A.alloc([128, D], F32) for _ in range(2)])
        h1_ring = Ring([A.alloc([128, D], F32) for _ in range(2)])
        small2 = Ring([A.alloc([128, 8], F32) for _ in range(4)])
        junk2 = A.alloc([128, 512], BF16); t_junk2 = T()
        last = []
        for t in range(n_tiles):
            ogt, t_ogt = ogt_ring.next()
            S.dma("sp", ogt, og_d.ap()[t, :, :, :], writes=[t_ogt])
            bs = (0, 1, 2, 3) if t % 2 == 0 else (4, 5, 6, 7)
            for n in range(4):
                b = bs[n]
                for c in range(16):
                    mm(banks[b][:, :], ogt[:, c, :], wo[:, c, n * 512:(n + 1) * 512], c == 0, c == 15,
                       [t_ogt, t_wos[c]], BT(b))
            sm, t_sm = small2.next()
            for n in range(4):
                b = bs[n]
                S.op("act", lambda h, j=junk2, i=banks[b][:, :], a=sm[:, n:n + 1]: h.activation(j, i, AF.Square, accum_out=a),
                     BT(b), [t_junk2, t_sm])
            S.op("dve", lambda h, a=sm: h.tensor_reduce(a[:, 4:5], a[:, 0:4], AX.X, ALU.add), [t_sm], [t_sm])
            S.op("dve", lambda h, a=sm: h.tensor_scalar(a[:, 5:6], a[:, 4:5], 1.0 / D, EPS, ALU.mult, ALU.add), [t_sm], [t_sm])
            S.op("act", lambda h, a=sm: h.activation(a[:, 6:7], a[:, 5:6], AF.Sqrt), [t_sm], [t_sm])
            S.op("dve", lambda h, a=sm: h.reciprocal(a[:, 7:8], a[:, 6:7]), [t_sm], [t_sm])
            res, t_res = resid_fn(t, xs_ring2)
            h1, t_h1 = h1_ring.next()
            for n in range(4):
                b = bs[n]
                S.op("dve", lambda h, o=h1[:, n * 512:(n + 1) * 512], i=banks[b][:, :], a=sm, g=gp[:, n * 512:(n + 1) * 512]:
                     h.scalar_tensor_tensor(o, i, a[:, 7:8], g, ALU.mult, ALU.mult), BT(b) + [t_sm, t_gp], [t_h1])
            S.op("dve", lambda h, o=h1, r=res: h.tensor_tensor(o, o, r, ALU.add), [t_h1, t_res], [t_h1])
            last.append(S.dma("pool", dst_fn(t), h1, reads=[t_h1]))
        S.barrier()
        A.reset(m)
        return last

    if upto >= 2:
        def resid0(t, ring):
            xs, t_xs = ring.next()
            S.dma("sp", xs, x_d.ap()[t * 128:(t + 1) * 128, :], writes=[t_xs])
            return xs, t_xs
        final_ops = outproj(w_out_a, 1, og0_d, NT, resid0, lambda t: h1_d.ap()[t * 128:(t + 1) * 128, :])

    if upto >= 3:
        m3 = A.mark()
        gk, t_gk = gain_tile(2)
        xs_ring = Ring([A.alloc([128, D], F32) for _ in range(4)])
        hb_ring = Ring([A.alloc([128, D], BF16) for _ in range(2)])
        junk = A.alloc([128, D], BF16); t_junk = T()
        small = Ring([A.alloc([128, 4], F32) for _ in range(4)])
        t_hT = [[T(), T()] for t in range(NT)]
        prev_b = None
        for t in range(NT):
            xs, t_xs = xs_ring.next()
            S.dma("sp", xs, h1_d.ap()[t * 128:(t + 1) * 128, :], writes=[t_xs])
            stb = norm_to_T(xs, t_xs, gk, t_gk, hT, t_hT[t], t * 128, junk, t_junk, small, hb_ring, (6, 7))
            if prev_b is not None:
                prev_b()
            prev_b = stb
        prev_b()
        S.barrier()
        A.reset(m3)
        stage_ring = Ring([A.alloc([128, 16, 128], F32) for _ in range(2)])
        wring = Ring([A.alloc([128, 16, 128], BF16) for _ in range(6)])
        w1 = [A.alloc([128, 32, 256], BF16) for _ in range(2)]
        t_w1 = [T(), T()]
        w2 = [A.alloc([128, 2, 128], BF16) for _ in range(2)]
        t_w2 = [T(), T()]
        posT = [A.alloc([128, 32], BF16) for _ in range(2)]
        t_posT = [T(), T()]
        pbias = [A.alloc([128, 2], F32) for _ in range(2)]
        t_pb = [T(), T()]
        ovlb = A.alloc([128, 33], BF16); t_ovl = T()
        for kvi, (w1_d, w2_d, pos_d) in enumerate(((cw1k, cw2k, cposk), (cw1v, cw2v, cposv))):
            for q4 in range(4):
                st, t_st = stage_ring.next()
                stv = st.rearrange("p a b -> p (a b)").rearrange("p (i n) -> p i n", i=8, n=256)
                src = bass.AP(w1_d, q4 * 8 * 128 * 256, [[256, 128], [128 * 256, 8], [1, 256]])
                S.dma("sp", stv, src, writes=[t_st])
                S.op("dve", lambda h, o=w1[kvi][:, q4 * 8:(q4 + 1) * 8, :], i=stv: h.tensor_copy(o, i), [t_st], [t_w1[kvi]])
            st, t_st = stage_ring.next()
            stv = st.rearrange("p a b -> p (a b)")[:, 0:256].rearrange("p (i n) -> p i n", i=2, n=128)
            src = bass.AP(w2_d, 0, [[128, 128], [128 * 128, 2], [1, 128]])
            S.dma("sp", stv, src, writes=[t_st])
            S.op("dve", lambda h, o=w2[kvi], i=stv: h.tensor_copy(o, i), [t_st], [t_w2[kvi]])
            st, t_st = stage_ring.next()
            stf = st.rearrange("p a b -> p (a b)")
            S.dma("sp", stf[0:32, 0:128], pos_d.ap()[:, :], writes=[t_st])
            S.op("dve", lambda h, o=stf[0:32, 256:320].bitcast(BF16), i=stf[0:32, 0:128]: h.tensor_copy(o, i), [t_st], [t_st])
            pv = banks[6][:, 0:16].bitcast(BF16)
            S.op("pe", lambda h, o=pv, i=stf[0:32, 256:320].bitcast(BF16): h.transpose(o, i, ident[0:32, 0:32]),
                 [t_st, t_ident], BT(6))
            S.op("dve", lambda h, o=posT[kvi], i=pv: h.tensor_copy(o, i), BT(6), [t_posT[kvi]])
            for hc in range(2):
                for i in range(32):
                    mm(banks[7][:, hc:hc + 1], w1[kvi][:, i, hc * 128:(hc + 1) * 128], posT[kvi][:, i:i + 1], i == 0, i == 31,
                       [t_w1[kvi], t_posT[kvi]], BT(7))
                S.op("dve", lambda h, o=pbias[kvi][:, hc:hc + 1], i=banks[7][:, hc:hc + 1]: h.tensor_copy(o, i),
                     BT(7), [t_pb[kvi]])
        st, t_st = stage_ring.next()
        stf = st.rearrange("p a b -> p (a b)")
        S.dma("sp", stf[:, 0:33], ovl_d.ap()[:, :], writes=[t_st])
        S.op("dve", lambda h, o=ovlb, i=stf[:, 0:33]: h.tensor_copy(o, i), [t_st], [t_ovl])

        tmpT = [A.alloc([128, SEQ], BF16) for _ in range(4)]
        t_tmpT = [T() for _ in range(4)]
        kvbuf = A.alloc([128, KVW], BF16); t_kv = T()
        O_KS, O_KW, O_VS, O_VW, O_KC, O_VC = 0, 2048, 4096, 4096 + 2080, 4096 + 4160, 4096 + 4160 + 128
        xg = A.alloc([128, 128], F32); t_xg = T()
        x2 = A.alloc([128, 128], F32); t_x2 = T()
        gT = [[A.alloc([128, 128], BF16) for _ in range(2)] for _ in range(2)]
        t_gT = [[T(), T()], [T(), T()]]
        hT_all = [x_ for p_ in t_hT for x_ in p_]
        srcT = lambda c, c0, n: hT[:, c, c0:c0 + n]

        def evac_to(dst, t_dst, eng="dve"):
            def f(tg, bank, t_bank):
                if eng == "dve":
                    S.op("dve", lambda h, o=dst[:, tg * 512:(tg + 1) * 512], i=bank[:, :]: h.tensor_copy(o, i), t_bank, [t_dst])
                else:
                    S.op("act", lambda h, o=dst[:, tg * 512:(tg + 1) * 512], i=bank[:, :]: h.copy(o, i), t_bank, [t_dst])
            return f

        kv_out = []
        wpf3 = WPrefetch([(w_kv, 3072, i * 512 + g * 128) for g in range(4) for i in range(6)], stage_ring, wring, 4, "dve")
        for g in range(4):
            S.op("dve", lambda h, o=kvbuf[:, O_VS:O_KC]: h.memset(o, 1.0), [], [t_kv])
            S.op("dve", lambda h, o=kvbuf[:, O_KC:KVW]: h.memset(o, 0.0), [], [t_kv])
            S.op("dve", lambda h, o=kvbuf[:, O_VC + 128:O_VC + 161], i=ovlb: h.tensor_copy(o, i), [t_ovl], [t_kv])
            dsts = [(tmpT[0], t_tmpT[0]), (tmpT[1], t_tmpT[1]), (kvbuf[:, O_KS:O_KS + 2048], t_kv),
                    (tmpT[2], t_tmpT[2]), (kvbuf[:, O_KW:O_KW + 2048], t_kv), (tmpT[3], t_tmpT[3])]
            for i in range(6):
                wsi = wpf3.get()
                proj_T(wsi[0], wsi[1], srcT, hT_all, SEQ, evac_to(dsts[i][0], dsts[i][1], "dve" if i % 2 == 0 else "act"), (4, 5))
            for which, off in ((2, O_VS), (3, O_VW)):
                aug = kvbuf[:, off:off + 2080].rearrange("p (t c) -> p t c", c=130)
                for half in range(2):
                    b = 6 + half
                    pv = banks[b][:, :].bitcast(BF16).rearrange("p (a c) -> p a c", a=8, c=128)
                    for k in range(8):
                        t = half * 8 + k
                        tr(pv[:, k, :], tmpT[which][:, t * 128:(t + 1) * 128], [t_tmpT[which]], BT(b))
                    S.op("dve", lambda h, o=aug[:, half * 8:half * 8 + 8, 0:128], i=pv: h.tensor_copy(o, i), BT(b), [t_kv])
            for kvi in range(2):
                srcv = tmpT[kvi].rearrange("p (c i) -> p c i", i=16)
                for hc in range(2):
                    b = 4 + hc
                    for i in range(32):
                        mm(banks[b][:, 0:127], w1[kvi][:, i, hc * 128:(hc + 1) * 128],
                           srcv[:, (i // 16):(i // 16) + 127, i % 16], i == 0, i == 31, [t_w1[kvi], t_tmpT[kvi]], BT(b))
                    S.op("act", lambda h, o=xg[:, 0:127], i=banks[b][:, 0:127], bb=pbias[kvi][:, hc:hc + 1]:
                         h.activation(o, i, AF.Identity, bias=bb), BT(b) + [t_pb[kvi]], [t_xg])
                    S.op("dve", lambda h: h.tensor_tensor(x2[:, 0:127], xg[:, 0:127], xg[:, 0:127], ALU.mult), [t_xg], [t_x2])
                    S.op("dve", lambda h: h.tensor_scalar(x2[:, 0:127], x2[:, 0:127], 0.044715, 1.0, ALU.mult, ALU.add), [t_x2], [t_x2])
                    S.op("dve", lambda h: h.tensor_tensor(x2[:, 0:127], x2[:, 0:127], xg[:, 0:127], ALU.mult), [t_x2, t_xg], [t_x2])
                    S.op("act", lambda h: h.activation(x2[:, 0:127], x2[:, 0:127], AF.Sigmoid, scale=1.5957691216057308), [t_x2], [t_x2])
                    S.op("dve", lambda h, o=gT[kvi][hc][:, 0:127]: h.tensor_tensor(o, x2[:, 0:127], xg[:, 0:127], ALU.mult),
                         [t_x2, t_xg], [t_gT[kvi][hc]])
                if kvi == 0:
                    for hc in range(2):
                        mm(banks[6][:, 0:127], w2[0][:, hc, :], gT[0][hc][:, 0:127], hc == 0, hc == 1,
                           [t_w2[0], t_gT[0][hc]], BT(6))
                    S.op("dve", lambda h, o=kvbuf[:, O_KC:O_KC + 127], i=banks[6][:, 0:127]: h.tensor_copy(o, i), BT(6), [t_kv])
                else:
                    for hc in range(2):
                        mm(banks[7][0:127, 0:128], gT[1][hc][:, 0:127], w2[1][:, hc, :], hc == 0, hc == 1,
                           [t_w2[1], t_gT[1][hc]], BT(7))
                    S.op("dve", lambda h, o=kvbuf[0:127, O_VC:O_VC + 128], i=banks[7][0:127, 0:128]: h.tensor_copy(o, i),
                         BT(7), [t_kv])
            kv_out.append(S.dma("pool", kv_d.ap()[g, :, :], kvbuf, reads=[t_kv]))
        final_ops = kv_out
        S.barrier()
        A.reset(m3)

    if upto >= 4:
        m4 = A.mark()
        A2 = Arena(nc, 65536, flat=hT.rearrange("p a b -> p (a b)"))
        hn1T = A2.alloc([128, 16, NOWN * 128], BF16)
        t_hn1 = [[T(), T()] for _ in range(NOWN)]
        gate = A.alloc([128, NOWN, 48], F32); t_gate = T()
        stage_ring = Ring([A.alloc([128, 16, 128], F32) for _ in range(2)])
        wring = Ring([A.alloc([128, 16, 128], BF16) for _ in range(6)])
        m4a = A.mark()
        gp1, t_gp1 = gain_tile(3)
        xs_ring = Ring([A.alloc([128, D], F32) for _ in range(6)])
        hb_ring = Ring([A.alloc([128, D], BF16) for _ in range(2)])
        junk = A.alloc([128, D], BF16); t_junk = T()
        small = Ring([A.alloc([128, 4], F32) for _ in range(4)])
        wg, t_wg = load_wslice(w_in_b, 8240, 8192, stage_ring, wring, width=48)
        prev_b = None
        for i in range(NOWN):
            xs0, t_x0 = xs_ring.next()
            xs1, t_x1 = xs_ring.next()
            S.dma("sp", xs0, h1_d.ap()[(2 * i) * 128:(2 * i + 1) * 128, :], writes=[t_x0])
            S.dma("sp", xs1, h1_d.ap()[(2 * i + 1) * 128:(2 * i + 2) * 128, :], writes=[t_x1])
            S.op("dve", lambda h, a=xs0: h.tensor_scalar(a, a, blend[:, 0:1], None, ALU.mult), [t_x0, t_blend], [t_x0])
            S.op("dve", lambda h, a=xs0, b=xs1: h.scalar_tensor_tensor(a, b, blend[:, 1:2], a, ALU.mult, ALU.add),
                 [t_x0, t_x1, t_blend], [t_x0])
            S.dma("pool", h1o_d.ap()[i * 128:(i + 1) * 128, :], xs0, reads=[t_x0])
            stb = norm_to_T(xs0, t_x0, gp1, t_gp1, hn1T, t_hn1[i], i * 128, junk, t_junk, small, hb_ring, (6, 7))

            def stage_b(i=i, stb=stb):
                stb()
                b = 4 + (i % 2)
                for c in range(16):
                    mm(banks[b][:, 0:48], hn1T[:, c, i * 128:(i + 1) * 128], wg[:, c, 0:48], c == 0, c == 15,
                       t_hn1[i] + [t_wg], BT(b))
                S.op("act", lambda h, o=gate[:, i, :], p=banks[b][:, 0:48]: h.activation(o, p, AF.Sigmoid), BT(b), [t_gate])
            if prev_b is not None:
                prev_b()
            prev_b = stage_b
        prev_b()
        S.barrier()
        A.reset(m4a)
        tkc = A.alloc([128, 2, NOWN, 32], F32); t_tkc = T()
        S.dma("sp", tkc.rearrange("p a b c -> p a (b c)"), tk_d.ap().rearrange("a p n -> p a n"), writes=[t_tkc])
        emat = A.alloc([128, 2048], BF16); t_emat = T()
        S.op("dve", lambda h: h.memset(emat, 0.0), [], [t_emat])
        for q4 in range(2):
            st, t_st = stage_ring.next()
            stf = st.rearrange("p a b -> p (a b)")
            S.dma("sp", stf[0:32, 0:1024], e_d.ap()[:, q4 * 1024:(q4 + 1) * 1024], writes=[t_st])
            S.op("dve", lambda h, o=emat[0:32, q4 * 1024:(q4 + 1) * 1024], i=stf[0:32, 0:1024]: h.tensor_copy(o, i), [t_st, t_emat], [t_emat])
        winm = A.alloc([128, 768], F32); t_winm = T()
        S.dma("sp", winm, winm_d.ap()[:, :], writes=[t_winm])
        S.op("dve", lambda h: h.tensor_scalar(winm, winm, 1.0 / SCALE, None, ALU.mult), [t_winm], [t_winm])
        kvbuf = A2.alloc([128, KVW], BF16); t_kv = T()
        O_KS, O_KW, O_VS, O_VW, O_KC, O_VC = 0, 2048, 4096, 4096 + 2080, 4096 + 4160, 4096 + 4160 + 128
        QTs = [A2.alloc([128, NOWN * 128], BF16) for _ in range(4)]
        t_QTs = [T() for _ in range(4)]
        ocn = [A.alloc([128, NOWN, 128], BF16) for _ in range(4)]
        t_ocn = [T() for _ in range(4)]
        zsets = [[A.alloc([128, NOWN * 128], BF16) for _ in range(3)] for _ in range(2)]
        t_zsets = [[T() for _ in range(3)] for _ in range(2)]
        sf_ring = Ring([A.alloc([128, SW], F32) for _ in range(1)])
        es_ring = Ring([A.alloc([128, SW], BF16) for _ in range(2)])
        wes_ring = Ring([A.alloc([128, 768], BF16) for _ in range(2)])
        bcf_ring = Ring([A.alloc([128, NOWN * 128], F32) for _ in range(1)])
        bc_ring = Ring([A.alloc([128, NOWN * 128], BF16) for _ in range(2)])
        yTs = [A2.alloc([128, NOWN * 128], F32), A.alloc([128, NOWN * 128], F32)]
        t_yTs = [T(), T()]
        tmpf = Ring([A.alloc([128, 128], F32) for _ in range(3)])
        og_ring = Ring([A.alloc([128, NOWN * 128], BF16) for _ in range(2)])
        pt_ring = Ring([A.alloc([128, 512], BF16) for _ in range(4)])
        on_ring = Ring([A.alloc([128, 128], BF16) for _ in range(4)])
        rd_ring = Ring([A.alloc([128, 4], F32) for _ in range(6)])
        imp = A.alloc([128, NOWN, 32], F32); t_imp = T()
        impf = A.alloc([128, 32], F32); t_impf = T()
        imp2 = A.alloc([128, 32], F32); t_imp2 = T()
        m8 = A.alloc([128, 16], F32); t_m8 = T()
        selb_ring = Ring([A.alloc([128, 32], BF16) for _ in range(8)])
        selmT = A.alloc([128, NOWN * 128], BF16); t_selmT = T()
        S.op("dve", lambda h: h.memset(selmT, 0.0), [], [t_selmT])
        o_slots = make_oslots()
        hn_all = [x_ for p_ in t_hn1 for x_ in p_]
        srcT1 = lambda c, c0, n: hn1T[:, c, c0:c0 + n]
        KsT = kvbuf[:, O_KS:O_KS + 2048]
        KwT = kvbuf[:, O_KW:O_KW + 2048]
        Vs = kvbuf[:, O_VS:O_VS + 2080].rearrange("p (t c) -> p t c", c=130)
        Vw = kvbuf[:, O_VW:O_VW + 2080].rearrange("p (t c) -> p t c", c=130)
        KcT = kvbuf[:, O_KC:O_KC + 128]
        Vc = kvbuf[:, O_VC:O_VC + 161]

        def evac_copy1(dst, t_dst):
            def f(tg, bank, t_bank):
                S.op("dve", lambda h, o=dst[:, tg * 512:(tg + 1) * 512], i=bank[:, :]: h.tensor_copy(o, i), t_bank, [t_dst])
            return f

        def evac_silu1(dst, t_dst):
            def f(tg, bank, t_bank):
                S.op("act", lambda h, o=dst[:, tg * 512:(tg + 1) * 512], i=bank[:, :]: h.activation(o, i, AF.Silu), t_bank, [t_dst])
            return f

        og_out = []
        reqs4 = [(w_in_b, 8240, j_ * 128) for j_ in range(4)]
        for g_ in range(4):
            for j_ in range(4):
                for br_ in range(3):
                    reqs4.append((w_in_b, 8240, 2048 + br_ * 2048 + (4 * g_ + j_) * 128))
                if g_ < 3:
                    reqs4.append((w_in_b, 8240, (4 * (g_ + 1) + j_) * 128))
        wpf4 = WPrefetch(reqs4, stage_ring, wring, 4, "act")
        P4CUT = 99
        imps = [imp, A.alloc([128, NOWN, 32], F32)]
        t_imps = [t_imp, T()]
        kcbufs = [A.alloc([128, KVW - O_KC], BF16) for _ in range(2)]
        t_kcs = [T(), T()]

        def load_kc(g):
            S.dma("sp", kcbufs[g % 2], kv_d.ap()[g, :, O_KC:KVW], writes=[t_kcs[g % 2]])
            S.op("dve", lambda h, o=imps[g % 2]: h.memset(o, 0.0), [], [t_imps[g % 2]])

        ptc = [A.alloc([128, 512], BF16) for _ in range(2)]
        t_ptc = [T(), T()]

        def pass1_head(g, j):
            DQ2.flush()
            hd = 4 * g + j
            kc, t_kc = kcbufs[g % 2], t_kcs[g % 2]
            KcT_g = kc[:, 0:128]
            Vc_g = kc[:, 128:128 + 161]
            imp_g, t_imp_g = imps[g % 2], t_imps[g % 2]
            wq = wpf4.get()
            bcf, t_bcf = bcf_ring.next()
            S.dma("sp", bcf, bc_d.ap()[hd, :, :], writes=[t_bcf])
            bc, t_bc = bc_ring.next()
            S.op("act", lambda h, o=bc, e=bcf: h.activation(o, e, AF.Copy, scale=1.0 / SCALE), [t_bcf], [t_bc])
            proj_T(wq[0], wq[1], srcT1, hn_all, NOWN * 128, evac_copy1(QTs[j], t_QTs[j]), (6, 7))
            for half in range(2):
                b = half
                mm(banks[b][0:127, :], KcT_g[:, 0:127], QTs[j][:, half * 512:(half + 1) * 512], True, False,
                   [t_kc, t_QTs[j]], BT(b))
                mm(banks[b][0:127, :], ident[0:127, 0:127], bc[0:127, half * 512:(half + 1) * 512], False, True,
                   [t_bc, t_ident], BT(b))
                pt, t_pt = ptc[half], t_ptc[half]
                S.op("act", lambda h, o=pt[0:127, :], i=banks[b][0:127, :]: h.activation(o, i, AF.Exp, scale=SCALE),
                     BT(b), [t_pt])
                for ii in range(4):
                    def tile_work(ii=ii, half=half, pt=pt, t_pt=t_pt):
                        i = half * 4 + ii
                        ob = 7
                        mm(banks[ob][:, 0:161], pt[0:127, ii * 128:(ii + 1) * 128], Vc_g[0:127, :], True, True,
                           [t_pt, t_kc], BT(ob))
                        rd, t_rd = rd_ring.next()
                        S.op("dve", lambda h, o=rd, a=banks[ob]: h.tensor_scalar(o[:, 0:1], a[:, 160:161], 1e-30, None, ALU.max),
                             BT(ob), [t_rd])
                        S.op("dve", lambda h, o=rd: h.reciprocal(o[:, 1:2], o[:, 0:1]), [t_rd], [t_rd])
                        S.op("dve", lambda h, o=imp_g[:, i, :], a=banks[ob], r=rd: h.scalar_tensor_tensor(o, a[:, 128:160], r[:, 1:2], o, ALU.mult, ALU.add),
                             BT(ob) + [t_rd, t_imp_g], [t_imp_g])
                        S.op("dve", lambda h, r=rd, gg=gate[:, i, hd:hd + 1]: h.tensor_tensor(r[:, 2:3], r[:, 1:2], gg, ALU.mult),
                             [t_rd, t_gate], [t_rd])
                        S.op("dve", lambda h, o=ocn[j][:, i, :], a=banks[ob], r=rd: h.tensor_scalar(o, a[:, 0:128], r[:, 2:3], None, ALU.mult),
                             BT(ob) + [t_rd], [t_ocn[j]])
                    DQ2.push(tile_work)

        load_kc(0)
        for j in range(4):
            pass1_head(0, j)
        DQ2.flush()
        for g in range(4):
            S.dma("sp", kvbuf, kv_d.ap()[g, :, :], writes=[t_kv])
            imp, t_imp = imps[g % 2], t_imps[g % 2]
            def zproj(hd):
                for br in range(3):
                    wz = wpf4.get()
                    proj_T(wz[0], wz[1], srcT1, hn_all, NOWN * 128, evac_silu1(zsets[hd % 2][br], t_zsets[hd % 2][br]), (6, 7))

            def prep_strips(hd):
                sf, t_sf = sf_ring.next()
                S.dma("sp", sf, s1_d.ap()[hd, :, :], writes=[t_sf])
                es, t_es = es_ring.next()
                wes, t_wes = wes_ring.next()
                S.op("dve", lambda h, o=wes, e=sf: h.scalar_tensor_tensor(o, e[:, 0:768], 1.0 / SCALE, winm, ALU.mult, ALU.add),
                     [t_sf, t_winm], [t_wes])
                S.op("act", lambda h, o=es, e=sf: h.activation(o, e, AF.Copy, scale=1.0 / SCALE), [t_sf], [t_es])
                return es, t_es, wes, t_wes

            if P4CUT > 3:
                nxt_strips = prep_strips(4 * g)
                zproj(4 * g)
            DQ2.flush()
            if P4CUT <= 2:
                break
            for i in range(NOWN):
                S.op("dve", lambda h, a=imp[:, i, :], k=tkc[:, 0, i, :]: h.tensor_tensor(impf, a, k, ALU.mult), [t_imp, t_tkc], [t_impf])
                S.op("dve", lambda h, f=tkc[:, 1, i, :]: h.tensor_tensor(impf, impf, f, ALU.add), [t_impf, t_tkc], [t_impf])
                S.op("dve", lambda h: h.max(m8[:, 0:8], impf), [t_impf], [t_m8])
                S.op("dve", lambda h: h.match_replace(imp2, m8[:, 0:8], impf, -3e9), [t_impf, t_m8], [t_imp2])
                S.op("dve", lambda h: h.max(m8[:, 8:16], imp2), [t_imp2], [t_m8])
                S.op("dve", lambda h: h.tensor_scalar(imp2, impf, m8[:, 15:16], None, ALU.is_ge), [t_impf, t_m8], [t_imp2])
                sb, t_sb = selb_ring.next()
                S.op("dve", lambda h, sb=sb: h.tensor_scalar(sb, imp2, 29952.0, -29952.0, ALU.mult, ALU.add), [t_imp2], [t_sb])
                def selT_later(i=i, sb=sb, t_sb=t_sb):
                    tp, t_tp = tslots.next()
                    pv = tp[0:32, :].bitcast(BF16)
                    tr(pv, sb, [t_sb], [t_tp])
                    S.op("dve", lambda h, o=selmT[0:32, i * 128:(i + 1) * 128], p=pv: h.tensor_copy(o, p), [t_tp, t_selmT], [t_selmT])
                DQ.push(selT_later)
            if P4CUT <= 3 and P4CUT < 30:
                break
            for j in range(4 if P4CUT > 10 else 1):
                hd = 4 * g + j
                if j > 0:
                    zproj(hd)
                zTs, t_zTs = zsets[hd % 2], t_zsets[hd % 2]
                yT, t_yT = yTs[hd % 2], t_yTs[hd % 2]
                es, t_es, wes, t_wes = nxt_strips
                if j < 3:
                    nxt_strips = prep_strips(hd + 1)
                if P4CUT == 32:
                    break
                og, t_og = og_ring.next()
                pv8 = banks[7][:, :].bitcast(BF16)
                for i in range(NOWN):
                    tr(pv8[:, i * 128:(i + 1) * 128], ocn[j][:, i, :], [t_ocn[j]], BT(7))
                S.op("dve", lambda h, o=yT, p=pv8, z=zTs[0]: h.tensor_tensor(o, p, z, ALU.mult), BT(7) + [t_zTs[0]], [t_yT])

                def mk_finish(br, gcol, last, zTs=zTs, t_zTs=t_zTs, yT=yT, t_yT=t_yT):
                    def finish(i, oap, t_o, og=og, t_og=t_og):
                        rd, t_rd = rd_ring.next()
                        S.op("dve", lambda h, o=rd, a=oap: h.reciprocal(o[:, 0:1], a[:, 128:129]), [t_o], [t_rd])
                        on, t_on = on_ring.next()
                        S.op("dve", lambda h, o=on, a=oap, r=rd, gg=gate[:, i, gcol:gcol + 1]:
                             h.tensor_scalar(o, a[:, 0:128], r[:, 0:1], gg, ALU.mult, ALU.mult), [t_o, t_rd, t_gate], [t_on])

                        def later(i=i, on=on, t_on=t_on):
                            tp, t_tp = tslots.next()
                            pv = tp.bitcast(BF16)
                            tr(pv, on, [t_on], [t_tp])
                            tm, t_tm = tmpf.next()
                            S.op("dve", lambda h, o=tm, p=pv, z=zTs[br][:, i * 128:(i + 1) * 128]: h.tensor_tensor(o, p, z, ALU.mult),
                                 [t_tp, t_zTs[br]], [t_tm])
                            if not last:
                                S.op("dve", lambda h, o=yT[:, i * 128:(i + 1) * 128], a=tm: h.tensor_tensor(o, o, a, ALU.add),
                                     [t_tm, t_yT], [t_yT])
                            else:
                                S.op("dve", lambda h, o=og[:, i * 128:(i + 1) * 128], y=yT[:, i * 128:(i + 1) * 128], a=tm:
                                     h.tensor_tensor(o, y, a, ALU.add), [t_tm, t_yT], [t_og])
                        return later
                    return finish

                if P4CUT <= 4 or P4CUT in (31, 32, 33):
                    break
                attention(QTs[j], t_QTs[j], NOWN, lambda i: max(0, 2 * i - 4), lambda i: 2 * i + 1, lambda i: 2 * i + 1, 2,
                          lambda ki: KwT[:, ki * 128:(ki + 1) * 128], t_kv, lambda ki: Vw[:, ki, 0:129], t_kv,
                          wes, t_wes, pt_ring, mk_finish(2, 32 + hd, False), o_slots)
                if P4CUT <= 5:
                    break
                if j == 0:
                    DQ.flush()

                def extra(ki, ia, ib):
                    return emat[:, ki * 128:(ki + 1) * 128], selmT[:, ia * 128:ib * 128], [t_emat, t_selmT]
                attention(QTs[j], t_QTs[j], NOWN, lambda i: 0, lambda i: 2 * i + 1, lambda i: 2 * i + 1, 2,
                          lambda ki: KsT[:, ki * 128:(ki + 1) * 128], t_kv, lambda ki: Vs[:, ki, 0:129], t_kv,
                          es, t_es, pt_ring, mk_finish(1, 16 + hd, True), o_slots, extra=extra)
                dst = og1_d.ap()[:, :, hd, :].rearrange("t p c -> p t c")

                def spill1(dst=dst, og=og, t_og=t_og):
                    og_out.append(S.dma("pool", dst, og.rearrange("p (t c) -> p t c", c=128), reads=[t_og]))
                DQ.push(spill1)
                if g + 1 < 4:
                    if j == 0:
                        load_kc(g + 1)
                    pass1_head(g + 1, j)
        DQ.flush()
        final_ops = og_out
        S.barrier()
        A.reset(m4)

    if upto >= 5:
        def resid1(t, ring):
            xs, t_xs = ring.next()
            S.dma("sp", xs, h1o_d.ap()[t * 128:(t + 1) * 128, :], writes=[t_xs])
            return xs, t_xs
        final_ops = outproj(w_out_b, 4, og1_d, NOWN, resid1, lambda t: out_d.ap()[t * 128:(t + 1) * 128, :])

    st = S.emit("sp", final_ops)
    return nc, st, A.peak


def rel_bucket_np(dist):
    n = np.maximum(dist, 0)
    nf = np.maximum(n, 1).astype(np.float32)
    lb = 16 + (np.log(nf / np.float32(16)) / np.float32(math.log(2048 / 16)) * np.float32(16)).astype(np.int32)
    return np.where(n < 16, n, np.minimum(lb, 31)).astype(np.int64)


def host_consts(rel_table, par):
    rel_table = np.asarray(rel_table, np.float32)
    k = np.arange(128)[:, None]
    j = np.arange(SW)[None, :]
    d0 = j - k
    idx0 = rel_bucket_np(d0)
    s0 = np.where((d0 >= 0)[None], rel_table[idx0].transpose(2, 0, 1), np.float32(NEG)).astype(np.float32)
    d1 = j - k - 128 * (1 - par)
    idx1 = rel_bucket_np(d1)
    s1 = np.where((d1 >= 0)[None], rel_table[idx1].transpose(2, 0, 1), np.float32(NEG)).astype(np.float32)
    mult = ((d0 >= 0) & (d0 <= 128)).astype(np.float32) + ((d0 >= 0) & (d0 % 4 == 0) & (d0 <= 512)) + \
        ((d0 >= 0) & (d0 % 16 == 0) & (d0 <= 2048))
    logm = np.where(mult > 0, np.log(np.maximum(mult, 1)), NEG).astype(np.float32)
    d1w = d1[:, :768]
    winm = np.where((d1w >= 0) & (d1w < 512), 0.0, NEG).astype(np.float32)
    i = np.arange(NOWN)
    tq = ((2 * i + par)[:, None] * 128 + np.arange(128)[None, :]).reshape(-1)
    c = np.arange(128)
    dc = tq[None, :] - (c[:, None] * 16 + 31)
    bc = np.where(((dc >= 0) & (c[:, None] < 127))[None], rel_table[rel_bucket_np(dc)].transpose(2, 0, 1),
                  np.float32(NEG)).astype(np.float32)
    cur = (tq // 64).reshape(NOWN, 128).T[:, :, None]
    blk = np.arange(32)[None, None, :]
    forced = (blk == 0) | (blk == cur) | (blk == cur - 1)
    invalid = blk > cur
    keep = (~(forced | invalid)).astype(np.float32)
    force = np.where(forced, 1e9 + blk * 1e6, np.where(invalid, -1e9 - blk * 1e6, 0.0)).astype(np.float32)
    topk = np.stack([keep.reshape(128, -1), force.reshape(128, -1)]).astype(np.float32)
    ci = np.arange(128)[:, None] * 16
    sj = np.arange(32)[None, :] * 64
    ovl = ((ci < sj + 64) & (ci + 32 > sj) & (np.arange(128)[:, None] < 127)).astype(np.float32)
    ovl = np.concatenate([ovl, np.ones((128, 1), np.float32)], axis=1)
    emat = (np.arange(2048)[None, :] // 64 == np.arange(32)[:, None]).astype(np.float32)
    blend = np.zeros((128, 2), np.float32)
    blend[:, par] = 1.0
    return dict(strip0=s0, strip1=s1, logm=logm, winmask=winm, biasc=bc, topk=topk, ovl=ovl, emat=emat,
                ident=np.eye(128, dtype=np.float32), blend=blend)


def make_in_maps(inputs):
    f = lambda a: np.ascontiguousarray(np.asarray(a, dtype=np.float32))
    x = f(inputs["x"])
    gains = np.stack([f(inputs["norm_pre"])[0], f(inputs["norm_post"])[0], f(inputs["kv_norm"]),
                      f(inputs["norm_pre"])[1], f(inputs["norm_post"])[1]])
    common = dict(gains=gains, w_in_a=f(inputs["w_in_a"])[0], w_out_a=f(inputs["w_out_a"])[0], w_kv=f(inputs["w_kv"]),
                  w_in_b=f(inputs["w_in_b"])[0], w_out_b=f(inputs["w_out_b"])[0],
                  cw1k=f(inputs["cmp_w1_k"]), cw1v=f(inputs["cmp_w1_v"]), cw2k=f(inputs["cmp_w2_k"]),
                  cw2v=f(inputs["cmp_w2_v"]), cposk=f(inputs["cmp_pos_k"]), cposv=f(inputs["cmp_pos_v"]))
    hc = [host_consts(inputs["rel_table"], par) for par in range(2)]
    maps = []
    for c in range(8):
        m = dict(common)
        m["x"] = x[c // 2]
        m.update(hc[c % 2])
        maps.append(m)
    return maps


_CACHE = {}


def kernel(**inputs):
    if "nc" not in _CACHE:
        _CACHE["nc"] = build()[0]
    nc = _CACHE["nc"]
    maps = make_in_maps(inputs)
    res = run_bass_kernel_spmd(nc, maps, core_ids=list(range(8)))
    out = np.zeros((4, SEQ, D), np.float32)
    for c in range(8):
        o = np.asarray(res.results[c]["out"]).reshape(NOWN, 128, D)
        b, par = c // 2, c % 2
        out[b].reshape(NT, 128, D)[par::2] = o
    return out
```

```python
import math
import os
import numpy as np
import concourse.bass as bass
import concourse.mybir as mybir
from concourse.bass_utils import run_bass_kernel_spmd

F32 = mybir.dt.float32
BF16 = mybir.dt.bfloat16
AF = mybir.ActivationFunctionType
ALU = mybir.AluOpType
AX = mybir.AxisListType

NEG = -30000.0
D = 2048
SEQ = 2048
NH = 16
DH = 128
NT = 16
NOWN = 8
SW = 17 * 128
SCALE = DH ** -0.5
EPS = 1e-6


class T:
    __slots__ = ("w", "r", "name", "dsem", "dcount")

    def __init__(self, name=""):
        self.w = None
        self.r = {}
        self.name = name
        self.dsem = None
        self.dcount = 0


class Op:
    __slots__ = ("eng", "fn", "deps", "marked", "ev_sem", "ev_val", "is_dma")

    def __init__(self, eng, fn, deps, is_dma=False):
        self.eng = eng
        self.fn = fn
        self.deps = deps
        self.marked = False
        self.ev_sem = None
        self.ev_val = None
        self.is_dma = is_dma


class Sched:
    def __init__(self, nc, same_engine_sync=True):
        self.nc = nc
        self.ops = []
        self.h = {"pe": nc.tensor, "act": nc.scalar, "dve": nc.vector, "pool": nc.gpsimd, "sp": nc.sync}
        self.esem = {}
        self.same_engine_sync = same_engine_sync
        self._ctx = []
        for k in self.h:
            cm = nc.semaphore("sem_" + k)
            self.esem[k] = cm.__enter__()
            self._ctx.append(cm)
        self.ndsem = 0
        self.last = {}
        self.dma_since_barrier = []

    def tile_dsem(self, t):
        if t.dsem is None:
            cm = self.nc.semaphore("ds_%d" % self.ndsem)
            self.ndsem += 1
            t.dsem = cm.__enter__()
            self._ctx.append(cm)
        return t.dsem

    def _deps(self, reads, writes, join_sem=None):
        deps = []
        for t in reads:
            if t.w is not None:
                deps.append(t.w)
        for t in writes:
            if t.w is not None and not (join_sem is not None and t.w.is_dma and t.w.ev_sem is join_sem):
                deps.append(t.w)
            deps.extend(t.r.values())
        return deps

    def op(self, eng, fn, reads=(), writes=()):
        o = Op(eng, fn, self._deps(reads, writes))
        for t in reads:
            t.r[eng] = o
        for t in writes:
            t.w = o
            t.r = {}
        self.ops.append(o)
        self.last[eng] = o
        return o

    def dma(self, q, out, in_, reads=(), writes=(), semt=None, **kw):
        if semt is None:
            semt = writes[0] if writes else reads[0]
        sem = self.tile_dsem(semt)

        def fn(h, out=out, in_=in_, kw=kw):
            return h.dma_start(out=out, in_=in_, **kw)

        o = Op(q, fn, self._deps(reads, writes, join_sem=sem), is_dma=True)
        semt.dcount += 16
        o.ev_sem = sem
        o.ev_val = semt.dcount
        key = ("dma", id(sem))
        for t in reads:
            t.r[key] = o
        for t in writes:
            t.w = o
            t.r = {}
        self.ops.append(o)
        self.dma_since_barrier.append(o)
        return o

    def barrier(self):
        deps = list(self.last.values()) + list(self.dma_since_barrier)
        for e in self.h:
            o = Op(e, None, list(deps))
            self.ops.append(o)
        self.dma_since_barrier = []

    def _skip(self, d, o):
        return (not d.is_dma) and d.eng == o.eng and (d.eng == "pe" or not self.same_engine_sync)

    def emit(self, final_wait_eng="sp", final_ops=()):
        for o in self.ops:
            for d in o.deps:
                if not d.is_dma and not self._skip(d, o):
                    d.marked = True
        for d in final_ops:
            if not d.is_dma:
                d.marked = True
        cnt = {k: 0 for k in self.h}
        for o in self.ops:
            if not o.is_dma and o.marked and o.fn is not None:
                cnt[o.eng] += 1
                o.ev_sem = self.esem[o.eng]
                o.ev_val = cnt[o.eng]
        seen = {k: {} for k in self.h}
        nwait = 0
        for o in self.ops:
            h = self.h[o.eng]
            sn = seen[o.eng]
            need = {}
            for d in o.deps:
                if self._skip(d, o) or d.ev_sem is None:
                    continue
                sid = id(d.ev_sem)
                if sn.get(sid, 0) >= d.ev_val:
                    continue
                if sid not in need or need[sid][1] < d.ev_val:
                    need[sid] = (d.ev_sem, d.ev_val)
            for sid, (sem, val) in need.items():
                h.wait_ge(sem, val)
                sn[sid] = val
                nwait += 1
            if o.fn is None:
                continue
            inst = o.fn(h)
            if o.is_dma:
                inst.then_inc(o.ev_sem, 16)
            elif o.marked:
                inst.then_inc(o.ev_sem, 1)
        h = self.h[final_wait_eng]
        for d in final_ops:
            h.wait_ge(d.ev_sem, d.ev_val)
        self.stats = dict(n_ops=len(self.ops), n_wait=nwait, marked=cnt, ndsem=self.ndsem)
        return self.stats


class Arena:
    def __init__(self, nc, nbytes, flat=None):
        self.t = nc.sbuf_tensor("arena", [128, nbytes // 2], BF16).__enter__() if flat is None else flat
        self.off = 0
        self.cap = nbytes
        self.peak = 0

    def alloc(self, shape, dt):
        esz = 4 if dt == F32 else 2
        n = 1
        for s in shape[1:]:
            n *= s
        nb = (n * esz + 31) // 32 * 32
        start = self.off
        self.off += nb
        self.peak = max(self.peak, self.off)
        assert self.off <= self.cap, ("arena overflow", self.off, self.cap)
        ap = self.t[0:shape[0], start // 2: start // 2 + (n * esz) // 2]
        if dt != BF16:
            ap = ap.bitcast(dt)
        if len(shape) == 3:
            ap = ap.rearrange("p (a b) -> p a b", a=shape[1], b=shape[2])
        elif len(shape) == 4:
            ap = ap.rearrange("p (a b c) -> p a b c", a=shape[1], b=shape[2], c=shape[3])
        return ap

    def mark(self):
        return self.off

    def reset(self, m):
        self.off = m


class Ring:
    def __init__(self, aps):
        self.aps = aps
        self.ts = [T() for _ in aps]
        self.i = 0

    def next(self):
        k = self.i % len(self.aps)
        self.i += 1
        return self.aps[k], self.ts[k]


def build(upto=99, dbg=None):
    nc = bass.Bass("TRN2", target_bir_lowering=False)
    S = Sched(nc)
    A = Arena(nc, 200 * 1024)

    def din(name, shape, dt=F32):
        return nc.dram_tensor(name, list(shape), dt, kind="ExternalInput")

    x_d = din("x", [SEQ, D])
    gains_d = din("gains", [5, D])
    w_in_a = din("w_in_a", [D, 8192])
    w_out_a = din("w_out_a", [D, D])
    w_kv = din("w_kv", [D, 3072])
    w_in_b = din("w_in_b", [D, 8240])
    w_out_b = din("w_out_b", [D, D])
    cw1k = din("cw1k", [4096, 256])
    cw1v = din("cw1v", [4096, 256])
    cw2k = din("cw2k", [256, 128])
    cw2v = din("cw2v", [256, 128])
    cposk = din("cposk", [32, 128])
    cposv = din("cposv", [32, 128])
    s0_d = din("strip0", [NH, 128, SW])
    s1_d = din("strip1", [NH, 128, SW])
    logm_d = din("logm", [128, SW])
    winm_d = din("winmask", [128, 768])
    bc_d = din("biasc", [NH, 128, 1024])
    tk_d = din("topk", [2, 128, NOWN * 32])
    ovl_d = din("ovl", [128, 33])
    e_d = din("emat", [32, 2048])
    ident_d = din("ident", [128, 128])
    blend_d = din("blend", [128, 2])
    out_d = nc.dram_tensor("out", [NOWN * 128, D], F32, kind="ExternalOutput")
    og0_d = nc.dram_tensor("og0", [NT, 128, NH, 128], BF16, kind="Internal" if dbg != "og0" else "ExternalOutput")
    h1_d = nc.dram_tensor("h1s", [SEQ, D], F32, kind="Internal" if dbg != "h1" else "ExternalOutput")
    KVW = 2048 + 2048 + 16 * 130 + 16 * 130 + 128 + 176
    kv_d = nc.dram_tensor("kvs", [4, 128, KVW], BF16, kind="Internal" if dbg != "kv" else "ExternalOutput")
    h1o_d = nc.dram_tensor("h1own", [NOWN * 128, D], F32, kind="Internal")
    if dbg == "og1":
        pass
    og1_d = nc.dram_tensor("og1", [NOWN, 128, NH, 128], BF16, kind="Internal" if dbg != "og1" else "ExternalOutput")

    banks = [nc.psum_tensor("bank%d" % i, [128, 512], F32).__enter__() for i in range(8)]
    bankT = [T("bank%d" % i) for i in range(8)]

    ident = A.alloc([128, 128], BF16)
    ident_f = A.alloc([128, 128], F32)
    t_ident = T()
    S.dma("sp", ident_f, ident_d.ap()[:, :], writes=[t_ident])
    S.op("dve", lambda h: h.tensor_copy(ident, ident_f), reads=[t_ident], writes=[t_ident])
    blend = A.alloc([128, 2], F32)
    t_blend = T()
    S.dma("sp", blend, blend_d.ap()[:, :], writes=[t_blend])
    zeros = A.alloc([128, 512], BF16)
    t_zeros = T()
    S.op("dve", lambda h: h.memset(zeros, 0.0), [], [t_zeros])
    persist_mark = A.mark()

    def mm(out, lhsT, rhs, start, stop, reads, writes):
        return S.op("pe", lambda h, o=out, l=lhsT, r=rhs, s=start, e=stop: h.matmul(o, l, r, start=s, stop=e),
                    reads, writes)

    def tr(out, in_, reads, writes):
        return S.op("pe", lambda h, o=out, i=in_: h.transpose(o, i, ident), list(reads) + [t_ident], writes)

    def gain_tile(idx):
        g = A.alloc([128, D], F32)
        tg = T()
        src = bass.AP(gains_d, idx * D, [[0, 128], [1, D]])
        S.dma("sp", g, src, writes=[tg])
        return g, tg

    def norm_to_T(src, t_src, gain, t_gain, dstT, t_dst, col0, junk, t_junk, small, hb_ring, bank_ids):
        ssq, t_ssq = small.next()
        S.op("act", lambda h, j=junk, s=src, a=ssq: h.activation(j, s, AF.Square, accum_out=a[:, 0:1]),
             [t_src], [t_junk, t_ssq])
        S.op("dve", lambda h, a=ssq: h.tensor_scalar(a[:, 1:2], a[:, 0:1], 1.0 / D, EPS, ALU.mult, ALU.add),
             [t_ssq], [t_ssq])
        S.op("act", lambda h, a=ssq: h.activation(a[:, 2:3], a[:, 1:2], AF.Sqrt), [t_ssq], [t_ssq])
        S.op("dve", lambda h, a=ssq: h.reciprocal(a[:, 3:4], a[:, 2:3]), [t_ssq], [t_ssq])
        hb, t_hb = hb_ring.next()
        S.op("dve", lambda h, o=hb, s=src, a=ssq, g=gain: h.scalar_tensor_tensor(o, s, a[:, 3:4], g, ALU.mult, ALU.mult),
             [t_src, t_ssq, t_gain], [t_hb])
        return lambda: norm_stage_b(hb, t_hb, dstT, t_dst, col0, bank_ids)

    def norm_stage_b(hb, t_hb, dstT, t_dst, col0, bank_ids):
        for half in range(2):
            b = bank_ids[half]
            pv = banks[b][:, :].bitcast(BF16).rearrange("p (a c) -> p a c", a=8, c=128)
            for k in range(8):
                c = half * 8 + k
                tr(pv[:, k, :], hb[:, c * 128:(c + 1) * 128], [t_hb], BT(b))
            eng = "act" if half == 0 else "dve"
            dst = dstT[:, half * 8:half * 8 + 8, col0:col0 + 128]
            if eng == "act":
                S.op("act", lambda h, o=dst, i=pv: h.copy(o, i), BT(b), [t_dst[half]])
            else:
                S.op("dve", lambda h, o=dst, i=pv: h.tensor_copy(o, i), BT(b), [t_dst[half]])

    class WPrefetch:
        def __init__(self, reqs, stage_ring, wring, depth, cast_eng):
            self.reqs = reqs
            self.stage_ring, self.wring, self.depth, self.cast_eng = stage_ring, wring, depth, cast_eng
            self.issued = 0
            self.taken = 0
            self.ready = []

        def get(self):
            while self.issued < min(len(self.reqs), self.taken + 1 + self.depth):
                w_d, ncols, c0 = self.reqs[self.issued]
                self.ready.append(load_wslice(w_d, ncols, c0, self.stage_ring, self.wring, cast_eng=self.cast_eng))
                self.issued += 1
            self.taken += 1
            return self.ready.pop(0)

    def load_wslice(w_d, ncols, c0, stage_ring, wring, width=128, cast_eng="pool"):
        st, t_st = stage_ring.next()
        for hh in range(2):
            src = bass.AP(w_d, c0 + hh * 8 * 128 * ncols, [[ncols, 128], [128 * ncols, 8], [1, width]])
            S.dma("sp", st[:, hh * 8:(hh + 1) * 8, 0:width], src, writes=[t_st])
        wb, t_wb = wring.next()
        if cast_eng == "act":
            S.op("act", lambda h, o=wb, i=st, w=width: h.copy(o[:, :, 0:w], i[:, :, 0:w]), [t_st], [t_wb])
        else:
            S.op(cast_eng, lambda h, o=wb, i=st, w=width: h.tensor_copy(o[:, :, 0:w], i[:, :, 0:w]), [t_st], [t_wb])
        return wb, t_wb

    def proj_T(wb, t_wb, srcT, t_srcs, ntok, evac, bank_ids):
        ng = ntok // 512
        for tg in range(ng):
            DQ.tick()
            DQ2.tick()
            b = bank_ids[tg % len(bank_ids)]
            for c in range(16):
                mm(banks[b][:, :], wb[:, c, :], srcT(c, tg * 512, 512), c == 0, c == 15,
                   [t_wb] + t_srcs, BT(b))
            evac(tg, banks[b], BT(b))

    class Deferred:
        def __init__(self):
            self.q = []
            self.t = 0

        def push(self, fn):
            self.q.append((self.t, fn))

        def tick(self, lag=4):
            self.t += 1
            while self.q and self.q[0][0] <= self.t - lag:
                self.q.pop(0)[1]()

        def flush(self):
            while self.q:
                self.q.pop(0)[1]()

    DQ = Deferred()

    class Trickle:
        def __init__(self):
            self.q = []

        def push(self, fn):
            self.q.append(fn)

        def tick(self):
            if self.q:
                self.q.pop(0)()

        def flush(self):
            while self.q:
                self.q.pop(0)()

    DQ2 = Trickle()

    def attention(QT, t_q, n_qt, kt_lo, kt_hi, qbase, qstep, KTt, t_k, Vt, t_v, estrip, t_es,
                  pt_ring, finish, o_slots, extra=None, vw=129, s_banks=(0, 1, 6)):
        es3 = estrip.rearrange("p (n c) -> p n c", c=128)
        step_no = [0]
        for g in range((n_qt + 3) // 4):
            tiles = list(range(4 * g, min(4 * g + 4, n_qt)))
            lo = min(kt_lo(i) for i in tiles)
            hi = max(kt_hi(i) for i in tiles)
            oslot = {}
            for i in tiles:
                oslot[i] = o_slots.next()
            steps = []
            for ki in range(lo, hi + 1):
                act = [i for i in tiles if kt_lo(i) <= ki <= kt_hi(i)]
                if not act:
                    continue
                steps.append((ki, act[0], act[-1] + 1))

            def front(st):
                ki, ia, ib = st
                n = ib - ia
                b = s_banks[step_no[0] % len(s_banks)]
                step_no[0] += 1
                N = n * 128
                mm(banks[b][:, 0:N], KTt(ki), QT[:, ia * 128:ib * 128], True, False, [t_k, t_q], BT(b))
                if extra is not None:
                    el, er, et = extra(ki, ia, ib)
                    mm(banks[b][:, 0:N], el, er, False, False, et, BT(b))
                b0 = qbase(ia) - ki
                if qstep == 1:
                    mm(banks[b][:, 0:N], ident, estrip[:, b0 * 128:(b0 + n) * 128], False, True, [t_es, t_ident], BT(b))
                else:
                    esv = es3[:, b0:b0 + (n - 1) * qstep + 1:qstep, :]
                    mm(banks[b][:, 0:N].rearrange("p (n c) -> p n c", c=128), ident, esv, False, True,
                       [t_es, t_ident], BT(b))
                pt, t_pt = pt_ring.next()
                S.op("act", lambda h, o=pt[:, 0:N], i=banks[b][:, 0:N]: h.activation(o, i, AF.Exp, scale=SCALE),
                     BT(b), [t_pt])
                return (ki, ia, ib, pt, t_pt)

            def back(fr):
                ki, ia, ib, pt, t_pt = fr
                DQ.tick()
                DQ2.tick()
                for i in range(ia, ib):
                    oap, t_o = oslot[i]
                    mm(oap[:, 0:vw], pt[:, (i - ia) * 128:(i - ia + 1) * 128], Vt(ki), ki == kt_lo(i),
                       ki == kt_hi(i), [t_pt, t_v], [t_o])
                    if ki == kt_hi(i):
                        DQ.push(finish(i, oap, t_o))

            fq = []
            for st in steps:
                fq.append(front(st))
                if len(fq) > 2:
                    back(fq.pop(0))
            while fq:
                back(fq.pop(0))

    def make_oslots():
        r = Ring([banks[b][:, :] for b in (2, 3, 4, 5)])
        r.ts = [bankT[b] for b in (2, 3, 4, 5)]
        return r

    tslots = Ring([banks[7][:, 0:64]])
    tslots.ts = [bankT[7]]

    def BT(b):
        return [bankT[b]]

    hT = A.alloc([128, 16, SEQ], BF16)
    t_hT = [[T(), T()] for t in range(NT)]
    p0_mark = A.mark()
    g0, t_g0 = gain_tile(0)
    xs_ring = Ring([A.alloc([128, D], F32) for _ in range(4)])
    hb_ring = Ring([A.alloc([128, D], BF16) for _ in range(2)])
    junk = A.alloc([128, D], BF16)
    t_junk = T()
    small = Ring([A.alloc([128, 4], F32) for _ in range(4)])
    prev_b = None
    for t in range(NT):
        xs, t_xs = xs_ring.next()
        S.dma("sp", xs, x_d.ap()[t * 128:(t + 1) * 128, :], writes=[t_xs])
        stb = norm_to_T(xs, t_xs, g0, t_g0, hT, t_hT[t], t * 128, junk, t_junk, small, hb_ring, (6, 7))
        if prev_b is not None:
            prev_b()
        prev_b = stb
    prev_b()
    S.barrier()
    A.reset(p0_mark)
    final_ops = []
    if dbg == "hT":
        dbg_d = nc.dram_tensor("dbg_hT", [128, 16 * SEQ], BF16, kind="ExternalOutput")
        final_ops = [S.dma("sp", dbg_d.ap()[:, c * SEQ:(c + 1) * SEQ], hT[:, c, :], reads=[x_ for p_ in t_hT for x_ in p_], semt=t_hT[c][0]) for c in range(16)]

    if upto >= 1:
        QT = A.alloc([128, SEQ], BF16); t_QT = T()
        KT = A.alloc([128, SEQ], BF16); t_KT = T()
        VT = A.alloc([128, SEQ], BF16); t_VT = T()
        zT = A.alloc([128, SEQ], BF16); t_zT = T()
        Vaug = A.alloc([128, 16, 130], BF16); t_V = T()
        S.op("dve", lambda h: h.memset(Vaug, 1.0), [], [t_V])
        logm = A.alloc([128, SW], F32); t_logm = T()
        S.dma("sp", logm, logm_d.ap()[:, :], writes=[t_logm])
        S.op("dve", lambda h: h.tensor_scalar(logm, logm, 1.0 / SCALE, None, ALU.mult), [t_logm], [t_logm])
        strip_ring = Ring([A.alloc([128, SW], F32) for _ in range(1)])
        esb_ring = Ring([A.alloc([128, SW], BF16) for _ in range(2)])
        stage_ring = Ring([A.alloc([128, 16, 128], F32) for _ in range(3)])
        wring = Ring([A.alloc([128, 16, 128], BF16) for _ in range(8)])
        pt_ring = Ring([A.alloc([128, 512], BF16) for _ in range(4)])
        og_ring = Ring([A.alloc([128, SEQ], BF16) for _ in range(2)])
        on_ring = Ring([A.alloc([128, 128], BF16) for _ in range(4)])
        rd_ring = Ring([A.alloc([128, 2], F32) for _ in range(4)])
        o_slots = make_oslots()
        hT_all = [x_ for p_ in t_hT for x_ in p_]
        nheads = NH if upto >= 2 or dbg is None else 1
        wpf = WPrefetch([(w_in_a, 8192, k * 2048 + hd * 128) for hd in range(NH) for k in range(4)],
                        stage_ring, wring, 5, "dve")

        def load_head(hd):
            ws = None
            sf, t_sf = strip_ring.next()
            S.dma("sp", sf, s0_d.ap()[hd, :, :], writes=[t_sf])
            es, t_es = esb_ring.next()
            S.op("dve", lambda h, o=es, e=sf: h.scalar_tensor_tensor(o, e, 1.0 / SCALE, logm, ALU.mult, ALU.add),
                 [t_sf, t_logm], [t_es])
            return ws, es, t_es

        nxt = load_head(0)
        for hd in range(NH):
            _, es, t_es = nxt
            wq = wpf.get()
            srcT = lambda c, c0, n: hT[:, c, c0:c0 + n]

            def evac_copy(dst, t_dst):
                def f(tg, bank, t_bank):
                    S.op("dve", lambda h, o=dst[:, tg * 512:(tg + 1) * 512], i=bank[:, :]: h.tensor_copy(o, i),
                         t_bank, [t_dst])
                return f

            def evac_silu(dst, t_dst):
                def f(tg, bank, t_bank):
                    S.op("act", lambda h, o=dst[:, tg * 512:(tg + 1) * 512], i=bank[:, :]: h.activation(o, i, AF.Silu),
                         t_bank, [t_dst])
                return f

            proj_T(wq[0], wq[1], srcT, hT_all, SEQ, evac_copy(QT, t_QT), (6, 7))
            wk = wpf.get()
            proj_T(wk[0], wk[1], srcT, hT_all, SEQ, evac_copy(KT, t_KT), (6, 7))
            wv = wpf.get()
            proj_T(wv[0], wv[1], srcT, hT_all, SEQ, evac_copy(VT, t_VT), (6, 7))
            wz = wpf.get()
            proj_T(wz[0], wz[1], srcT, hT_all, SEQ, evac_silu(zT, t_zT), (6, 7))
            if hd + 1 < NH:
                nxt = load_head(hd + 1)
            for half in range(2):
                b = 6 + half
                pv = banks[b][:, :].bitcast(BF16).rearrange("p (a c) -> p a c", a=8, c=128)
                for k in range(8):
                    t = half * 8 + k
                    tr(pv[:, k, :], VT[:, t * 128:(t + 1) * 128], [t_VT], BT(b))
                S.op("dve", lambda h, o=Vaug[:, half * 8:half * 8 + 8, 0:128], i=pv: h.tensor_copy(o, i),
                     BT(b), [t_V])
            og, t_og = og_ring.next()

            def finish(i, oap, t_o, og=og, t_og=t_og):
                rd, t_rd = rd_ring.next()
                S.op("dve", lambda h, o=rd, a=oap: h.reciprocal(o[:, 0:1], a[:, 128:129]), [t_o], [t_rd])
                on, t_on = on_ring.next()
                S.op("dve", lambda h, o=on, a=oap, r=rd: h.tensor_scalar(o, a[:, 0:128], r[:, 0:1], None, ALU.mult),
                     [t_o, t_rd], [t_on])

                def later(i=i, on=on, t_on=t_on):
                    tp, t_tp = tslots.next()
                    pv = tp.bitcast(BF16)
                    tr(pv, on, [t_on], [t_tp])
                    S.op("dve", lambda h, o=og[:, i * 128:(i + 1) * 128], p=pv, z=zT[:, i * 128:(i + 1) * 128]:
                         h.tensor_tensor(o, p, z, ALU.mult), [t_tp, t_zT], [t_og])
                return later

            attention(QT, t_QT, NT, lambda i: 0, lambda i: i, lambda i: i, 1,
                      lambda ki: KT[:, ki * 128:(ki + 1) * 128], t_KT,
                      lambda ki: Vaug[:, ki, 0:129], t_V, es, t_es, pt_ring, finish, o_slots)
            dst = og0_d.ap()[:, :, hd, :].rearrange("t p c -> p t c")
            def spill(dst=dst, og=og, t_og=t_og):
                o_sp = S.dma("pool", dst, og.rearrange("p (t c) -> p t c", c=128), reads=[t_og])
                if dbg == "og0":
                    final_ops.append(o_sp)
            DQ.push(spill)
        DQ.flush()
        S.barrier()
        A.reset(p0_mark)

    def outproj(w_d, gain_idx, og_d, n_tiles, resid_fn, dst_fn):
        m = A.mark()
        wo = hT
        t_wos = [T() for _ in range(16)]
        st_ring = Ring([A.alloc([128, D], F32) for _ in range(3)])
        for c in range(16):
            st, t_st = st_ring.next()
            S.dma("sp" if c % 2 == 0 else "act", st, w_d.ap()[c * 128:(c + 1) * 128, :], writes=[t_st])
            ce = ("dve", "act")[c % 2]
            if ce == "act":
                S.op("act", lambda h, o=wo[:, c, :], i=st: h.copy(o, i), [t_st], [t_wos[c]])
            else:
                S.op(ce, lambda h, o=wo[:, c, :], i=st: h.tensor_copy(o, i), [t_st], [t_wos[c]])
        gp, t_gp = gain_tile(gain_idx)
        ogt_ring = Ring([A.alloc([128, 16, 128], BF16) for _ in range(2)])
        xs_ring2 = Ring([A.alloc([128, D], F32) for _ in range(2)])
        h1_ring = Ring([A.alloc([128, D], F32) for _ in range(2)])
        small2 = Ring([A.alloc([128, 8], F32) for _ in range(4)])
        junk2 = A.alloc([128, 512], BF16); t_junk2 = T()
        last = []
        for t in range(n_tiles):
            ogt, t_ogt = ogt_ring.next()
            S.dma("sp", ogt, og_d.ap()[t, :, :, :], writes=[t_ogt])
            bs = (0, 1, 2, 3) if t % 2 == 0 else (4, 5, 6, 7)
            for n in range(4):
                b = bs[n]
                for c in range(16):
                    mm(banks[b][:, :], ogt[:, c, :], wo[:, c, n * 512:(n + 1) * 512], c == 0, c == 15,
                       [t_ogt, t_wos[c]], BT(b))
            sm, t_sm = small2.next()
            for n in range(4):
                b = bs[n]
                S.op("act", lambda h, j=junk2, i=banks[b][:, :], a=sm[:, n:n + 1]: h.activation(j, i, AF.Square, accum_out=a),
                     BT(b), [t_junk2, t_sm])
            S.op("dve", lambda h, a=sm: h.tensor_reduce(a[:, 4:5], a[:, 0:4], AX.X, ALU.add), [t_sm], [t_sm])
            S.op("dve", lambda h, a=sm: h.tensor_scalar(a[:, 5:6], a[:, 4:5], 1.0 / D, EPS, ALU.mult, ALU.add), [t_sm], [t_sm])
            S.op("act", lambda h, a=sm: h.activation(a[:, 6:7], a[:, 5:6], AF.Sqrt), [t_sm], [t_sm])
            S.op("dve", lambda h, a=sm: h.reciprocal(a[:, 7:8], a[:, 6:7]), [t_sm], [t_sm])
            res, t_res = resid_fn(t, xs_ring2)
            h1, t_h1 = h1_ring.next()
            for n in range(4):
                b = bs[n]
                S.op("dve", lambda h, o=h1[:, n * 512:(n + 1) * 512], i=banks[b][:, :], a=sm, g=gp[:, n * 512:(n + 1) * 512]:
                     h.scalar_tensor_tensor(o, i, a[:, 7:8], g, ALU.mult, ALU.mult), BT(b) + [t_sm, t_gp], [t_h1])
            S.op("dve", lambda h, o=h1, r=res: h.tensor_tensor(o, o, r, ALU.add), [t_h1, t_res], [t_h1])
            last.append(S.dma("pool", dst_fn(t), h1, reads=[t_h1]))
        S.barrier()
        A.reset(m)
        return last

    if upto >= 2:
        def resid0(t, ring):
            xs, t_xs = ring.next()
            S.dma("sp", xs, x_d.ap()[t * 128:(t + 1) * 128, :], writes=[t_xs])
            return xs, t_xs
        final_ops = outproj(w_out_a, 1, og0_d, NT, resid0, lambda t: h1_d.ap()[t * 128:(t + 1) * 128, :])

    if upto >= 3:
        m3 = A.mark()
        gk, t_gk = gain_tile(2)
        xs_ring = Ring([A.alloc([128, D], F32) for _ in range(4)])
        hb_ring = Ring([A.alloc([128, D], BF16) for _ in range(2)])
        junk = A.alloc([128, D], BF16); t_junk = T()
        small = Ring([A.alloc([128, 4], F32) for _ in range(4)])
        t_hT = [[T(), T()] for t in range(NT)]
        prev_b = None
        for t in range(NT):
            xs, t_xs = xs_ring.next()
            S.dma("sp", xs, h1_d.ap()[t * 128:(t + 1) * 128, :], writes=[t_xs])
            stb = norm_to_T(xs, t_xs, gk, t_gk, hT, t_hT[t], t * 128, junk, t_junk, small, hb_ring, (6, 7))
            if prev_b is not None:
                prev_b()
            prev_b = stb
        prev_b()
        S.barrier()
        A.reset(m3)
        stage_ring = Ring([A.alloc([128, 16, 128], F32) for _ in range(2)])
        wring = Ring([A.alloc([128, 16, 128], BF16) for _ in range(6)])
        w1 = [A.alloc([128, 32, 256], BF16) for _ in range(2)]
        t_w1 = [T(), T()]
        w2 = [A.alloc([128, 2, 128], BF16) for _ in range(2)]
        t_w2 = [T(), T()]
        posT = [A.alloc([128, 32], BF16) for _ in range(2)]
        t_posT = [T(), T()]
        pbias = [A.alloc([128, 2], F32) for _ in range(2)]
        t_pb = [T(), T()]
        ovlb = A.alloc([128, 33], BF16); t_ovl = T()
        for kvi, (w1_d, w2_d, pos_d) in enumerate(((cw1k, cw2k, cposk), (cw1v, cw2v, cposv))):
            for q4 in range(4):
                st, t_st = stage_ring.next()
                stv = st.rearrange("p a b -> p (a b)").rearrange("p (i n) -> p i n", i=8, n=256)
                src = bass.AP(w1_d, q4 * 8 * 128 * 256, [[256, 128], [128 * 256, 8], [1, 256]])
                S.dma("sp", stv, src, writes=[t_st])
                S.op("dve", lambda h, o=w1[kvi][:, q4 * 8:(q4 + 1) * 8, :], i=stv: h.tensor_copy(o, i), [t_st], [t_w1[kvi]])
            st, t_st = stage_ring.next()
            stv = st.rearrange("p a b -> p (a b)")[:, 0:256].rearrange("p (i n) -> p i n", i=2, n=128)
            src = bass.AP(w2_d, 0, [[128, 128], [128 * 128, 2], [1, 128]])
            S.dma("sp", stv, src, writes=[t_st])
            S.op("dve", lambda h, o=w2[kvi], i=stv: h.tensor_copy(o, i), [t_st], [t_w2[kvi]])
            st, t_st = stage_ring.next()
            stf = st.rearrange("p a b -> p (a b)")
            S.dma("sp", stf[0:32, 0:128], pos_d.ap()[:, :], writes=[t_st])
            S.op("dve", lambda h, o=stf[0:32, 256:320].bitcast(BF16), i=stf[0:32, 0:128]: h.tensor_copy(o, i), [t_st], [t_st])
            pv = banks[6][:, 0:16].bitcast(BF16)
            S.op("pe", lambda h, o=pv, i=stf[0:32, 256:320].bitcast(BF16): h.transpose(o, i, ident[0:32, 0:32]),
                 [t_st, t_ident], BT(6))
            S.op("dve", lambda h, o=posT[kvi], i=pv: h.tensor_copy(o, i), BT(6), [t_posT[kvi]])
            for hc in range(2):
                for i in range(32):
                    mm(banks[7][:, hc:hc + 1], w1[kvi][:, i, hc * 128:(hc + 1) * 128], posT[kvi][:, i:i + 1], i == 0, i == 31,
                       [t_w1[kvi], t_posT[kvi]], BT(7))
                S.op("dve", lambda h, o=pbias[kvi][:, hc:hc + 1], i=banks[7][:, hc:hc + 1]: h.tensor_copy(o, i),
                     BT(7), [t_pb[kvi]])
        st, t_st = stage_ring.next()
        stf = st.rearrange("p a b -> p (a b)")
        S.dma("sp", stf[:, 0:33], ovl_d.ap()[:, :], writes=[t_st])
        S.op("dve", lambda h, o=ovlb, i=stf[:, 0:33]: h.tensor_copy(o, i), [t_st], [t_ovl])

        tmpT = [A.alloc([128, SEQ], BF16) for _ in range(4)]
        t_tmpT = [T() for _ in range(4)]
        kvbuf = A.alloc([128, KVW], BF16); t_kv = T()
        O_KS, O_KW, O_VS, O_VW, O_KC, O_VC = 0, 2048, 4096, 4096 + 2080, 4096 + 4160, 4096 + 4160 + 128
        xg = A.alloc([128, 128], F32); t_xg = T()
        x2 = A.alloc([128, 128], F32); t_x2 = T()
        gT = [[A.alloc([128, 128], BF16) for _ in range(2)] for _ in range(2)]
        t_gT = [[T(), T()], [T(), T()]]
        hT_all = [x_ for p_ in t_hT for x_ in p_]
        srcT = lambda c, c0, n: hT[:, c, c0:c0 + n]

        def evac_to(dst, t_dst, eng="dve"):
            def f(tg, bank, t_bank):
                if eng == "dve":
                    S.op("dve", lambda h, o=dst[:, tg * 512:(tg + 1) * 512], i=bank[:, :]: h.tensor_copy(o, i), t_bank, [t_dst])
                else:
                    S.op("act", lambda h, o=dst[:, tg * 512:(tg + 1) * 512], i=bank[:, :]: h.copy(o, i), t_bank, [t_dst])
            return f

        kv_out = []
        wpf3 = WPrefetch([(w_kv, 3072, i * 512 + g * 128) for g in range(4) for i in range(6)], stage_ring, wring, 4, "dve")
        for g in range(4):
            S.op("dve", lambda h, o=kvbuf[:, O_VS:O_KC]: h.memset(o, 1.0), [], [t_kv])
            S.op("dve", lambda h, o=kvbuf[:, O_KC:KVW]: h.memset(o, 0.0), [], [t_kv])
            S.op("dve", lambda h, o=kvbuf[:, O_VC + 128:O_VC + 161], i=ovlb: h.tensor_copy(o, i), [t_ovl], [t_kv])
            dsts = [(tmpT[0], t_tmpT[0]), (tmpT[1], t_tmpT[1]), (kvbuf[:, O_KS:O_KS + 2048], t_kv),
                    (tmpT[2], t_tmpT[2]), (kvbuf[:, O_KW:O_KW + 2048], t_kv), (tmpT[3], t_tmpT[3])]
            for i in range(6):
                wsi = wpf3.get()
                proj_T(wsi[0], wsi[1], srcT, hT_all, SEQ, evac_to(dsts[i][0], dsts[i][1], "dve" if i % 2 == 0 else "act"), (4, 5))
            for which, off in ((2, O_VS), (3, O_VW)):
                aug = kvbuf[:, off:off + 2080].rearrange("p (t c) -> p t c", c=130)
                for half in range(2):
                    b = 6 + half
                    pv = banks[b][:, :].bitcast(BF16).rearrange("p (a c) -> p a c", a=8, c=128)
                    for k in range(8):
                        t = half * 8 + k
                        tr(pv[:, k, :], tmpT[which][:, t * 128:(t + 1) * 128], [t_tmpT[which]], BT(b))
                    S.op("dve", lambda h, o=aug[:, half * 8:half * 8 + 8, 0:128], i=pv: h.tensor_copy(o, i), BT(b), [t_kv])
            for kvi in range(2):
                srcv = tmpT[kvi].rearrange("p (c i) -> p c i", i=16)
                for hc in range(2):
                    b = 4 + hc
                    for i in range(32):
                        mm(banks[b][:, 0:127], w1[kvi][:, i, hc * 128:(hc + 1) * 128],
                           srcv[:, (i // 16):(i // 16) + 127, i % 16], i == 0, i == 31, [t_w1[kvi], t_tmpT[kvi]], BT(b))
                    S.op("act", lambda h, o=xg[:, 0:127], i=banks[b][:, 0:127], bb=pbias[kvi][:, hc:hc + 1]:
                         h.activation(o, i, AF.Identity, bias=bb), BT(b) + [t_pb[kvi]], [t_xg])
                    S.op("dve", lambda h: h.tensor_tensor(x2[:, 0:127], xg[:, 0:127], xg[:, 0:127], ALU.mult), [t_xg], [t_x2])
                    S.op("dve", lambda h: h.tensor_scalar(x2[:, 0:127], x2[:, 0:127], 0.044715, 1.0, ALU.mult, ALU.add), [t_x2], [t_x2])
                    S.op("dve", lambda h: h.tensor_tensor(x2[:, 0:127], x2[:, 0:127], xg[:, 0:127], ALU.mult), [t_x2, t_xg], [t_x2])
                    S.op("act", lambda h: h.activation(x2[:, 0:127], x2[:, 0:127], AF.Sigmoid, scale=1.5957691216057308), [t_x2], [t_x2])
                    S.op("dve", lambda h, o=gT[kvi][hc][:, 0:127]: h.tensor_tensor(o, x2[:, 0:127], xg[:, 0:127], ALU.mult),
                         [t_x2, t_xg], [t_gT[kvi][hc]])
                if kvi == 0:
                    for hc in range(2):
                        mm(banks[6][:, 0:127], w2[0][:, hc, :], gT[0][hc][:, 0:127], hc == 0, hc == 1,
                           [t_w2[0], t_gT[0][hc]], BT(6))
                    S.op("dve", lambda h, o=kvbuf[:, O_KC:O_KC + 127], i=banks[6][:, 0:127]: h.tensor_copy(o, i), BT(6), [t_kv])
                else:
                    for hc in range(2):
                        mm(banks[7][0:127, 0:128], gT[1][hc][:, 0:127], w2[1][:, hc, :], hc == 0, hc == 1,
                           [t_w2[1], t_gT[1][hc]], BT(7))
                    S.op("dve", lambda h, o=kvbuf[0:127, O_VC:O_VC + 128], i=banks[7][0:127, 0:128]: h.tensor_copy(o, i),
                         BT(7), [t_kv])
            kv_out.append(S.dma("pool", kv_d.ap()[g, :, :], kvbuf, reads=[t_kv]))
        final_ops = kv_out
        S.barrier()
        A.reset(m3)

    if upto >= 4:
        m4 = A.mark()
        A2 = Arena(nc, 65536, flat=hT.rearrange("p a b -> p (a b)"))
        hn1T = A2.alloc([128, 16, NOWN * 128], BF16)
        t_hn1 = [[T(), T()] for _ in range(NOWN)]
        gate = A.alloc([128, NOWN, 48], F32); t_gate = T()
        stage_ring = Ring([A.alloc([128, 16, 128], F32) for _ in range(2)])
        wring = Ring([A.alloc([128, 16, 128], BF16) for _ in range(6)])
        m4a = A.mark()
        gp1, t_gp1 = gain_tile(3)
        xs_ring = Ring([A.alloc([128, D], F32) for _ in range(6)])
        hb_ring = Ring([A.alloc([128, D], BF16) for _ in range(2)])
        junk = A.alloc([128, D], BF16); t_junk = T()
        small = Ring([A.alloc([128, 4], F32) for _ in range(4)])
        wg, t_wg = load_wslice(w_in_b, 8240, 8192, stage_ring, wring, width=48)
        prev_b = None
        for i in range(NOWN):
            xs0, t_x0 = xs_ring.next()
            xs1, t_x1 = xs_ring.next()
            S.dma("sp", xs0, h1_d.ap()[(2 * i) * 128:(2 * i + 1) * 128, :], writes=[t_x0])
            S.dma("sp", xs1, h1_d.ap()[(2 * i + 1) * 128:(2 * i + 2) * 128, :], writes=[t_x1])
            S.op("dve", lambda h, a=xs0: h.tensor_scalar(a, a, blend[:, 0:1], None, ALU.mult), [t_x0, t_blend], [t_x0])
            S.op("dve", lambda h, a=xs0, b=xs1: h.scalar_tensor_tensor(a, b, blend[:, 1:2], a, ALU.mult, ALU.add),
                 [t_x0, t_x1, t_blend], [t_x0])
            S.dma("pool", h1o_d.ap()[i * 128:(i + 1) * 128, :], xs0, reads=[t_x0])
            stb = norm_to_T(xs0, t_x0, gp1, t_gp1, hn1T, t_hn1[i], i * 128, junk, t_junk, small, hb_ring, (6, 7))

            def stage_b(i=i, stb=stb):
                stb()
                b = 4 + (i % 2)
                for c in range(16):
                    mm(banks[b][:, 0:48], hn1T[:, c, i * 128:(i + 1) * 128], wg[:, c, 0:48], c == 0, c == 15,
                       t_hn1[i] + [t_wg], BT(b))
                S.op("act", lambda h, o=gate[:, i, :], p=banks[b][:, 0:48]: h.activation(o, p, AF.Sigmoid), BT(b), [t_gate])
            if prev_b is not None:
                prev_b()
            prev_b = stage_b
        prev_b()
        S.barrier()
        A.reset(m4a)
        tkc = A.alloc([128, 2, NOWN, 32], F32); t_tkc = T()
        S.dma("sp", tkc.rearrange("p a b c -> p a (b c)"), tk_d.ap().rearrange("a p n -> p a n"), writes=[t_tkc])
        emat = A.alloc([128, 2048], BF16); t_emat = T()
        S.op("dve", lambda h: h.memset(emat, 0.0), [], [t_emat])
        for q4 in range(2):
            st, t_st = stage_ring.next()
            stf = st.rearrange("p a b -> p (a b)")
            S.dma("sp", stf[0:32, 0:1024], e_d.ap()[:, q4 * 1024:(q4 + 1) * 1024], writes=[t_st])
            S.op("dve", lambda h, o=emat[0:32, q4 * 1024:(q4 + 1) * 1024], i=stf[0:32, 0:1024]: h.tensor_copy(o, i), [t_st, t_emat], [t_emat])
        winm = A.alloc([128, 768], F32); t_winm = T()
        S.dma("sp", winm, winm_d.ap()[:, :], writes=[t_winm])
        S.op("dve", lambda h: h.tensor_scalar(winm, winm, 1.0 / SCALE, None, ALU.mult), [t_winm], [t_winm])
        kvbuf = A2.alloc([128, KVW], BF16); t_kv = T()
        O_KS, O_KW, O_VS, O_VW, O_KC, O_VC = 0, 2048, 4096, 4096 + 2080, 4096 + 4160, 4096 + 4160 + 128
        QTs = [A2.alloc([128, NOWN * 128], BF16) for _ in range(4)]
        t_QTs = [T() for _ in range(4)]
        ocn = [A.alloc([128, NOWN, 128], BF16) for _ in range(4)]
        t_ocn = [T() for _ in range(4)]
        zsets = [[A.alloc([128, NOWN * 128], BF16) for _ in range(3)] for _ in range(2)]
        t_zsets = [[T() for _ in range(3)] for _ in range(2)]
        sf_ring = Ring([A.alloc([128, SW], F32) for _ in range(1)])
        es_ring = Ring([A.alloc([128, SW], BF16) for _ in range(2)])
        wes_ring = Ring([A.alloc([128, 768], BF16) for _ in range(2)])
        bcf_ring = Ring([A.alloc([128, NOWN * 128], F32) for _ in range(1)])
        bc_ring = Ring([A.alloc([128, NOWN * 128], BF16) for _ in range(2)])
        yTs = [A2.alloc([128, NOWN * 128], F32), A.alloc([128, NOWN * 128], F32)]
        t_yTs = [T(), T()]
        tmpf = Ring([A.alloc([128, 128], F32) for _ in range(3)])
        og_ring = Ring([A.alloc([128, NOWN * 128], BF16) for _ in range(2)])
        pt_ring = Ring([A.alloc([128, 512], BF16) for _ in range(4)])
        on_ring = Ring([A.alloc([128, 128], BF16) for _ in range(4)])
        rd_ring = Ring([A.alloc([128, 4], F32) for _ in range(6)])
        imp = A.alloc([128, NOWN, 32], F32); t_imp = T()
        impf = A.alloc([128, 32], F32); t_impf = T()
        imp2 = A.alloc([128, 32], F32); t_imp2 = T()
        m8 = A.alloc([128, 16], F32); t_m8 = T()
        selb_ring = Ring([A.alloc([128, 32], BF16) for _ in range(8)])
        selmT = A.alloc([128, NOWN * 128], BF16); t_selmT = T()
        S.op("dve", lambda h: h.memset(selmT, 0.0), [], [t_selmT])
        o_slots = make_oslots()
        hn_all = [x_ for p_ in t_hn1 for x_ in p_]
        srcT1 = lambda c, c0, n: hn1T[:, c, c0:c0 + n]
        KsT = kvbuf[:, O_KS:O_KS + 2048]
        KwT = kvbuf[:, O_KW:O_KW + 2048]
        Vs = kvbuf[:, O_VS:O_VS + 2080].rearrange("p (t c) -> p t c", c=130)
        Vw = kvbuf[:, O_VW:O_VW + 2080].rearrange("p (t c) -> p t c", c=130)
        KcT = kvbuf[:, O_KC:O_KC + 128]
        Vc = kvbuf[:, O_VC:O_VC + 161]

        def evac_copy1(dst, t_dst):
            def f(tg, bank, t_bank):
                S.op("dve", lambda h, o=dst[:, tg * 512:(tg + 1) * 512], i=bank[:, :]: h.tensor_copy(o, i), t_bank, [t_dst])
            return f

        def evac_silu1(dst, t_dst):
            def f(tg, bank, t_bank):
                S.op("act", lambda h, o=dst[:, tg * 512:(tg + 1) * 512], i=bank[:, :]: h.activation(o, i, AF.Silu), t_bank, [t_dst])
            return f

        og_out = []
        reqs4 = [(w_in_b, 8240, j_ * 128) for j_ in range(4)]
        for g_ in range(4):
            for j_ in range(4):
                for br_ in range(3):
                    reqs4.append((w_in_b, 8240, 2048 + br_ * 2048 + (4 * g_ + j_) * 128))
                if g_ < 3:
                    reqs4.append((w_in_b, 8240, (4 * (g_ + 1) + j_) * 128))
        wpf4 = WPrefetch(reqs4, stage_ring, wring, 4, "act")
        P4CUT = 99
        imps = [imp, A.alloc([128, NOWN, 32], F32)]
        t_imps = [t_imp, T()]
        kcbufs = [A.alloc([128, KVW - O_KC], BF16) for _ in range(2)]
        t_kcs = [T(), T()]

        def load_kc(g):
            S.dma("sp", kcbufs[g % 2], kv_d.ap()[g, :, O_KC:KVW], writes=[t_kcs[g % 2]])
            S.op("dve", lambda h, o=imps[g % 2]: h.memset(o, 0.0), [], [t_imps[g % 2]])

        ptc = [A.alloc([128, 512], BF16) for _ in range(2)]
        t_ptc = [T(), T()]

        def pass1_head(g, j):
            DQ2.flush()
            hd = 4 * g + j
            kc, t_kc = kcbufs[g % 2], t_kcs[g % 2]
            KcT_g = kc[:, 0:128]
            Vc_g = kc[:, 128:128 + 161]
            imp_g, t_imp_g = imps[g % 2], t_imps[g % 2]
            wq = wpf4.get()
            bcf, t_bcf = bcf_ring.next()
            S.dma("sp", bcf, bc_d.ap()[hd, :, :], writes=[t_bcf])
            bc, t_bc = bc_ring.next()
            S.op("act", lambda h, o=bc, e=bcf: h.activation(o, e, AF.Copy, scale=1.0 / SCALE), [t_bcf], [t_bc])
            proj_T(wq[0], wq[1], srcT1, hn_all, NOWN * 128, evac_copy1(QTs[j], t_QTs[j]), (6, 7))
            for half in range(2):
                b = half
                mm(banks[b][0:127, :], KcT_g[:, 0:127], QTs[j][:, half * 512:(half + 1) * 512], True, False,
                   [t_kc, t_QTs[j]], BT(b))
                mm(banks[b][0:127, :], ident[0:127, 0:127], bc[0:127, half * 512:(half + 1) * 512], False, True,
                   [t_bc, t_ident], BT(b))
                pt, t_pt = ptc[half], t_ptc[half]
                S.op("act", lambda h, o=pt[0:127, :], i=banks[b][0:127, :]: h.activation(o, i, AF.Exp, scale=SCALE),
                     BT(b), [t_pt])
                for ii in range(4):
                    def tile_work(ii=ii, half=half, pt=pt, t_pt=t_pt):
                        i = half * 4 + ii
                        ob = 7 if g > 0 else 2 + ii
                        mm(banks[ob][:, 0:161], pt[0:127, ii * 128:(ii + 1) * 128], Vc_g[0:127, :], True, True,
                           [t_pt, t_kc], BT(ob))
                        rd, t_rd = rd_ring.next()
                        S.op("dve", lambda h, o=rd, a=banks[ob]: h.tensor_scalar(o[:, 0:1], a[:, 160:161], 1e-30, None, ALU.max),
                             BT(ob), [t_rd])
                        S.op("dve", lambda h, o=rd: h.reciprocal(o[:, 1:2], o[:, 0:1]), [t_rd], [t_rd])
                        S.op("dve", lambda h, o=imp_g[:, i, :], a=banks[ob], r=rd: h.scalar_tensor_tensor(o, a[:, 128:160], r[:, 1:2], o, ALU.mult, ALU.add),
                             BT(ob) + [t_rd, t_imp_g], [t_imp_g])
                        S.op("dve", lambda h, r=rd, gg=gate[:, i, hd:hd + 1]: h.tensor_tensor(r[:, 2:3], r[:, 1:2], gg, ALU.mult),
                             [t_rd, t_gate], [t_rd])
                        S.op("dve", lambda h, o=ocn[j][:, i, :], a=banks[ob], r=rd: h.tensor_scalar(o, a[:, 0:128], r[:, 2:3], None, ALU.mult),
                             BT(ob) + [t_rd], [t_ocn[j]])
                    if g > 0:
                        DQ2.push(tile_work)
                    else:
                        tile_work()

        load_kc(0)
        for j in range(4):
            pass1_head(0, j)
        DQ2.flush()
        for g in range(4):
            S.dma("sp", kvbuf, kv_d.ap()[g, :, :], writes=[t_kv])
            imp, t_imp = imps[g % 2], t_imps[g % 2]
            def zproj(hd):
                for br in range(3):
                    wz = wpf4.get()
                    proj_T(wz[0], wz[1], srcT1, hn_all, NOWN * 128, evac_silu1(zsets[hd % 2][br], t_zsets[hd % 2][br]), (6, 7))

            def prep_strips(hd):
                sf, t_sf = sf_ring.next()
                S.dma("sp", sf, s1_d.ap()[hd, :, :], writes=[t_sf])
                es, t_es = es_ring.next()
                wes, t_wes = wes_ring.next()
                S.op("dve", lambda h, o=wes, e=sf: h.scalar_tensor_tensor(o, e[:, 0:768], 1.0 / SCALE, winm, ALU.mult, ALU.add),
                     [t_sf, t_winm], [t_wes])
                S.op("act", lambda h, o=es, e=sf: h.activation(o, e, AF.Copy, scale=1.0 / SCALE), [t_sf], [t_es])
                return es, t_es, wes, t_wes

            if P4CUT > 3:
                nxt_strips = prep_strips(4 * g)
                zproj(4 * g)
            DQ2.flush()
            if P4CUT <= 2:
                break
            for i in range(NOWN):
                S.op("dve", lambda h, a=imp[:, i, :], k=tkc[:, 0, i, :]: h.tensor_tensor(impf, a, k, ALU.mult), [t_imp, t_tkc], [t_impf])
                S.op("dve", lambda h, f=tkc[:, 1, i, :]: h.tensor_tensor(impf, impf, f, ALU.add), [t_impf, t_tkc], [t_impf])
                S.op("dve", lambda h: h.max(m8[:, 0:8], impf), [t_impf], [t_m8])
                S.op("dve", lambda h: h.match_replace(imp2, m8[:, 0:8], impf, -3e9), [t_impf, t_m8], [t_imp2])
                S.op("dve", lambda h: h.max(m8[:, 8:16], imp2), [t_imp2], [t_m8])
                S.op("dve", lambda h: h.tensor_scalar(imp2, impf, m8[:, 15:16], None, ALU.is_ge), [t_impf, t_m8], [t_imp2])
                sb, t_sb = selb_ring.next()
                S.op("dve", lambda h, sb=sb: h.tensor_scalar(sb, imp2, 29952.0, -29952.0, ALU.mult, ALU.add), [t_imp2], [t_sb])
                def selT_later(i=i, sb=sb, t_sb=t_sb):
                    tp, t_tp = tslots.next()
                    pv = tp[0:32, :].bitcast(BF16)
                    tr(pv, sb, [t_sb], [t_tp])
                    S.op("dve", lambda h, o=selmT[0:32, i * 128:(i + 1) * 128], p=pv: h.tensor_copy(o, p), [t_tp, t_selmT], [t_selmT])
                DQ.push(selT_later)
            if P4CUT <= 3 and P4CUT < 30:
                break
            for j in range(4 if P4CUT > 10 else 1):
                hd = 4 * g + j
                if j > 0:
                    zproj(hd)
                zTs, t_zTs = zsets[hd % 2], t_zsets[hd % 2]
                yT, t_yT = yTs[hd % 2], t_yTs[hd % 2]
                es, t_es, wes, t_wes = nxt_strips
                if j < 3:
                    nxt_strips = prep_strips(hd + 1)
                if P4CUT == 32:
                    break
                og, t_og = og_ring.next()
                pv8 = banks[7][:, :].bitcast(BF16)
                for i in range(NOWN):
                    tr(pv8[:, i * 128:(i + 1) * 128], ocn[j][:, i, :], [t_ocn[j]], BT(7))
                S.op("dve", lambda h, o=yT, p=pv8, z=zTs[0]: h.tensor_tensor(o, p, z, ALU.mult), BT(7) + [t_zTs[0]], [t_yT])

                def mk_finish(br, gcol, last, zTs=zTs, t_zTs=t_zTs, yT=yT, t_yT=t_yT):
                    def finish(i, oap, t_o, og=og, t_og=t_og):
                        rd, t_rd = rd_ring.next()
                        S.op("dve", lambda h, o=rd, a=oap: h.reciprocal(o[:, 0:1], a[:, 128:129]), [t_o], [t_rd])
                        on, t_on = on_ring.next()
                        S.op("dve", lambda h, o=on, a=oap, r=rd, gg=gate[:, i, gcol:gcol + 1]:
                             h.tensor_scalar(o, a[:, 0:128], r[:, 0:1], gg, ALU.mult, ALU.mult), [t_o, t_rd, t_gate], [t_on])

                        def later(i=i, on=on, t_on=t_on):
                            tp, t_tp = tslots.next()
                            pv = tp.bitcast(BF16)
                            tr(pv, on, [t_on], [t_tp])
                            tm, t_tm = tmpf.next()
                            S.op("dve", lambda h, o=tm, p=pv, z=zTs[br][:, i * 128:(i + 1) * 128]: h.tensor_tensor(o, p, z, ALU.mult),
                                 [t_tp, t_zTs[br]], [t_tm])
                            if not last:
                                S.op("dve", lambda h, o=yT[:, i * 128:(i + 1) * 128], a=tm: h.tensor_tensor(o, o, a, ALU.add),
                                     [t_tm, t_yT], [t_yT])
                            else:
                                S.op("dve", lambda h, o=og[:, i * 128:(i + 1) * 128], y=yT[:, i * 128:(i + 1) * 128], a=tm:
                                     h.tensor_tensor(o, y, a, ALU.add), [t_tm, t_yT], [t_og])
                        return later
                    return finish

                if P4CUT <= 4 or P4CUT in (31, 32, 33):
                    break
                attention(QTs[j], t_QTs[j], NOWN, lambda i: max(0, 2 * i - 4), lambda i: 2 * i + 1, lambda i: 2 * i + 1, 2,
                          lambda ki: KwT[:, ki * 128:(ki + 1) * 128], t_kv, lambda ki: Vw[:, ki, 0:129], t_kv,
                          wes, t_wes, pt_ring, mk_finish(2, 32 + hd, False), o_slots)
                if P4CUT <= 5:
                    break
                if j == 0:
                    DQ.flush()

                def extra(ki, ia, ib):
                    return emat[:, ki * 128:(ki + 1) * 128], selmT[:, ia * 128:ib * 128], [t_emat, t_selmT]
                attention(QTs[j], t_QTs[j], NOWN, lambda i: 0, lambda i: 2 * i + 1, lambda i: 2 * i + 1, 2,
                          lambda ki: KsT[:, ki * 128:(ki + 1) * 128], t_kv, lambda ki: Vs[:, ki, 0:129], t_kv,
                          es, t_es, pt_ring, mk_finish(1, 16 + hd, True), o_slots, extra=extra)
                dst = og1_d.ap()[:, :, hd, :].rearrange("t p c -> p t c")

                def spill1(dst=dst, og=og, t_og=t_og):
                    og_out.append(S.dma("pool", dst, og.rearrange("p (t c) -> p t c", c=128), reads=[t_og]))
                DQ.push(spill1)
                if g + 1 < 4:
                    if j == 0:
                        load_kc(g + 1)
                    pass1_head(g + 1, j)
        DQ.flush()
        final_ops = og_out
        S.barrier()
        A.reset(m4)

    if upto >= 5:
        def resid1(t, ring):
            xs, t_xs = ring.next()
            S.dma("sp", xs, h1o_d.ap()[t * 128:(t + 1) * 128, :], writes=[t_xs])
            return xs, t_xs
        final_ops = outproj(w_out_b, 4, og1_d, NOWN, resid1, lambda t: out_d.ap()[t * 128:(t + 1) * 128, :])

    st = S.emit("sp", final_ops)
    return nc, st, A.peak


def rel_bucket_np(dist):
    n = np.maximum(dist, 0)
    nf = np.maximum(n, 1).astype(np.float32)
    lb = 16 + (np.log(nf / np.float32(16)) / np.float32(math.log(2048 / 16)) * np.float32(16)).astype(np.int32)
    return np.where(n < 16, n, np.minimum(lb, 31)).astype(np.int64)


def host_consts(rel_table, par):
    rel_table = np.asarray(rel_table, np.float32)
    k = np.arange(128)[:, None]
    j = np.arange(SW)[None, :]
    d0 = j - k
    idx0 = rel_bucket_np(d0)
    s0 = np.where((d0 >= 0)[None], rel_table[idx0].transpose(2, 0, 1), np.float32(NEG)).astype(np.float32)
    d1 = j - k - 128 * (1 - par)
    idx1 = rel_bucket_np(d1)
    s1 = np.where((d1 >= 0)[None], rel_table[idx1].transpose(2, 0, 1), np.float32(NEG)).astype(np.float32)
    mult = ((d0 >= 0) & (d0 <= 128)).astype(np.float32) + ((d0 >= 0) & (d0 % 4 == 0) & (d0 <= 512)) + \
        ((d0 >= 0) & (d0 % 16 == 0) & (d0 <= 2048))
    logm = np.where(mult > 0, np.log(np.maximum(mult, 1)), NEG).astype(np.float32)
    d1w = d1[:, :768]
    winm = np.where((d1w >= 0) & (d1w < 512), 0.0, NEG).astype(np.float32)
    i = np.arange(NOWN)
    tq = ((2 * i + par)[:, None] * 128 + np.arange(128)[None, :]).reshape(-1)
    c = np.arange(128)
    dc = tq[None, :] - (c[:, None] * 16 + 31)
    bc = np.where(((dc >= 0) & (c[:, None] < 127))[None], rel_table[rel_bucket_np(dc)].transpose(2, 0, 1),
                  np.float32(NEG)).astype(np.float32)
    cur = (tq // 64).reshape(NOWN, 128).T[:, :, None]
    blk = np.arange(32)[None, None, :]
    forced = (blk == 0) | (blk == cur) | (blk == cur - 1)
    invalid = blk > cur
    keep = (~(forced | invalid)).astype(np.float32)
    force = np.where(forced, 1e9 + blk * 1e6, np.where(invalid, -1e9 - blk * 1e6, 0.0)).astype(np.float32)
    topk = np.stack([keep.reshape(128, -1), force.reshape(128, -1)]).astype(np.float32)
    ci = np.arange(128)[:, None] * 16
    sj = np.arange(32)[None, :] * 64
    ovl = ((ci < sj + 64) & (ci + 32 > sj) & (np.arange(128)[:, None] < 127)).astype(np.float32)
    ovl = np.concatenate([ovl, np.ones((128, 1), np.float32)], axis=1)
    emat = (np.arange(2048)[None, :] // 64 == np.arange(32)[:, None]).astype(np.float32)
    blend = np.zeros((128, 2), np.float32)
    blend[:, par] = 1.0
    return dict(strip0=s0, strip1=s1, logm=logm, winmask=winm, biasc=bc, topk=topk, ovl=ovl, emat=emat,
                ident=np.eye(128, dtype=np.float32), blend=blend)


def make_in_maps(inputs):
    f = lambda a: np.ascontiguousarray(np.asarray(a, dtype=np.float32))
    x = f(inputs["x"])
    gains = np.stack([f(inputs["norm_pre"])[0], f(inputs["norm_post"])[0], f(inputs["kv_norm"]),
                      f(inputs["norm_pre"])[1], f(inputs["norm_post"])[1]])
    common = dict(gains=gains, w_in_a=f(inputs["w_in_a"])[0], w_out_a=f(inputs["w_out_a"])[0], w_kv=f(inputs["w_kv"]),
                  w_in_b=f(inputs["w_in_b"])[0], w_out_b=f(inputs["w_out_b"])[0],
                  cw1k=f(inputs["cmp_w1_k"]), cw1v=f(inputs["cmp_w1_v"]), cw2k=f(inputs["cmp_w2_k"]),
                  cw2v=f(inputs["cmp_w2_v"]), cposk=f(inputs["cmp_pos_k"]), cposv=f(inputs["cmp_pos_v"]))
    hc = [host_consts(inputs["rel_table"], par) for par in range(2)]
    maps = []
    for c in range(8):
        m = dict(common)
        m["x"] = x[c // 2]
        m.update(hc[c % 2])
        maps.append(m)
    return maps


_CACHE = {}


def kernel(**inputs):
    if "nc" not in _CACHE:
        _CACHE["nc"] = build()[0]
    nc = _CACHE["nc"]
    maps = make_in_maps(inputs)
    res = run_bass_kernel_spmd(nc, maps, core_ids=list(range(8)))
    out = np.zeros((4, SEQ, D), np.float32)
    for c in range(8):
        o = np.asarray(res.results[c]["out"]).reshape(NOWN, 128, D)
        b, par = c // 2, c % 2
        out[b].reshape(NT, 128, D)[par::2] = o
    return out
```
